# Optimizing a Trainium2 kernel written in Bass

```python
import math
import jax, jax.numpy as jnp
from jax import lax
import numpy as np

D_MODEL = 1024
BATCH = 8
SEQ = 4096
DEPTH = 2

CHUNK = 64
Q_BLOCK = 128
N_GROUPS = 4
GROUP_WIDTH = D_MODEL // N_GROUPS
D_MIX = N_GROUPS * GROUP_WIDTH
HEAD_DIM = 64
N_HEADS_G = GROUP_WIDTH // HEAD_DIM
DIFF_D = HEAD_DIM // 2
Q_LORA = 256
KV_LORA = 128
QK_NOPE = 64
QK_ROPE = 32
V_HEAD = HEAD_DIM
ROPE_THETA = 10000.0
IDX_HEADS = 8
IDX_DIM = 32
TOPK_MAX = 256
BAND_CHUNKS = 8
REL_CLIP = 128
D_FF = 2816
N_EXPERTS = 8
TOP_K = 2
D_FF_EXPERT = 3584
N_DENSE = (DEPTH + 1) // 2
N_MOE = DEPTH // 2
EPS = 1e-6

IN_LAYOUT = (
    ('a_q', N_HEADS_G * 2 * DIFF_D), ('a_k', N_HEADS_G * 2 * DIFF_D), ('a_v', N_HEADS_G * 2 * DIFF_D),
    ('b_qd', Q_LORA), ('b_kvd', KV_LORA), ('b_kr', QK_ROPE),
    ('c_q', GROUP_WIDTH), ('c_k', GROUP_WIDTH), ('c_v', GROUP_WIDTH),
    ('c_qi', IDX_HEADS * IDX_DIM), ('c_ki', IDX_DIM), ('c_wi', IDX_HEADS),
    ('d_q', GROUP_WIDTH), ('d_k', GROUP_WIDTH), ('d_v', GROUP_WIDTH),
)
D_IN = int(sum(w for _, w in IN_LAYOUT))
IN_SPLITS = tuple(int(v) for v in np.cumsum([w for _, w in IN_LAYOUT])[:-1])

kernel_name = 'hybrid_chunk_causal_encoder'


def rmsnorm(x, g):
    xf = x.astype(jnp.float32)
    y = xf * lax.rsqrt(jnp.mean(xf * xf, axis=-1, keepdims=True) + EPS)
    return (y * g.astype(jnp.float32)).astype(x.dtype)


def adaln(x, g, shift, scale):
    return rmsnorm(x, g) * (1 + scale[:, None, :]) + shift[:, None, :]


def alibi_slopes(n):
    return 2.0 ** (-8.0 * jnp.arange(1, n + 1, dtype=jnp.float32) / n)


def rope_tables(s):
    inv = ROPE_THETA ** (-jnp.arange(0, QK_ROPE, 2, dtype=jnp.float32) / QK_ROPE)
    ang = jnp.arange(s, dtype=jnp.float32)[:, None] * inv[None, :]
    return jnp.cos(ang), jnp.sin(ang)


def apply_rope(x, cos, sin):
    x1, x2 = jnp.split(x, 2, axis=-1)
    cs = cos[None, :, None, :].astype(x.dtype)
    sn = sin[None, :, None, :].astype(x.dtype)
    return jnp.concatenate([x1 * cs - x2 * sn, x1 * sn + x2 * cs], axis=-1)


def to_blocks(a):
    b, s = a.shape[:2]
    return jnp.moveaxis(a.reshape(b, s // Q_BLOCK, Q_BLOCK, *a.shape[2:]), 1, 0)


def from_blocks(a):
    nb, b, qb = a.shape[:3]
    return jnp.moveaxis(a, 0, 1).reshape(b, nb * qb, *a.shape[3:])


def sweep_query_blocks(fn, *qs):
    nb = qs[0].shape[1] // Q_BLOCK
    out = lax.map(lambda a: fn(*a), (jnp.arange(nb), *[to_blocks(q) for q in qs]))
    return from_blocks(out)


def chunk_mask(blk, s_len):
    t = blk * Q_BLOCK + jnp.arange(Q_BLOCK)
    s = jnp.arange(s_len)
    return t, s, (s[None, :] // CHUNK) <= (t[:, None] // CHUNK)


def diff_attention(q, k, v, lam_vecs, norm_g, layer_idx):
    b, s_len, h = q.shape[:3]
    lam_init = 0.8 - 0.6 * math.exp(-0.3 * layer_idx)
    lv = lam_vecs.astype(jnp.float32)
    lam = jnp.exp(jnp.sum(lv[0] * lv[1])) - jnp.exp(jnp.sum(lv[2] * lv[3])) + lam_init
    slopes = alibi_slopes(h)
    scale = DIFF_D ** -0.5

    def block(blk, qb):
        t, s, mask = chunk_mask(blk, s_len)
        bias = -slopes[:, None, None] * jnp.abs(t[:, None] - s[None, :]).astype(jnp.float32)
        bias = jnp.where(mask[None], bias, -jnp.inf)
        logits = jnp.einsum('bqhcd,bshcd->bchqs', qb, k).astype(jnp.float32) * scale + bias[None, None]
        p = jax.nn.softmax(logits, axis=-1)
        p = p[:, 0] - lam * p[:, 1]
        return jnp.einsum('bhqs,bshe->bqhe', p.astype(v.dtype), v)

    o = sweep_query_blocks(block, q)
    o = rmsnorm(o, norm_g) * (1.0 - lam_init)
    return o.reshape(b, s_len, h * 2 * DIFF_D)


def latent_attention(qd, kvd, kr, q_norm_g, kv_norm_g, w_uq, w_ukv, cos, sin):
    b, s_len = qd.shape[:2]
    h = N_HEADS_G
    q = (rmsnorm(qd, q_norm_g) @ w_uq).reshape(b, s_len, h, QK_NOPE + QK_ROPE)
    q = jnp.concatenate([q[..., :QK_NOPE], apply_rope(q[..., QK_NOPE:], cos, sin)], axis=-1)
    kv = (rmsnorm(kvd, kv_norm_g) @ w_ukv).reshape(b, s_len, h, QK_NOPE + V_HEAD)
    k_nope, v = kv[..., :QK_NOPE], kv[..., QK_NOPE:]
    k_rope = apply_rope(kr[:, :, None, :], cos, sin)
    k = jnp.concatenate([k_nope, jnp.broadcast_to(k_rope, (b, s_len, h, QK_ROPE))], axis=-1)
    scale = (QK_NOPE + QK_ROPE) ** -0.5

    def block(blk, qb):
        _, _, mask = chunk_mask(blk, s_len)
        logits = jnp.einsum('bqhd,bshd->bhqs', qb, k).astype(jnp.float32) * scale
        logits = jnp.where(mask[None, None], logits, -jnp.inf)
        p = jax.nn.softmax(logits, axis=-1)
        return jnp.einsum('bhqs,bshd->bqhd', p.astype(v.dtype), v)

    return sweep_query_blocks(block, q).reshape(b, s_len, h * V_HEAD)


def indexed_sparse_attention(q, k, v, q_idx, k_idx, w_idx):
    b, s_len, h, dh = q.shape
    n_sel = min(TOPK_MAX, s_len // 4)
    slopes = alibi_slopes(h)
    scale = dh ** -0.5
    w = w_idx.astype(jnp.float32) * (IDX_HEADS ** -0.5 * IDX_DIM ** -0.5)

    def block(blk, qb, qib, wb):
        t, _, mask = chunk_mask(blk, s_len)
        score = jax.nn.relu(jnp.einsum('bqhd,bsd->bqhs', qib, k_idx).astype(jnp.float32))
        score = jnp.einsum('bqhs,bqh->bqs', score, wb)
        score = jnp.where(mask[None], score, -jnp.inf)
        top_val, top_idx = lax.top_k(score, n_sel)
        valid = jnp.isfinite(top_val)
        k_sel = jax.vmap(lambda kk, ii: kk[ii])(k, top_idx)
        v_sel = jax.vmap(lambda vv, ii: vv[ii])(v, top_idx)
        logits = jnp.einsum('bqhd,bqnhd->bhqn', qb, k_sel).astype(jnp.float32) * scale
        dist = jnp.abs(t[None, :, None] - top_idx).astype(jnp.float32)
        logits = logits - slopes[None, :, None, None] * dist[:, None]
        logits = jnp.where(valid[:, None], logits, -jnp.inf)
        p = jax.nn.softmax(logits, axis=-1)
        return jnp.einsum('bhqn,bqnhd->bqhd', p.astype(v.dtype), v_sel)

    return sweep_query_blocks(block, q, q_idx, w).reshape(b, s_len, h * dh)


def chunk_band_attention(q, k, v, rel_bias):
    b, s_len, h, dh = q.shape
    nc = s_len // CHUNK
    band = (BAND_CHUNKS + 1) * CHUNK
    qc = q.reshape(b, nc, CHUNK, h, dh)
    pad = ((0, 0), (BAND_CHUNKS * CHUNK, 0), (0, 0), (0, 0))
    kp = jnp.pad(k, pad).reshape(b, nc + BAND_CHUNKS, CHUNK, h, dh)
    vp = jnp.pad(v, pad).reshape(b, nc + BAND_CHUNKS, CHUNK, h, dh)
    k_band = jnp.concatenate([kp[:, j:j + nc] for j in range(BAND_CHUNKS + 1)], axis=2)
    v_band = jnp.concatenate([vp[:, j:j + nc] for j in range(BAND_CHUNKS + 1)], axis=2)
    i = jnp.arange(CHUNK)
    j = jnp.arange(band)
    rel = BAND_CHUNKS * CHUNK + i[:, None] - j[None, :]
    bias = rel_bias[:, jnp.clip(rel, -REL_CLIP, REL_CLIP) + REL_CLIP].astype(jnp.float32)
    key_chunk = jnp.arange(nc)[:, None] - BAND_CHUNKS + (j // CHUNK)[None, :]
    valid = key_chunk >= 0
    logits = jnp.einsum('bnqhd,bnkhd->bnhqk', qc, k_band).astype(jnp.float32) * (dh ** -0.5)
    logits = logits + bias[None, None]
    logits = jnp.where(valid[None, :, None, None, :], logits, -jnp.inf)
    p = jax.nn.softmax(logits, axis=-1)
    o = jnp.einsum('bnhqk,bnkhd->bnqhd', p.astype(v.dtype), v_band)
    return o.reshape(b, s_len, h * dh)


def swiglu(h, w1, w3, w2):
    return (jax.nn.silu(h @ w1) * (h @ w3)) @ w2


def moe_swiglu(h, router, w1, w3, w2):
    b, s_len, d = h.shape
    ht = h.reshape(-1, d)
    logits = (ht @ router).astype(jnp.float32)
    top_val, top_idx = lax.top_k(logits, TOP_K)
    gates = jax.nn.softmax(top_val, axis=-1)
    combine = jnp.sum(jax.nn.one_hot(top_idx, N_EXPERTS, dtype=jnp.float32) * gates[..., None], axis=1)
    out = jnp.zeros_like(ht)
    for e in range(N_EXPERTS):
        out = out + combine[:, e:e + 1].astype(ht.dtype) * swiglu(ht, w1[e], w3[e], w2[e])
    return out.reshape(b, s_len, d)


def setup_inputs(seed: int = 0) -> dict:
    key = jax.random.key(seed)
    ks = jax.random.split(key, 24)
    h = N_HEADS_G

    def nrm(k, shape, fan_in, s=1.0):
        return jax.random.normal(k, shape, jnp.float32) * (s * fan_in ** -0.5)

    def gain(k, shape):
        return 1.0 + 0.1 * jax.random.normal(k, shape, jnp.float32)

    return {
        'x': jax.random.normal(ks[0], (BATCH, SEQ, D_MODEL), jnp.float32),
        'c': jax.random.normal(ks[1], (BATCH, D_MODEL), jnp.float32),
        'ada_w': nrm(ks[2], (DEPTH, D_MODEL, 6 * D_MODEL), D_MODEL, 0.5),
        'ada_b': 0.02 * jax.random.normal(ks[3], (DEPTH, 6 * D_MODEL), jnp.float32),
        'mix_norm_g': gain(ks[4], (DEPTH, D_MODEL)),
        'ffn_norm_g': gain(ks[5], (DEPTH, D_MODEL)),
        'w_in': nrm(ks[6], (DEPTH, D_MODEL, D_IN), D_MODEL),
        'w_out': nrm(ks[7], (DEPTH, D_MIX, D_MODEL), D_MIX),
        'diff_lambda': 0.1 * jax.random.normal(ks[8], (DEPTH, 4, DIFF_D), jnp.float32),
        'diff_norm_g': gain(ks[9], (DEPTH, 2 * DIFF_D)),
        'mla_q_norm_g': gain(ks[10], (DEPTH, Q_LORA)),
        'mla_kv_norm_g': gain(ks[11], (DEPTH, KV_LORA)),
        'mla_w_uq': nrm(ks[12], (DEPTH, Q_LORA, h * (QK_NOPE + QK_ROPE)), Q_LORA),
        'mla_w_ukv': nrm(ks[13], (DEPTH, KV_LORA, h * (QK_NOPE + V_HEAD)), KV_LORA),
        'band_rel_bias': 0.2 * jax.random.normal(ks[14], (DEPTH, h, 2 * REL_CLIP + 1), jnp.float32),
        'ffn_w1': nrm(ks[15], (N_DENSE, D_MODEL, D_FF), D_MODEL),
        'ffn_w3': nrm(ks[16], (N_DENSE, D_MODEL, D_FF), D_MODEL),
        'ffn_w2': nrm(ks[17], (N_DENSE, D_FF, D_MODEL), D_FF),
        'moe_router': nrm(ks[18], (N_MOE, D_MODEL, N_EXPERTS), D_MODEL),
        'moe_w1': nrm(ks[19], (N_MOE, N_EXPERTS, D_MODEL, D_FF_EXPERT), D_MODEL),
        'moe_w3': nrm(ks[20], (N_MOE, N_EXPERTS, D_MODEL, D_FF_EXPERT), D_MODEL),
        'moe_w2': nrm(ks[21], (N_MOE, N_EXPERTS, D_FF_EXPERT, D_MODEL), D_FF_EXPERT),
        'final_norm_g': gain(ks[22], (D_MODEL,)),
    }


def reference(x, c, ada_w, ada_b, mix_norm_g, ffn_norm_g, w_in, w_out, diff_lambda, diff_norm_g,
              mla_q_norm_g, mla_kv_norm_g, mla_w_uq, mla_w_ukv, band_rel_bias,
              ffn_w1, ffn_w3, ffn_w2, moe_router, moe_w1, moe_w3, moe_w2, final_norm_g):
    b, s_len, _ = x.shape
    h = N_HEADS_G
    cos, sin = rope_tables(s_len)
    for l in range(DEPTH):
        mod = jax.nn.silu(c) @ ada_w[l] + ada_b[l]
        sh_m, sc_m, g_m, sh_f, sc_f, g_f = jnp.split(mod, 6, axis=-1)
        hn = adaln(x, mix_norm_g[l], sh_m, sc_m)
        (a_q, a_k, a_v, b_qd, b_kvd, b_kr, c_q, c_k, c_v,
         c_qi, c_ki, c_wi, d_q, d_k, d_v) = jnp.split(hn @ w_in[l], IN_SPLITS, axis=-1)
        o_a = diff_attention(a_q.reshape(b, s_len, h, 2, DIFF_D), a_k.reshape(b, s_len, h, 2, DIFF_D),
                             a_v.reshape(b, s_len, h, 2 * DIFF_D), diff_lambda[l], diff_norm_g[l], l)
        o_b = latent_attention(b_qd, b_kvd, b_kr, mla_q_norm_g[l], mla_kv_norm_g[l],
                               mla_w_uq[l], mla_w_ukv[l], cos, sin)
        o_c = indexed_sparse_attention(c_q.reshape(b, s_len, h, HEAD_DIM), c_k.reshape(b, s_len, h, HEAD_DIM),
                                       c_v.reshape(b, s_len, h, HEAD_DIM),
                                       c_qi.reshape(b, s_len, IDX_HEADS, IDX_DIM), c_ki, c_wi)
        o_d = chunk_band_attention(d_q.reshape(b, s_len, h, HEAD_DIM), d_k.reshape(b, s_len, h, HEAD_DIM),
                                   d_v.reshape(b, s_len, h, HEAD_DIM), band_rel_bias[l])
        y = jnp.concatenate([o_a, o_b, o_c, o_d], axis=-1) @ w_out[l]
        x = x + g_m[:, None, :] * y
        hn = adaln(x, ffn_norm_g[l], sh_f, sc_f)
        if l % 2 == 0:
            f = swiglu(hn, ffn_w1[l // 2], ffn_w3[l // 2], ffn_w2[l // 2])
        else:
            f = moe_swiglu(hn, moe_router[l // 2], moe_w1[l // 2], moe_w3[l // 2], moe_w2[l // 2])
        x = x + g_f[:, None, :] * f
    return rmsnorm(x, final_norm_g)
```

```python
import math
import os
from contextlib import ExitStack
import numpy as np
import concourse.bass as bass
import concourse.mybir as mybir
from concourse.bass_utils import run_bass_kernel_spmd

F32 = mybir.dt.float32
BF16 = mybir.dt.bfloat16
AF = mybir.ActivationFunctionType
ALU = mybir.AluOpType
AX = mybir.AxisListType

D = 1024
T = 4096
DEPTH = 2
NT = T // 128
KT = D // 128
EPS = 1e-6
D_FF = 2816
D_FFE = 3584
NEXP = 8
NEG = -30000.0


class Prog:
    NSLOT = 12

    def __init__(self, nc, es):
        self.nc = nc
        self.ops = []
        self.engs = {"pe": nc.tensor, "act": nc.scalar, "dve": nc.vector,
                     "pool": nc.gpsimd, "sp": nc.sync}
        self.esem = {e: es.enter_context(nc.semaphore("sem_" + e)) for e in self.engs}
        self.dq = ("sp", "act", "pool")
        self.dsem = {q: [es.enter_context(nc.semaphore("dsem_%s%d" % (q, k))) for k in range(self.NSLOT)]
                     for q in self.dq}
        self.ecount = {e: 0 for e in self.engs}
        self.dcount = {q: 0 for q in self.dq}
        self.eclock = {e: {} for e in self.engs}
        self.sig = {}
        self.done_clock = {}
        self.gid = 0
        self.last_comp = {}
        self.dhist = {q: [] for q in self.dq}
        self.n_ops = 0
        self.n_waits = 0
        self.n_sig = 0

    def op(self, eng, fn, reads=(), writes=(), dma=False):
        self.ops.append((eng, fn, tuple(reads), tuple(writes), dma))

    def pe(self, fn, r=(), w=()): self.op("pe", fn, r, w)
    def act(self, fn, r=(), w=()): self.op("act", fn, r, w)
    def dve(self, fn, r=(), w=()): self.op("dve", fn, r, w)
    def pool(self, fn, r=(), w=()): self.op("pool", fn, r, w)
    def dma(self, q, fn, r=(), w=()): self.op(q, fn, r, w, True)

    def _sem(self, sk):
        return self.esem[sk[1]] if sk[0] == "e" else self.dsem[sk[1]][sk[2]]

    def _bar_deps(self):
        d = set(self.last_comp.values())
        for q in self.dq:
            d.update(self.dhist[q][-self.NSLOT:])
        return d

    def flush(self):
        ops = self.ops
        self.ops = []
        n = len(ops)
        base = self.gid
        self.gid += n
        bar = self._bar_deps()
        last_w = {}
        readers = {}
        deps = [None] * n
        seen = set()
        openg = {}
        for i, (eng, fn, rd, wr, is_dma) in enumerate(ops):
            g = base + i
            openg[g] = (eng, is_dma)
            d = set()
            if eng not in seen:
                seen.add(eng)
                d |= bar
            for r in rd:
                lw = last_w.get(r)
                if lw is not None:
                    d.add(lw)
            for r in wr:
                lw = last_w.get(r)
                if lw is not None:
                    d.add(lw)
                for x in readers.get(r, ()):
                    d.add(x)
            for r in rd:
                readers.setdefault(r, []).append(g)
            for r in wr:
                last_w[r] = g
                readers[r] = []
            if is_dma:
                h = self.dhist[eng]
                if len(h) >= self.NSLOT:
                    d.add(h[-self.NSLOT])
                h.append(g)
            d.discard(g)
            if eng == "pe" and not is_dma:
                d = {x for x in d if not (x >= base and openg[x] == ("pe", False))}
            deps[i] = d
            if not is_dma:
                self.last_comp[eng] = g
        signal = set(self.last_comp.values())
        for i in range(n):
            if ops[i][4]:
                signal.add(base + i)
            signal |= deps[i]
        for q in self.dq:
            self.dhist[q] = self.dhist[q][-self.NSLOT:]
        for i, (eng, fn, rd, wr, is_dma) in enumerate(ops):
            g = base + i
            if is_dma:
                k = self.dcount[eng]
                self.dcount[eng] += 1
                self.sig[g] = (("d", eng, k % self.NSLOT), 16 * (k // self.NSLOT + 1))
            elif g in signal:
                self.ecount[eng] += 1
                self.sig[g] = (("e", eng), self.ecount[eng])
        for i, (eng, fn, rd, wr, is_dma) in enumerate(ops):
            g = base + i
            clk = self.eclock[eng]
            e = self.engs[eng]
            need = {}
            for x in deps[i]:
                sk, sv = self.sig[x]
                if clk.get(sk, 0) < sv and need.get(sk, 0) < sv:
                    need[sk] = sv
            for x in deps[i]:
                for k2, v2 in self.done_clock[x].items():
                    if clk.get(k2, 0) < v2:
                        clk[k2] = v2
            for sk, sv in need.items():
                e.wait_ge(self._sem(sk), sv)
                self.n_waits += 1
                if clk.get(sk, 0) < sv:
                    clk[sk] = sv
            inst = fn()
            if g in self.sig:
                sk, sv = self.sig[g]
                inst.then_inc(self._sem(sk), 16 if is_dma else 1)
                dc = dict(clk)
                dc[sk] = sv
                self.done_clock[g] = dc
                self.n_sig += 1
        self.n_ops += n
        keep = self._bar_deps()
        self.sig = {g: v for g, v in self.sig.items() if g in keep}
        self.done_clock = {g: v for g, v in self.done_clock.items() if g in keep}

    def finish(self):
        self.flush()
        e = self.nc.sync
        clk = self.eclock["sp"]
        for x in self._bar_deps():
            sk, sv = self.sig[x]
            if clk.get(sk, 0) < sv:
                e.wait_ge(self._sem(sk), sv)
                clk[sk] = sv
        return dict(n_ops=self.n_ops, n_waits=self.n_waits, n_sig=self.n_sig, ecount=dict(self.ecount), dcount=dict(self.dcount))


class Builder:
    def __init__(self, debug=None, stop_after=None, mixers="abcd"):
        self.mixers = mixers
        import os
        self.dbg = os.environ.get("MK_DBG", "")
        self.dbg2s = set(os.environ.get("MK_DBG2", "").split(","))
        self.dbg2 = ""
        self.debug = debug or []
        self.stop_after = stop_after
        self.nc = bass.Bass("TRN2", target_bir_lowering=False)
        self.es = ExitStack()
        self.P = Prog(self.nc, self.es)
        self.uid = 0
        self.inputs = {}

    def din(self, name, shape, dt=F32):
        t = self.nc.dram_tensor(name, list(shape), dt, kind="ExternalInput").ap()
        self.inputs[name] = t
        return t

    def dscr(self, name, shape, dt):
        kind = "ExternalOutput"
        return self.nc.dram_tensor(name, list(shape), dt, kind=kind).ap()

    def sb(self, stack, name, shape, dt):
        self.uid += 1
        return stack.enter_context(self.nc.sbuf_tensor("%s_%d" % (name, self.uid), list(shape), dt))

    def ps(self, stack, name, shape, dt=F32):
        self.uid += 1
        return stack.enter_context(self.nc.psum_tensor("%s_%d" % (name, self.uid), list(shape), dt))

    def build(self):
        nc, P = self.nc, self.P
        with self.es as es:
            self.declare_io()
            self.consts(es)
            P.flush()
            self.phase_l0()
            for l in range(DEPTH):
                if self.layer(l) == "stop" or self.stop_after == ("layer", l):
                    break
            else:
                self.final_norm()
            st = P.finish()
        self.stats = st
        return nc

    def declare_io(self):
        nc = self.nc
        self.x_in = self.din("x", [T, D])
        self.c_in = self.din("c", [128, KT])
        self.ada_w = self.din("ada_w", [DEPTH, KT, 128, 6 * D])
        self.ada_b = self.din("ada_b", [DEPTH, 128, 48])
        self.mix_g = self.din("mix_g", [DEPTH, 128, KT])
        self.ffn_g = self.din("ffn_g", [DEPTH, 128, KT])
        self.fin_g = self.din("fin_g", [128, KT])
        self.ident_in = self.din("ident", [128, 128])
        self.w_d = self.din("w_d", [DEPTH, KT, 128, 768])
        self.w_out = self.din("w_out", [DEPTH, KT, 128, D])
        self.w_b = self.din("w_b", [DEPTH, KT, 128, 576])
        self.w_uq = self.din("w_uq", [DEPTH, 2, 128, 384])
        self.w_uqr = self.din("w_uqr", [DEPTH, 2, 128, 384])
        self.w_ukvk = self.din("w_ukvk", [DEPTH, 1, 128, 256])
        self.w_ukvv = self.din("w_ukvv", [DEPTH, 1, 128, 256])
        self.gq = self.din("gq", [DEPTH, 128, 2])
        self.gkv = self.din("gkv", [DEPTH, 128, 1])
        self.rope = self.din("rope", [128, 2, T])
        self.diagmask = self.din("diagmask", [128, 512])
        self.w_a = self.din("w_a", [DEPTH, KT, 128, 768])
        self.w_c = self.din("w_c", [DEPTH, KT, 128, 1160])
        self.corr_c = self.din("corr_c", [128, 512])
        self.ident4 = self.din("ident4", [128, 512])
        self.corr_a = self.din("corr_a", [2, 128, 512])
        self.augq = self.din("augq", [4, 3, T])
        self.augk = self.din("augk", [4, 3, T])
        self.dlam = self.din("dlam", [DEPTH, 128, 128])
        self.dng = self.din("dng", [DEPTH, 128, 64])
        self.ffn_w13 = self.din("ffn_w13", [1, 1, D_FF // 128, 2, 128, KT, 128])
        self.ffn_w2 = self.din("ffn_w2", [1, 1, D_FF // 128, 128, D])
        self.moe_w13 = self.din("moe_w13", [1, NEXP, D_FFE // 128, 2, 128, KT, 128])
        self.moe_w2 = self.din("moe_w2", [1, NEXP, D_FFE // 128, 128, D])
        self.router = self.din("router", [KT, 128, 8])
        self.bias_d = self.din("bias_d", [DEPTH, 5, 128, 512])
        self.out = nc.dram_tensor("out", [T, D], F32, kind="ExternalOutput").ap()
        self.xT = self.dscr("xT", [D, T], F32)
        self.hnT = self.dscr("hnT", [D, T], BF16)
        self.oT = self.dscr("oT", [D, T], BF16)

    def consts(self, es):
        nc, P = self.nc, self.P
        self.ident_f = self.sb(es, "identf", [128, 128], F32)
        self.ident_b = self.sb(es, "identb", [128, 128], BF16)
        self.ones_b = self.sb(es, "onesb", [128, 128], BF16)
        self.modv = self.sb(es, "modv", [128, 48], F32)
        self.gvec = self.sb(es, "gvec", [128, 4 * KT], F32)
        P.dma("sp", lambda: nc.sync.dma_start(out=self.ident_f[:], in_=self.ident_in), w=["identf"])
        P.dve(lambda: nc.vector.tensor_copy(self.ident_b[:], self.ident_f[:]), r=["identf"], w=["identb"])
        P.dve(lambda: nc.vector.memset(self.ones_b[:], 1.0), w=["onesb"])

    def phase_l0(self):
        nc, P = self.nc, self.P
        with ExitStack() as st:
            xin = [self.sb(st, "xin", [128, 4, D], F32) for _ in range(2)]
            xo = [self.sb(st, "xo", [128, KT, 512], F32) for _ in range(2)]
            pt = [self.ps(st, "pt", [128, 512]) for _ in range(2)]
            xv = self.x_in.rearrange("(tb j p) f -> tb p j f", j=4, p=128)
            xTv = self.xT.rearrange("(kt p) t -> p kt t", p=128)
            for tb in range(T // 512):
                b = tb % 2
                P.dma("sp", lambda tb=tb, b=b: nc.sync.dma_start(out=xin[b][:], in_=xv[tb]),
                      w=[("xin", b)])
                for kt in range(KT):
                    pb = kt % 2
                    for j in range(4):
                        P.pe(lambda b=b, kt=kt, j=j, pb=pb: nc.tensor.transpose(
                            pt[pb][:, j * 128:(j + 1) * 128], xin[b][:, j, kt * 128:(kt + 1) * 128],
                            self.ident_f[:]),
                            r=[("xin", b), "identf"], w=[("pt", pb)])
                    if kt % 2 == 0:
                        P.dve(lambda b=b, kt=kt, pb=pb: nc.vector.tensor_copy(xo[b][:, kt, :], pt[pb][:]),
                              r=[("pt", pb)], w=[("xo", b, kt)])
                    else:
                        P.act(lambda b=b, kt=kt, pb=pb: nc.scalar.copy(xo[b][:, kt, :], pt[pb][:]),
                              r=[("pt", pb)], w=[("xo", b, kt)])
                P.dma("sp", lambda tb=tb, b=b: nc.sync.dma_start(
                    out=xTv[:, :, tb * 512:(tb + 1) * 512], in_=xo[b][:]),
                    r=[("xo", b, kt) for kt in range(KT)], w=[("dram", "xT", tb)])
            P.flush()

    def phase_mod(self, l):
        nc, P = self.nc, self.P
        with ExitStack() as st:
            cs = self.sb(st, "cs", [128, KT], F32)
            sc = self.sb(st, "sc", [128, KT], BF16)
            wb = [self.sb(st, "adaw", [128, 6 * D], BF16) for _ in range(2)]
            ab = self.sb(st, "adab", [128, 48], F32)
            gg = self.sb(st, "gg", [128, 2 * KT], F32)
            pm = self.ps(st, "pm", [128, 512])
            P.dma("sp", lambda: nc.sync.dma_start(out=cs[:], in_=self.c_in), w=["cs"])
            P.dma("sp", lambda: nc.sync.dma_start(out=ab[:], in_=self.ada_b[l]), w=["adab"])
            P.dma("sp", lambda: nc.sync.dma_start(out=gg[:, 0:KT], in_=self.mix_g[l]), w=["gg0"])
            P.dma("sp", lambda: nc.sync.dma_start(out=gg[:, KT:2 * KT], in_=self.ffn_g[l]), w=["gg1"])
            P.act(lambda: nc.scalar.activation(out=sc[:], in_=cs[:], func=AF.Silu), r=["cs"], w=["sc"])
            P.dve(lambda: nc.vector.memset(pm[:], 0.0), w=["pm"])
            for kt in range(KT):
                b = kt % 2
                P.dma("pool", lambda kt=kt, b=b: nc.gpsimd.dma_start(out=wb[b][:], in_=self.ada_w[l, kt]),
                      w=[("adaw", b)])
                for ft in range(48):
                    P.pe(lambda kt=kt, b=b, ft=ft: nc.tensor.matmul(
                        pm[:, ft:ft + 1], wb[b][:, ft * 128:(ft + 1) * 128], sc[:, kt:kt + 1],
                        start=False, stop=(kt == KT - 1), skip_group_check=True),
                        r=[("adaw", b), "sc"], w=["pm"])
            mv = self.modv
            P.dve(lambda: nc.vector.tensor_tensor(mv[:], pm[:, 0:48], ab[:], ALU.add),
                  r=["pm", "adab"], w=["modv"])
            gv = self.gvec
            P.dve(lambda: nc.vector.scalar_tensor_tensor(
                gv[:, 0:KT], mv[:, 8:16], 1.0, gg[:, 0:KT], ALU.add, ALU.mult),
                r=["modv", "gg0"], w=["gvec0"])
            P.dve(lambda: nc.vector.scalar_tensor_tensor(
                gv[:, KT:2 * KT], mv[:, 32:40], 1.0, gg[:, KT:2 * KT], ALU.add, ALU.mult),
                r=["modv", "gg1"], w=["gvec1"])
            P.flush()

    def phase_norm(self, which):
        nc, P = self.nc, self.P
        goff = which * KT
        shoff = 0 if which == 0 else 24
        with ExitStack() as st:
            xb = [self.sb(st, "nx", [128, KT, 512], F32) for _ in range(2)]
            sq = [self.sb(st, "nsq", [128, KT, 512], BF16) for _ in range(2)]
            ms = [self.sb(st, "nms", [128, 512], F32) for _ in range(2)]
            tmp = [self.sb(st, "ntmp", [128, KT, 512], F32) for _ in range(2)]
            hb = [self.sb(st, "nhb", [128, KT, 512], BF16) for _ in range(2)]
            pss = [self.ps(st, "nps", [128, 512]) for _ in range(2)]
            xTv = self.xT.rearrange("(kt p) t -> p kt t", p=128)
            hTv = self.hnT.rearrange("(kt p) t -> p kt t", p=128)
            for tb in range(T // 512):
                b = tb % 2
                P.dma("sp", lambda tb=tb, b=b: nc.sync.dma_start(
                    out=xb[b][:], in_=xTv[:, :, tb * 512:(tb + 1) * 512]),
                    r=[("dram", "xT", tb)], w=[("nx", b)])
                P.act(lambda b=b: nc.scalar.activation(out=sq[b][:], in_=xb[b][:], func=AF.Square),
                      r=[("nx", b)], w=[("nsq", b)])
                for kt in range(KT):
                    P.pe(lambda b=b, kt=kt: nc.tensor.matmul(
                        pss[b][:], self.ones_b[:], sq[b][:, kt, :], start=(kt == 0), stop=(kt == KT - 1)),
                        r=[("nsq", b), "onesb"], w=[("nps", b)])
                P.dve(lambda b=b: nc.vector.tensor_scalar(
                    ms[b][:], pss[b][:], 1.0 / D, EPS, ALU.mult, ALU.add),
                    r=[("nps", b)], w=[("nms", b)])
                P.act(lambda b=b: nc.scalar.activation(out=ms[b][:], in_=ms[b][:], func=AF.Sqrt),
                      r=[("nms", b)], w=[("nms", b)])
                P.dve(lambda b=b: nc.vector.reciprocal(ms[b][:], ms[b][:]),
                      r=[("nms", b)], w=[("nms", b)])
                for kt in range(KT):
                    P.dve(lambda b=b, kt=kt: nc.vector.scalar_tensor_tensor(
                        tmp[b][:, kt, :], xb[b][:, kt, :], self.gvec[:, goff + kt:goff + kt + 1],
                        ms[b][:], ALU.mult, ALU.mult),
                        r=[("nx", b), ("nms", b), "gvec%d" % which], w=[("ntmp", b, kt)])
                    P.act(lambda b=b, kt=kt: nc.scalar.activation(
                        out=hb[b][:, kt, :], in_=tmp[b][:, kt, :], func=AF.Identity,
                        bias=self.modv[:, shoff + kt:shoff + kt + 1], scale=1.0),
                        r=[("ntmp", b, kt), "modv"], w=[("nhb", b, kt)])
                P.dma("sp", lambda tb=tb, b=b: nc.sync.dma_start(
                    out=hTv[:, :, tb * 512:(tb + 1) * 512], in_=hb[b][:]),
                    r=[("nhb", b, kt) for kt in range(KT)], w=[("dram", "hnT", tb)])
            P.flush()

    def load_w(self, st, name, dram_ap, ncols, nk=KT):
        nc, P = self.nc, self.P
        w = self.sb(st, name, [128, nk, ncols], BF16)
        for k in range(nk):
            P.dma("pool", lambda k=k: nc.gpsimd.dma_start(out=w[:, k, :], in_=dram_ap[k]), w=[(name, k)])
        return w

    def hn_blocks(self, st):
        nc, P = self.nc, self.P
        hb = [self.sb(st, "hb", [128, KT, 512], BF16) for _ in range(2)]
        hTv = self.hnT.rearrange("(kt p) t -> p kt t", p=128)

        def load(tb):
            b = tb % 2
            P.dma("sp", lambda: nc.sync.dma_start(out=hb[b][:], in_=hTv[:, :, tb * 512:(tb + 1) * 512]),
                  w=[("hb", b)])
            return hb[b], ("hb", b)
        return load

    def proj_fm(self, pp, ppr, w, wres, c0, M, hb, hres, nk=KT):
        nc, P = self.nc, self.P
        for k in range(nk):
            P.pe(lambda k=k: nc.tensor.matmul(pp[0:M, :], w[:, k, c0:c0 + M], hb[:, k, :],
                                              start=(k == 0), stop=(k == nk - 1)),
                 r=[hres] + wres, w=[ppr])

    def proj_tm(self, pp, ppr, w, wres, c0, n, hb, hres, j, nk=KT):
        nc, P = self.nc, self.P
        for k in range(nk):
            P.pe(lambda k=k: nc.tensor.matmul(pp[:, 0:n], hb[:, k, j * 128:(j + 1) * 128], w[:, k, c0:c0 + n],
                                              start=(k == 0), stop=(k == nk - 1)),
                 r=[hres] + wres, w=[ppr])

    def attn_core(self, st, name, groups, kts_fn, pre_fn, qk_fn, v_fn, out_fn, before_qt=None, ns=3):
        nc, P = self.nc, self.P
        S = [self.ps(st, "S", [128, 512]) for _ in range(ns)]
        O = [self.ps(st, "O", [128, 512]) for _ in range(2)]
        Pt = [self.sb(st, "Pt", [128, 512], BF16) for _ in range(ns + 1)]
        rl = [self.sb(st, "rl", [128, 4], F32) for _ in range(2)]
        if os.environ.get("MK_NOATTN"):
            return
        tasks = []
        oi = 0
        for qt in range(NT):
            for g in range(groups):
                kts = kts_fn(qt)
                for ki, kt in enumerate(kts):
                    tasks.append((qt, g, ki, kt, len(kts), oi % 2, len(tasks) % ns, len(tasks) % (ns + 1)))
                oi += 1

        def emit_qk(t):
            qt, g, ki, kt, nk, ob, sbk, pb = t
            if ki == 0 and g == 0 and before_qt is not None:
                before_qt(qt)
            pre = pre_fn(g, qt, kt)
            first = True
            for (lh, rh, rr) in pre:
                P.pe(lambda lh=lh, rh=rh, first=first: nc.tensor.matmul(
                    S[sbk][:], lh, rh, start=first, stop=False, skip_group_check=True),
                    r=rr, w=[("S", sbk)])
                first = False
            for j in range(4):
                lh, rh = qk_fn(g, j, qt, kt)
                P.pe(lambda lh=lh, rh=rh, first=first, j=j: nc.tensor.matmul(
                    S[sbk][:, j * 128:(j + 1) * 128], lh, rh, start=first, stop=(j == 3),
                    skip_group_check=True), w=[("S", sbk)])
                first = False

        def emit_exp(t):
            qt, g, ki, kt, nk, ob, sbk, pb = t
            P.act(lambda: nc.scalar.activation(out=Pt[pb][:], in_=S[sbk][:], func=AF.Exp),
                  r=[("S", sbk)], w=[("Pt", pb)])

        def emit_pv(t):
            qt, g, ki, kt, nk, ob, sbk, pb = t
            for j in range(4):
                va = v_fn(g, j, kt)
                P.pe(lambda va=va, j=j: nc.tensor.matmul(
                    O[ob][:, j * 66:j * 66 + 65], Pt[pb][:, j * 128:(j + 1) * 128], va,
                    start=(ki == 0 and j == 0), stop=(ki == nk - 1), skip_group_check=True),
                    r=[("Pt", pb)], w=[("O", ob)])
            if ki == nk - 1:
                P.dve(lambda: nc.vector.reciprocal(
                    rl[ob][:, :], O[ob][:, 0:264].rearrange("p (j c) -> p j c", c=66)[:, :, 64]),
                    r=[("O", ob)], w=[("rl", ob)])
                out_fn(g, qt, O[ob], ("O", ob), rl[ob], ("rl", ob))

        la = ns - 1
        for i in range(min(la, len(tasks))):
            emit_qk(tasks[i])
        for i, t in enumerate(tasks):
            emit_exp(t)
            if i + la < len(tasks):
                emit_qk(tasks[i + la])
            emit_pv(t)

    def std_out(self, st, name, moff, ntp=2):
        nc, P = self.nc, self.P
        on = [self.sb(st, "on", [128, 256], BF16) for _ in range(2)]
        tp = [self.ps(st, "tp", [128, 2, 128], BF16) for _ in range(ntp)]
        stage = [self.sb(st, "ostg", [128, 2, 512], BF16) for _ in range(2)]
        oTv = self.oT[moff:moff + 256, :].rearrange("(i p) t -> p i t", p=128)

        def out_fn(g, qt, O, Or, rl, rlr):
            nb = qt % 2
            sbi = (qt // 4) % 2
            q4 = qt % 4
            tpb = qt % ntp
            for j in range(4):
                if True:
                    P.dve(lambda j=j: nc.vector.tensor_scalar(
                        on[nb][:, j * 64:(j + 1) * 64], O[:, j * 66:j * 66 + 64], rl[:, j:j + 1], None, ALU.mult),
                        r=[Or, rlr], w=[("on", nb, j)])
                else:
                    P.act(lambda j=j: nc.scalar.activation(
                        out=on[nb][:, j * 64:(j + 1) * 64], in_=O[:, j * 66:j * 66 + 64], func=AF.Copy,
                        scale=rl[:, j:j + 1]),
                        r=[Or, rlr], w=[("on", nb, j)])
            if "notp" in self.dbg2s:
                return
            for i in range(2):
                P.pe(lambda i=i: nc.tensor.transpose(tp[tpb][:, i, :], on[nb][:, i * 128:(i + 1) * 128],
                                                     self.ident_b[:]),
                     r=[("on", nb, 2 * i), ("on", nb, 2 * i + 1)], w=[("tp", tpb)])
            if "nostg" in self.dbg2s:
                return
            P.dve(lambda: nc.vector.tensor_copy(stage[sbi][:, 0, q4 * 128:(q4 + 1) * 128], tp[tpb][:, 0, :]),
                  r=[("tp", tpb)], w=[("ostg", sbi, 0)])
            P.dve(lambda: nc.vector.tensor_copy(stage[sbi][:, 1, q4 * 128:(q4 + 1) * 128], tp[tpb][:, 1, :]),
                  r=[("tp", tpb)], w=[("ostg", sbi, 1)])
            if q4 == 3 and "nodma" not in self.dbg2s:
                tb = qt // 4
                P.dma("sp", lambda: nc.sync.dma_start(out=oTv[:, :, tb * 512:(tb + 1) * 512], in_=stage[sbi][:]),
                      r=[("ostg", sbi, 0), ("ostg", sbi, 1)], w=[("dram", "oT", name, tb)])
        return out_fn

    def v_evac(self, V, pp, ppr, tt, eng_i):
        nc, P = self.nc, self.P
        for h in range(4):
            if (eng_i + h) % 2 == 0:
                P.dve(lambda h=h: nc.vector.tensor_copy(V[:, tt, h, 0:64], pp[:, h * 64:(h + 1) * 64]),
                      r=[ppr], w=[("V", tt)])
            else:
                P.act(lambda h=h: nc.scalar.copy(V[:, tt, h, 0:64], pp[:, h * 64:(h + 1) * 64]),
                      r=[ppr], w=[("V", tt)])

    def mixer_d(self, l):
        nc, P = self.nc, self.P
        with ExitStack() as st:
            QT = [self.sb(st, "dQT", [128, T], BF16) for _ in range(2)]
            KTt = [self.sb(st, "dKT", [128, T], BF16) for _ in range(4)]
            for h in range(4):
                P.dve(lambda h=h: nc.vector.memset(KTt[h][:], 0.0), w=[("KT", h)])
            V = self.sb(st, "dV", [128, NT, 4, 66], BF16)
            bias = self.sb(st, "dbias", [128, 5, 512], BF16)
            P.dve(lambda: nc.vector.memset(V[:], 1.0), w=[("V", tt) for tt in range(NT)])
            for dd in range(5):
                P.dma("pool", lambda dd=dd: nc.gpsimd.dma_start(out=bias[:, dd, :], in_=self.bias_d[l, dd]),
                      w=[("dbias", dd)])
            with ExitStack() as s2:
                w = self.load_w(s2, "wd", self.w_d[l], 768)
                wres = [("wd", k) for k in range(KT)]
                load = self.hn_blocks(s2)
                pp = [self.ps(s2, "pp", [128, 512]) for _ in range(4)]
                pi = 0
                for tb in range(T // 512):
                    hb, hres = load(tb)
                    sl = slice(tb * 512, (tb + 1) * 512)
                    for i in range(2):
                        p = pi % 4; pi += 1
                        self.proj_fm(pp[p], ("pp", p), w, wres, i * 128, 128, hb, hres)
                        P.act(lambda p=p, i=i, sl=sl: nc.scalar.mul(QT[i][:, sl], pp[p][:], 0.125),
                              r=[("pp", p)], w=[("QT", i, tb)])
                        p = pi % 4; pi += 1
                        self.proj_fm(pp[p], ("pp", p), w, wres, 256 + i * 128, 128, hb, hres)
                        P.dve(lambda p=p, i=i, sl=sl: nc.vector.tensor_copy(KTt[2 * i][0:64, sl], pp[p][0:64, :]),
                              r=[("pp", p)], w=[("KT", 2 * i)])
                        P.dve(lambda p=p, i=i, sl=sl: nc.vector.tensor_copy(KTt[2 * i + 1][64:128, sl], pp[p][64:128, :]),
                              r=[("pp", p)], w=[("KT", 2 * i + 1)])
                    for j in range(4):
                        if self.dbg2 == "noV":
                            continue
                        p = pi % 4; pi += 1
                        self.proj_tm(pp[p], ("pp", p), w, wres, 512, 256, hb, hres, j)
                        if self.dbg2 == "noVevac":
                            continue
                        self.v_evac(V, pp[p], ("pp", p), tb * 4 + j, j)
                P.flush()
            if self.dbg == "proj":
                return
            with ExitStack() as s3:
                out_fn = self.std_out(s3, "d", 768)

                def kts_fn(qt):
                    return list(range(max(0, qt - 4), qt + 1))

                def pre_fn(g, qt, kt):
                    if self.dbg2 == "nopre":
                        return []
                    return [(self.ident_b[:], bias[:, qt - kt, :], [])]

                def qk_fn(g, j, qt, kt):
                    return (KTt[j][:, kt * 128:(kt + 1) * 128],
                            QT[j // 2][:, qt * 128:(qt + 1) * 128])

                def v_fn(g, j, kt):
                    return V[:, kt, j, 0:65]
                self.attn_core(s3, "d", 1, kts_fn, pre_fn, qk_fn, v_fn, out_fn)
                P.flush()

    def rsqrt_pool(self, out_ap, in_ap, nh_ap, r, w):
        nc, P = self.nc, self.P
        P.pool(lambda: nc.gpsimd.tensor_tensor(out_ap, in_ap, nh_ap, ALU.pow), r=r, w=w)

    def mixer_b(self, l):
        nc, P = self.nc, self.P
        scale = 96.0 ** -0.5
        with ExitStack() as st:
            QTh = [self.sb(st, "bQT", [128, T], BF16) for _ in range(4)]
            KTh = [self.sb(st, "bKT", [128, T], BF16) for _ in range(4)]
            V = self.sb(st, "bV", [128, NT, 4, 66], BF16)
            dmask = self.sb(st, "bdm", [128, 512], BF16)
            P.dve(lambda: nc.vector.memset(V[:], 1.0), w=[("V", tt) for tt in range(NT)])
            for h in range(4):
                P.dve(lambda h=h: nc.vector.memset(QTh[h][:], 0.0), w=[("QT", h), ("QTr", h)])
                P.dve(lambda h=h: nc.vector.memset(KTh[h][:], 0.0), w=[("KT", h), ("KTr", h)])
            P.dma("pool", lambda: nc.gpsimd.dma_start(out=dmask[:], in_=self.diagmask), w=["bdm"])
            with ExitStack() as s2:
                w = self.load_w(s2, "wb", self.w_b[l], 576)
                wres = [("wb", k) for k in range(KT)]
                wuq = self.load_w(s2, "wuq", self.w_uq[l], 384, nk=2)
                wuqr = self.load_w(s2, "wuqr", self.w_uqr[l], 384, nk=2)
                wkk = self.load_w(s2, "wkk", self.w_ukvk[l], 256, nk=1)
                wkv = self.load_w(s2, "wkv", self.w_ukvv[l], 256, nk=1)
                wu_res = [("wuq", 0), ("wuq", 1), ("wuqr", 0), ("wuqr", 1), ("wkk", 0), ("wkv", 0)]
                gq = self.sb(s2, "bgq", [128, 3], F32)
                nh = self.sb(s2, "bnh", [128, 512], F32)
                P.dma("sp", lambda: nc.sync.dma_start(out=gq[:, 0:2], in_=self.gq[l]), w=["bgq"])
                P.dma("sp", lambda: nc.sync.dma_start(out=gq[:, 2:3], in_=self.gkv[l]), w=["bgq"])
                P.dve(lambda: nc.vector.memset(nh[:], -0.5), w=["bnh"])
                load = self.hn_blocks(s2)
                rope = [self.sb(s2, "brope", [128, 2, 512], F32) for _ in range(2)]
                qd = [self.sb(s2, "bqd", [128, 3, 512], F32) for _ in range(2)]
                sq = [self.sb(s2, "bsq", [128, 3, 512], BF16) for _ in range(2)]
                ms = [self.sb(s2, "bms", [128, 2, 512], F32) for _ in range(2)]
                qn = [self.sb(s2, "bqn", [128, 3, 512], BF16) for _ in range(2)]
                t1 = [self.sb(s2, "bt1", [128, 512], F32) for _ in range(2)]
                t2 = [self.sb(s2, "bt2", [128, 512], F32) for _ in range(2)]
                hbs = {}
                pp = [self.ps(s2, "pp", [128, 512]) for _ in range(6)]
                pi = 0
                ri = 0
                pc = [0]
                rc = [0]
                def stage1(tb):
                    b = tb % 2
                    hb, hres = load(tb)
                    sl = slice(tb * 512, (tb + 1) * 512)
                    P.dma("sp", lambda b=b, sl=sl: nc.sync.dma_start(out=rope[b][64:96, :, :], in_=self.rope[64:96, :, sl]),
                          w=[("brope", b)])
                    for i in range(3):
                        p = pc[0] % 6; pc[0] += 1
                        self.proj_fm(pp[p], ("pp", p), w, wres, i * 128, 128, hb, hres)
                        P.act(lambda p=p, i=i, b=b: nc.scalar.copy(qd[b][:, i, :], pp[p][:]),
                              r=[("pp", p)], w=[("bqd", b, i)])
                    P.act(lambda b=b: nc.scalar.activation(out=sq[b][:], in_=qd[b][:], func=AF.Square),
                          r=[("bqd", b, i) for i in range(3)], w=[("bsq", b)])
                    p = pc[0] % 6; pc[0] += 1
                    for i in range(2):
                        P.pe(lambda p=p, i=i, b=b: nc.tensor.matmul(pp[p][:], self.ones_b[:], sq[b][:, i, :],
                                                                   start=(i == 0), stop=(i == 1)),
                             r=[("bsq", b), "onesb"], w=[("pp", p)])
                    P.dve(lambda p=p, b=b: nc.vector.tensor_scalar(ms[b][:, 0, :], pp[p][:], 1.0 / 256, EPS, ALU.mult, ALU.add),
                          r=[("pp", p)], w=[("bms", b, 0)])
                    p = pc[0] % 6; pc[0] += 1
                    P.pe(lambda p=p, b=b: nc.tensor.matmul(pp[p][:], self.ones_b[:], sq[b][:, 2, :], start=True, stop=True),
                         r=[("bsq", b), "onesb"], w=[("pp", p)])
                    P.dve(lambda p=p, b=b: nc.vector.tensor_scalar(ms[b][:, 1, :], pp[p][:], 1.0 / 128, EPS, ALU.mult, ALU.add),
                          r=[("pp", p)], w=[("bms", b, 1)])
                    for i in range(2):
                        P.act(lambda b=b, i=i: nc.scalar.activation(out=ms[b][:, i, :], in_=ms[b][:, i, :], func=AF.Sqrt),
                              r=[("bms", b, i)], w=[("bms", b, i)])
                        P.dve(lambda b=b, i=i: nc.vector.reciprocal(ms[b][:, i, :], ms[b][:, i, :]),
                              r=[("bms", b, i)], w=[("bms", b, i)])
                    for i in range(3):
                        P.dve(lambda b=b, i=i: nc.vector.scalar_tensor_tensor(
                            qn[b][:, i, :], qd[b][:, i, :], gq[:, i:i + 1], ms[b][:, 0 if i < 2 else 1, :],
                            ALU.mult, ALU.mult),
                            r=[("bqd", b, i), ("bms", b, 0 if i < 2 else 1), "bgq"], w=[("bqn", b, i)])
                    p1 = pc[0] % 6; pc[0] += 1
                    self.proj_fm(pp[p1], ("pp", p1), w, wres, 384, 96, hb, hres)
                    p2 = pc[0] % 6; pc[0] += 1
                    self.proj_fm(pp[p2], ("pp", p2), w, wres, 480, 96, hb, hres)
                    r = rc[0] % 2; rc[0] += 1
                    P.dve(lambda p1=p1, b=b, r=r: nc.vector.tensor_tensor(
                        t1[r][64:96, :], pp[p1][64:96, :], rope[b][64:96, 0, :], ALU.mult),
                        r=[("pp", p1), ("brope", b)], w=[("bt1", r)])
                    P.dve(lambda p2=p2, b=b, r=r: nc.vector.tensor_tensor(
                        t2[r][64:96, :], pp[p2][64:96, :], rope[b][64:96, 1, :], ALU.mult),
                        r=[("pp", p2), ("brope", b)], w=[("bt2", r)])
                    for h in range(4):
                        P.dve(lambda h=h, r=r, sl=sl: nc.vector.tensor_tensor(
                            KTh[h][64:96, sl], t1[r][64:96, :], t2[r][64:96, :], ALU.add),
                            r=[("bt1", r), ("bt2", r)], w=[("KTr", h)])

                def stage2(tb):
                    b = tb % 2
                    sl = slice(tb * 512, (tb + 1) * 512)
                    for h in range(4):
                        p1 = pc[0] % 6; pc[0] += 1
                        for i in range(2):
                            P.pe(lambda p1=p1, i=i, h=h, b=b: nc.tensor.matmul(
                                pp[p1][0:96, :], wuq[:, i, h * 96:(h + 1) * 96], qn[b][:, i, :],
                                start=(i == 0), stop=(i == 1)),
                                r=[("bqn", b, i)] + wu_res, w=[("pp", p1)])
                        p2 = pc[0] % 6; pc[0] += 1
                        for i in range(2):
                            P.pe(lambda p2=p2, i=i, h=h, b=b: nc.tensor.matmul(
                                pp[p2][0:96, :], wuqr[:, i, h * 96:(h + 1) * 96], qn[b][:, i, :],
                                start=(i == 0), stop=(i == 1)),
                                r=[("bqn", b, i)] + wu_res, w=[("pp", p2)])
                        P.act(lambda p1=p1, h=h, sl=sl: nc.scalar.mul(QTh[h][0:64, sl], pp[p1][0:64, :], scale),
                              r=[("pp", p1)], w=[("QT", h)])
                        r = rc[0] % 2; rc[0] += 1
                        P.dve(lambda p1=p1, b=b, r=r: nc.vector.scalar_tensor_tensor(
                            t1[r][64:96, :], pp[p1][64:96, :], scale, rope[b][64:96, 0, :], ALU.mult, ALU.mult),
                            r=[("pp", p1), ("brope", b)], w=[("bt1", r)])
                        P.dve(lambda p2=p2, b=b, r=r: nc.vector.scalar_tensor_tensor(
                            t2[r][64:96, :], pp[p2][64:96, :], scale, rope[b][64:96, 1, :], ALU.mult, ALU.mult),
                            r=[("pp", p2), ("brope", b)], w=[("bt2", r)])
                        P.dve(lambda h=h, r=r, sl=sl: nc.vector.tensor_tensor(
                            QTh[h][64:96, sl], t1[r][64:96, :], t2[r][64:96, :], ALU.add),
                            r=[("bt1", r), ("bt2", r)], w=[("QTr", h)])
                        p3 = pc[0] % 6; pc[0] += 1
                        P.pe(lambda p3=p3, h=h, b=b: nc.tensor.matmul(
                            pp[p3][0:64, :], wkk[:, 0, h * 64:(h + 1) * 64], qn[b][:, 2, :], start=True, stop=True),
                            r=[("bqn", b, 2)] + wu_res, w=[("pp", p3)])
                        P.dve(lambda p3=p3, h=h, sl=sl: nc.vector.tensor_copy(KTh[h][0:64, sl], pp[p3][0:64, :]),
                              r=[("pp", p3)], w=[("KT", h)])
                    for j in range(4):
                        p = pc[0] % 6; pc[0] += 1
                        P.pe(lambda p=p, j=j, b=b: nc.tensor.matmul(
                            pp[p][:, 0:256], qn[b][:, 2, j * 128:(j + 1) * 128], wkv[:, 0, :], start=True, stop=True),
                            r=[("bqn", b, 2)] + wu_res, w=[("pp", p)])
                        self.v_evac(V, pp[p], ("pp", p), tb * 4 + j, j)

                stage1(0)
                for tb in range(T // 512):
                    if tb + 1 < T // 512:
                        stage1(tb + 1)
                    stage2(tb)
                P.flush()
            with ExitStack() as s3:
                out_fn = self.std_out(s3, "b", 256)

                def kts_fn(qt):
                    return list(range(qt + 1))

                def pre_fn(g, qt, kt):
                    if kt == qt:
                        return [(self.ident_b[:], dmask[:], [])]
                    return []

                def qk_fn(g, j, qt, kt):
                    return (KTh[j][:, kt * 128:(kt + 1) * 128], QTh[j][:, qt * 128:(qt + 1) * 128])

                def v_fn(g, j, kt):
                    return V[:, kt, j, 0:65]
                self.attn_core(s3, "b", 1, kts_fn, pre_fn, qk_fn, v_fn, out_fn)
                P.flush()

    def mixer_a(self, l):
        nc, P = self.nc, self.P
        scale = 32.0 ** -0.5
        lam_init = 0.8 - 0.6 * math.exp(-0.3 * l)
        with ExitStack() as st:
            QTa = [self.sb(st, "aQT", [128, T], BF16) for _ in range(4)]
            KTa = [[self.sb(st, "aKT", [128, T], BF16) for _ in range(2)] for _ in range(4)]
            V = self.sb(st, "aV", [128, NT, 4, 66], BF16)
            corr = self.sb(st, "acorr", [128, 2, 512], BF16)
            neglam = self.sb(st, "aneglam", [128, 1], F32)
            gAb = self.sb(st, "agAb", [128, 64], F32)
            nh = self.sb(st, "anh", [128, 2], F32)
            P.dve(lambda: nc.vector.memset(V[:], 1.0), w=[("V", tt) for tt in range(NT)])
            P.dve(lambda: nc.vector.memset(nh[:], -0.5), w=["anh"])
            for h in range(4):
                P.dve(lambda h=h: nc.vector.memset(QTa[h][:], 0.0), w=[("QT", h)])
                for c in range(2):
                    P.dve(lambda h=h, c=c: nc.vector.memset(KTa[h][c][:], 0.0), w=[("KT", h, c)])
            for g in range(2):
                P.dma("pool", lambda g=g: nc.gpsimd.dma_start(out=corr[:, g, :], in_=self.corr_a[g]), w=[("acorr", g)])
            for h in range(4):
                P.dma("pool", lambda h=h: nc.gpsimd.dma_start(out=QTa[h][64:67, :], in_=self.augq[h]),
                      r=[("QT", h)], w=[("QTaug", h)])
                for c in range(2):
                    P.dma("pool", lambda h=h, c=c: nc.gpsimd.dma_start(out=KTa[h][c][64:67, :], in_=self.augk[h]),
                          r=[("KT", h, c)], w=[("KTaug", h, c)])
            with ExitStack() as s2:
                lv = self.sb(s2, "alv", [128, 128], F32)
                pr = self.sb(s2, "apr", [128, 64], F32)
                s12 = self.sb(s2, "as12", [128, 2], F32)
                P.dma("sp", lambda: nc.sync.dma_start(out=lv[:], in_=self.dlam[l]), w=["alv"])
                P.dma("sp", lambda: nc.sync.dma_start(out=gAb[:], in_=self.dng[l]), w=["agAb"])
                P.dve(lambda: nc.vector.tensor_tensor(pr[:, 0:32], lv[:, 0:32], lv[:, 32:64], ALU.mult), r=["alv"], w=["apr0"])
                P.dve(lambda: nc.vector.tensor_tensor(pr[:, 32:64], lv[:, 64:96], lv[:, 96:128], ALU.mult), r=["alv"], w=["apr1"])
                P.dve(lambda: nc.vector.reduce_sum(out=s12[:], in_=pr[:, :].rearrange("p (a b) -> p a b", a=2), axis=AX.X),
                      r=["apr0", "apr1"], w=["as12"])
                P.act(lambda: nc.scalar.activation(out=s12[:], in_=s12[:], func=AF.Exp), r=["as12"], w=["as12"])
                P.dve(lambda: nc.vector.tensor_tensor(neglam[:], s12[:, 1:2], s12[:, 0:1], ALU.subtract), r=["as12"], w=["aneglam"])
                P.dve(lambda: nc.vector.tensor_scalar(neglam[:], neglam[:], -lam_init, None, ALU.add), r=["aneglam"], w=["aneglam"])
                P.dve(lambda: nc.vector.tensor_scalar(gAb[:], gAb[:], 1.0 - lam_init, None, ALU.mult), r=["agAb"], w=["agAb"])
                w = self.load_w(s2, "wa", self.w_a[l], 768)
                wres = [("wa", k) for k in range(KT)]
                load = self.hn_blocks(s2)
                pp = [self.ps(s2, "pp", [128, 512]) for _ in range(4)]
                pi = 0
                for tb in range(T // 512):
                    hb, hres = load(tb)
                    sl = slice(tb * 512, (tb + 1) * 512)
                    for h in range(4):
                        p = pi % 4; pi += 1
                        self.proj_fm(pp[p], ("pp", p), w, wres, h * 64, 64, hb, hres)
                        P.act(lambda p=p, h=h, sl=sl: nc.scalar.mul(QTa[h][0:64, sl], pp[p][0:64, :], scale),
                              r=[("pp", p)], w=[("QT", h)])
                        p = pi % 4; pi += 1
                        self.proj_fm(pp[p], ("pp", p), w, wres, 256 + h * 64, 64, hb, hres)
                        P.dve(lambda p=p, h=h, sl=sl: nc.vector.tensor_copy(KTa[h][0][0:32, sl], pp[p][0:32, :]),
                              r=[("pp", p)], w=[("KT", h, 0)])
                        P.dve(lambda p=p, h=h, sl=sl: nc.vector.tensor_copy(KTa[h][1][32:64, sl], pp[p][32:64, :]),
                              r=[("pp", p)], w=[("KT", h, 1)])
                    for j in range(4):
                        p = pi % 4; pi += 1
                        self.proj_tm(pp[p], ("pp", p), w, wres, 512, 256, hb, hres, j)
                        self.v_evac(V, pp[p], ("pp", p), tb * 4 + j, j)
                P.flush()
            with ExitStack() as s3:
                onA = [self.sb(s3, "aon", [128, 4, 64], F32) for _ in range(2)]
                cmb = [self.sb(s3, "acmb", [128, 2, 64], F32) for _ in range(2)]
                sqt = [self.sb(s3, "asq", [128, 2, 64], F32) for _ in range(2)]
                ss = [self.sb(s3, "ass", [128, 2], F32) for _ in range(2)]
                onb = [self.sb(s3, "aonb", [128, 128], BF16) for _ in range(2)]
                tp = [self.ps(s3, "atp", [128, 128], BF16) for _ in range(2)]
                stage = [self.sb(s3, "astg", [128, 2, 512], BF16) for _ in range(2)]
                oTv = self.oT[0:256, :].rearrange("(i p) t -> p i t", p=128)
                cnt = [0]

                def out_fn(g, qt, O, Or, rl, rlr):
                    nb = cnt[0] % 2
                    cnt[0] += 1
                    sbi = (qt // 4) % 2
                    q4 = qt % 4
                    for j in range(4):
                        P.dve(lambda j=j: nc.vector.tensor_scalar(
                            onA[nb][:, j, :], O[:, j * 66:j * 66 + 64], rl[:, j:j + 1], None, ALU.mult),
                            r=[Or, rlr], w=[("aon", nb, j)])
                    for hh in range(2):
                        P.dve(lambda hh=hh: nc.vector.scalar_tensor_tensor(
                            cmb[nb][:, hh, :], onA[nb][:, 2 * hh + 1, :], neglam[:, 0:1], onA[nb][:, 2 * hh, :],
                            ALU.mult, ALU.add),
                            r=[("aon", nb, 2 * hh), ("aon", nb, 2 * hh + 1)], w=[("acmb", nb, hh)])
                    P.dve(lambda: nc.vector.tensor_tensor(sqt[nb][:], cmb[nb][:], cmb[nb][:], ALU.mult),
                          r=[("acmb", nb, 0), ("acmb", nb, 1)], w=[("asq", nb)])
                    P.dve(lambda: nc.vector.reduce_sum(out=ss[nb][:], in_=sqt[nb][:], axis=AX.X),
                          r=[("asq", nb)], w=[("ass", nb)])
                    P.dve(lambda: nc.vector.tensor_scalar(ss[nb][:], ss[nb][:], 1.0 / 64, EPS, ALU.mult, ALU.add),
                          r=[("ass", nb)], w=[("ass", nb)])
                    self.rsqrt_pool(ss[nb][:], ss[nb][:], nh[:], [("ass", nb)], [("ass", nb)])
                    for hh in range(2):
                        P.dve(lambda hh=hh: nc.vector.scalar_tensor_tensor(
                            onb[nb][:, hh * 64:(hh + 1) * 64], cmb[nb][:, hh, :], ss[nb][:, hh:hh + 1], gAb[:],
                            ALU.mult, ALU.mult),
                            r=[("acmb", nb, hh), ("ass", nb)], w=[("aonb", nb, hh)])
                    P.pe(lambda: nc.tensor.transpose(tp[nb][:], onb[nb][:], self.ident_b[:]),
                         r=[("aonb", nb, 0), ("aonb", nb, 1)], w=[("atp", nb)])
                    P.dve(lambda: nc.vector.tensor_copy(stage[sbi][:, g, q4 * 128:(q4 + 1) * 128], tp[nb][:]),
                          r=[("atp", nb)], w=[("astg", sbi, g)])
                    if q4 == 3 and g == 1:
                        tb = qt // 4
                        P.dma("sp", lambda: nc.sync.dma_start(out=oTv[:, :, tb * 512:(tb + 1) * 512], in_=stage[sbi][:]),
                              r=[("astg", sbi, 0), ("astg", sbi, 1)], w=[("dram", "oT", "a", tb)])

                def kts_fn(qt):
                    return list(range(qt + 1))

                def pre_fn(g, qt, kt):
                    if kt == qt:
                        return [(self.ident_b[:], corr[:, g, :], [])]
                    return []

                def qk_fn(g, j, qt, kt):
                    h = 2 * g + j // 2
                    return (KTa[h][j % 2][:, kt * 128:(kt + 1) * 128], QTa[h][:, qt * 128:(qt + 1) * 128])

                def v_fn(g, j, kt):
                    return V[:, kt, 2 * g + j // 2, 0:65]
                self.attn_core(s3, "a", 2, kts_fn, pre_fn, qk_fn, v_fn, out_fn)
                P.flush()

    def mixer_c(self, l):
        nc, P = self.nc, self.P
        NR = 16
        with ExitStack() as st:
            QTc = [self.sb(st, "cQT", [128, T], BF16) for _ in range(4)]
            KTc = [self.sb(st, "cKT", [128, T], BF16) for _ in range(4)]
            V = self.sb(st, "cV", [128, NT, 4, 66], BF16)
            qiT = [self.sb(st, "cqi", [128, T], BF16) for _ in range(2)]
            kiT = self.sb(st, "cki", [128, T], BF16)
            wsb = self.sb(st, "cw", [128, NT, 8], F32)
            corr = self.sb(st, "ccorr", [128, 512], BF16)
            id4 = self.sb(st, "cid4", [128, 512], BF16)
            P.dve(lambda: nc.vector.memset(V[:], 1.0), w=[("V", tt) for tt in range(NT)])
            for h in range(4):
                P.dve(lambda h=h: nc.vector.memset(QTc[h][:], 0.0), w=[("QT", h)])
                P.dve(lambda h=h: nc.vector.memset(KTc[h][:], 0.0), w=[("KT", h)])
            P.dma("pool", lambda: nc.gpsimd.dma_start(out=corr[:], in_=self.corr_c), w=["ccorr"])
            P.dma("pool", lambda: nc.gpsimd.dma_start(out=id4[:], in_=self.ident4), w=["cid4"])
            for h in range(4):
                P.dma("pool", lambda h=h: nc.gpsimd.dma_start(out=QTc[h][64:67, :], in_=self.augq[h]),
                      r=[("QT", h)], w=[("QTaug", h)])
                P.dma("pool", lambda h=h: nc.gpsimd.dma_start(out=KTc[h][64:67, :], in_=self.augk[h]),
                      r=[("KT", h)], w=[("KTaug", h)])
            with ExitStack() as s2:
                w = self.load_w(s2, "wc", self.w_c[l], 1160)
                wres = [("wc", k) for k in range(KT)]
                load = self.hn_blocks(s2)
                pp = [self.ps(s2, "pp", [128, 512]) for _ in range(4)]
                pi = 0
                for tb in range(T // 512):
                    hb, hres = load(tb)
                    sl = slice(tb * 512, (tb + 1) * 512)
                    for h in range(4):
                        p = pi % 4; pi += 1
                        self.proj_fm(pp[p], ("pp", p), w, wres, h * 64, 64, hb, hres)
                        P.act(lambda p=p, h=h, sl=sl: nc.scalar.mul(QTc[h][0:64, sl], pp[p][0:64, :], 0.125),
                              r=[("pp", p)], w=[("QT", h)])
                        p = pi % 4; pi += 1
                        self.proj_fm(pp[p], ("pp", p), w, wres, 256 + h * 64, 64, hb, hres)
                        P.dve(lambda p=p, h=h, sl=sl: nc.vector.tensor_copy(KTc[h][0:64, sl], pp[p][0:64, :]),
                              r=[("pp", p)], w=[("KT", h)])
                    for i in range(2):
                        p = pi % 4; pi += 1
                        self.proj_fm(pp[p], ("pp", p), w, wres, 768 + i * 128, 128, hb, hres)
                        P.act(lambda p=p, i=i, sl=sl: nc.scalar.copy(qiT[i][:, sl], pp[p][:]),
                              r=[("pp", p)], w=[("cqi", i)])
                    p = pi % 4; pi += 1
                    self.proj_fm(pp[p], ("pp", p), w, wres, 1024, 128, hb, hres)
                    P.dve(lambda p=p, sl=sl: nc.vector.tensor_copy(kiT[:, sl], pp[p][:]), r=[("pp", p)], w=["cki"])
                    for j in range(4):
                        p = pi % 4; pi += 1
                        self.proj_tm(pp[p], ("pp", p), w, wres, 512, 256, hb, hres, j)
                        self.v_evac(V, pp[p], ("pp", p), tb * 4 + j, j)
                        p = pi % 4; pi += 1
                        self.proj_tm(pp[p], ("pp", p), w, wres, 1152, 8, hb, hres, j)
                        P.dve(lambda p=p, tt=tb * 4 + j: nc.vector.tensor_copy(wsb[:, tt, :], pp[p][:, 0:8]),
                              r=[("pp", p)], w=["cw"])
                P.flush()
            with ExitStack() as s3:
                out_fn = self.std_out(s3, "c", 512, ntp=1)
                scores2 = [self.sb(s3, "cscore", [128, T], F32) for _ in range(2)]
                junk = self.sb(s3, "cjunk", [128, T], BF16)
                Mb = [self.sb(s3, "cMb", [128, T], BF16) for _ in range(2)]
                Dg = [[self.sb(s3, "cDg", [128, 128], BF16) for _ in range(8)] for _ in range(2)]
                Qb = [[self.sb(s3, "cQb", [128, 128], BF16) for _ in range(8)] for _ in range(2)]
                R = [self.sb(s3, "cR", [128, 512], BF16) for _ in range(2)]
                X = [self.ps(s3, "cX", [128, 512]) for _ in range(2)]
                SC = self.ps(s3, "cSC", [128, 512])
                st4 = self.sb(s3, "cst", [128, 4 * (NR + 2)], F32)
                thrc = self.sb(s3, "cthrc", [128, 1], F32)
                steps = self.sb(s3, "csteps", [128, NR + 1], F32)
                cpow = self.sb(s3, "ccpow", [128, NR + 1], F32)
                for r in range(NR + 1):
                    P.pool(lambda r=r: nc.gpsimd.memset(cpow[:, r:r + 1], 2.0 ** -(r + 1)), w=["ccpow"])
                P.dve(lambda: nc.vector.memset(thrc[:], -1.0e30), w=["cthrc"])
                for b in range(2):
                    for ih in range(8):
                        P.pool(lambda b=b, ih=ih: nc.gpsimd.memset(Qb[b][ih][:], 0.0), w=[("cQb", b, ih)])
                xi = [0]
                LO, MID, CNT, STP = 0, NR + 2, 2 * (NR + 2), 3 * (NR + 2)

                def before_qt(qt):
                    if qt == 0:
                        do_scores(0)
                        do_scores(1)
                        do_select(0)
                    if qt + 2 < NT:
                        do_scores(qt + 2)
                    if qt + 1 < NT:
                        do_select(qt + 1)

                def do_scores(qt):
                    n = 128 * (qt + 1)
                    b = qt % 2
                    score = scores2[b]
                    qs = slice(qt * 128, (qt + 1) * 128)
                    for ih in range(8):
                        r0 = 32 * (ih % 4)
                        P.pool(lambda ih=ih, r0=r0: nc.gpsimd.tensor_copy(
                            Qb[b][ih][r0:r0 + 32, :], qiT[ih // 4][r0:r0 + 32, qs]), w=[("cQb", b, ih)])
                        P.dve(lambda ih=ih: nc.vector.tensor_scalar(
                            Dg[b][ih][:], self.ident_b[:], wsb[:, qt, ih:ih + 1], None, ALU.mult),
                            w=[("cDg", b, ih)])
                    nkb = (n + 511) // 512
                    for kb in range(nkb):
                        wd = min(512, n - 512 * kb)
                        ks = slice(512 * kb, 512 * kb + wd)
                        for ih in range(8):
                            x = xi[0] % 2
                            xi[0] += 1
                            P.pe(lambda ih=ih, x=x, ks=ks, wd=wd: nc.tensor.matmul(
                                X[x][:, 0:wd], Qb[b][ih][:], kiT[:, ks], start=True, stop=True),
                                r=[("cQb", b, ih)], w=[("cX", x)])
                            P.act(lambda x=x, wd=wd: nc.scalar.activation(out=R[x][:, 0:wd], in_=X[x][:, 0:wd], func=AF.Relu),
                                  r=[("cX", x)], w=[("cR", x)])
                            P.pe(lambda ih=ih, x=x, wd=wd: nc.tensor.matmul(
                                SC[:, 0:wd], Dg[b][ih][:], R[x][:, 0:wd], start=(ih == 0), stop=(ih == 7)),
                                r=[("cR", x), ("cDg", b, ih)], w=["cSC"])
                        P.act(lambda ks=ks, wd=wd: nc.scalar.copy(score[:, ks], SC[:, 0:wd]),
                              r=["cSC"], w=[("cscore", b)])

                def do_select(qt):
                    n = 128 * (qt + 1)
                    b = qt % 2
                    score = scores2[b]
                    P.dve(lambda: nc.vector.memset(score[0:64, n - 64:n], -3.0e38), r=[("cscore", b)], w=[("cscore", b)])
                    if qt >= 2:
                        W0 = CNT + NR + 1
                        MX = MID
                        P.dve(lambda: nc.vector.tensor_reduce(out=st4[:, MX:MX + 1], in_=score[:, 0:n], axis=AX.X, op=ALU.max),
                              r=[("cscore", b)], w=["cmx"])
                        P.dve(lambda: nc.vector.tensor_reduce(out=st4[:, STP:STP + 1], in_=score[:, 0:n - 64], axis=AX.X, op=ALU.min),
                              r=[("cscore", b)], w=["cmn"])
                        P.dve(lambda: nc.vector.tensor_tensor(st4[:, W0:W0 + 1], st4[:, MX:MX + 1], st4[:, STP:STP + 1], ALU.subtract),
                              r=["cmx", "cmn"], w=["cw0"])
                        P.dve(lambda: nc.vector.tensor_scalar(steps[:], cpow[:], st4[:, W0:W0 + 1], None, ALU.mult),
                              r=["cw0", "ccpow"], w=["csteps"])
                        P.dve(lambda: nc.vector.tensor_tensor(st4[:, LO:LO + 1], st4[:, STP:STP + 1], steps[:, 0:1], ALU.add),
                              r=["cmn", "csteps"], w=[("clo", 0)])
                        for r in range(NR):
                            P.dve(lambda r=r: nc.vector.tensor_scalar(
                                junk[:, 0:n], score[:, 0:n], st4[:, LO + r:LO + r + 1], 0.0, ALU.is_gt, ALU.add,
                                accum_out=st4[:, CNT + r:CNT + r + 1]),
                                r=[("cscore", b), ("clo", r)], w=[("ccnt", r), "cjunk"])
                            P.dve(lambda r=r: nc.vector.tensor_scalar(
                                st4[:, STP + 1 + r:STP + 2 + r], st4[:, CNT + r:CNT + r + 1], 255.5, 0.5, ALU.is_gt, ALU.subtract),
                                r=[("ccnt", r)], w=[("cstp", r)])
                            P.dve(lambda r=r: nc.vector.scalar_tensor_tensor(
                                st4[:, LO + r + 1:LO + r + 2], st4[:, STP + 1 + r:STP + 2 + r], steps[:, r:r + 1],
                                st4[:, LO + r:LO + r + 1], ALU.mult, ALU.add),
                                r=[("cstp", r), ("clo", r), "csteps"], w=[("clo", r + 1)])
                        P.dve(lambda: nc.vector.tensor_tensor(st4[:, MX:MX + 1], st4[:, LO + NR:LO + NR + 1], steps[:, NR:NR + 1], ALU.subtract),
                              r=[("clo", NR), "csteps"], w=["cthr"])
                        thr = st4[:, MX:MX + 1]
                        thr_r = ["cthr"]
                    else:
                        thr = thrc[:, 0:1]
                        thr_r = ["cthrc"]
                    P.dve(lambda: nc.vector.tensor_scalar(Mb[b][:, 0:n], score[:, 0:n], thr, NEG, ALU.is_le, ALU.mult),
                          r=[("cscore", b)] + thr_r, w=[("cMb", b)])

                def kts_fn(qt):
                    return list(range(qt + 1))

                def pre_fn(g, qt, kt):
                    b = qt % 2
                    pre = [(Mb[b][:, kt * 128:(kt + 1) * 128], id4[:], [("cMb", b)])]
                    if kt == qt:
                        pre.append((self.ident_b[:], corr[:], []))
                    return pre

                def qk_fn(g, j, qt, kt):
                    return (KTc[j][:, kt * 128:(kt + 1) * 128], QTc[j][:, qt * 128:(qt + 1) * 128])

                def v_fn(g, j, kt):
                    return V[:, kt, j, 0:65]
                self.attn_core(s3, "c", 1, kts_fn, pre_fn, qk_fn, v_fn, out_fn, before_qt=before_qt, ns=2)
                P.flush()

    def phase_wout(self, l):
        nc, P = self.nc, self.P
        with ExitStack() as st:
            wo = self.load_w(st, "wo", self.w_out[l], D)
            wres = [("wo", k) for k in range(KT)]
            ob = [self.sb(st, "wob", [128, KT, 512], BF16) for _ in range(2)]
            xb = [self.sb(st, "wxb", [128, KT, 512], F32) for _ in range(2)]
            py = [self.ps(st, "wpy", [128, 512]) for _ in range(2)]
            oTv = self.oT.rearrange("(kt p) t -> p kt t", p=128)
            xTv = self.xT.rearrange("(kt p) t -> p kt t", p=128)
            pi = 0
            for tb in range(T // 512):
                b = tb % 2
                sl = slice(tb * 512, (tb + 1) * 512)
                P.dma("sp", lambda b=b, sl=sl: nc.sync.dma_start(out=ob[b][:], in_=oTv[:, :, sl]), w=[("wob", b)])
                P.dma("sp", lambda b=b, sl=sl: nc.sync.dma_start(out=xb[b][:], in_=xTv[:, :, sl]),
                      r=[("dram", "xT", tb)], w=[("wxb", b, f) for f in range(KT)])
                for f in range(KT):
                    p = pi % 2; pi += 1
                    for k in range(KT):
                        P.pe(lambda b=b, f=f, k=k, p=p: nc.tensor.matmul(
                            py[p][:], wo[:, k, f * 128:(f + 1) * 128], ob[b][:, k, :],
                            start=(k == 0), stop=(k == KT - 1)),
                            r=[("wob", b)] + wres, w=[("wpy", p)])
                    P.dve(lambda b=b, f=f, p=p: nc.vector.scalar_tensor_tensor(
                        xb[b][:, f, :], py[p][:], self.modv[:, 16 + f:17 + f], xb[b][:, f, :], ALU.mult, ALU.add),
                        r=[("wpy", p), ("wxb", b, f)], w=[("wxb", b, f)])
                P.dma("sp", lambda b=b, sl=sl: nc.sync.dma_start(out=xTv[:, :, sl], in_=xb[b][:]),
                      r=[("wxb", b, f) for f in range(KT)], w=[("dram", "xT", tb)])
            P.flush()

    def phase_ffn(self, l, moe):
        nc, P = self.nc, self.P
        E = NEXP if moe else 1
        NJ = (D_FFE if moe else D_FF) // 128
        NH = 2
        JH = NJ // NH
        w13 = self.moe_w13 if moe else self.ffn_w13
        w2 = self.moe_w2 if moe else self.ffn_w2
        NB = 1024
        with ExitStack() as st:
            hb = self.sb(st, "fhb", [128, KT, NB], BF16)
            xb = self.sb(st, "fxb", [128, KT, NB], F32)
            g = self.sb(st, "fg", [128, JH, NB], BF16)
            w2e = [self.sb(st, "fw2", [128, JH, D], BF16) for _ in range(2)]
            wj = [self.sb(st, "fw13", [128, 2, KT, 128], BF16) for _ in range(6)]
            sl_t = [self.sb(st, "fsl", [128, 512], BF16) for _ in range(2)]
            pa = [self.ps(st, "fpa", [128, 512]) for _ in range(2)]
            pb = [self.ps(st, "fpb", [128, 512]) for _ in range(2)]
            pyy = [self.ps(st, "fpy", [128, 512]) for _ in range(2)]
            ytmp = [self.sb(st, "fyt", [128, 512], F32) for _ in range(2)]
            hTv = self.hnT.rearrange("(kt p) t -> p kt t", p=128)
            xTv = self.xT.rearrange("(kt p) t -> p kt t", p=128)
            if moe:
                cb = self.sb(st, "fcb", [128, E, NB], BF16)
                rt = self.sb(st, "frt", [128, KT, 8], BF16)
                lg = self.sb(st, "flg", [128, 8], F32)
                m8 = self.sb(st, "fm8", [128, 8], F32)
                sc4 = self.sb(st, "fsc4", [128, 8], F32)
                c1 = self.sb(st, "fc1", [128, 8], F32)
                c2 = self.sb(st, "fc2", [128, 8], F32)
                dg = [self.sb(st, "fdg", [128, 128], BF16) for _ in range(2)]
                plg = self.ps(st, "fplg", [128, 512])
                pcb = self.ps(st, "fpcb", [128, 512])
                for k in range(KT):
                    P.dma("pool", lambda k=k: nc.gpsimd.dma_start(out=rt[:, k, :], in_=self.router[k]), w=["frt"])
            wi = 0
            ai = 0
            yi = 0
            w2i = 0
            for tb in range(T // NB):
                sl = slice(tb * NB, (tb + 1) * NB)
                P.dma("sp", lambda sl=sl: nc.sync.dma_start(out=hb[:], in_=hTv[:, :, sl]), w=["fhb"])
                P.dma("sp", lambda sl=sl: nc.sync.dma_start(out=xb[:], in_=xTv[:, :, sl]),
                      w=[("fxb", f, s) for f in range(KT) for s in range(2)])
                if moe:
                    for tt in range(NB // 128):
                        for k in range(KT):
                            P.pe(lambda k=k, tt=tt: nc.tensor.matmul(
                                plg[:, 0:8], hb[:, k, tt * 128:(tt + 1) * 128], rt[:, k, :],
                                start=(k == 0), stop=(k == KT - 1)), r=["fhb", "frt"], w=["fplg"])
                        P.dve(lambda: nc.vector.tensor_copy(lg[:], plg[:, 0:8]), r=["fplg"], w=["flg"])
                        P.dve(lambda: nc.vector.max(out=m8[:], in_=lg[:]), r=["flg"], w=["fm8"])
                        P.dve(lambda: nc.vector.tensor_tensor(sc4[:, 0:1], m8[:, 1:2], m8[:, 0:1], ALU.subtract),
                              r=["fm8"], w=["fsc4a"])
                        P.act(lambda: nc.scalar.activation(out=sc4[:, 1:2], in_=sc4[:, 0:1], func=AF.Exp),
                              r=["fsc4a"], w=["fsc4b"])
                        P.dve(lambda: nc.vector.tensor_scalar(sc4[:, 2:3], sc4[:, 1:2], 1.0, None, ALU.add),
                              r=["fsc4b"], w=["fsc4c"])
                        P.dve(lambda: nc.vector.reciprocal(sc4[:, 3:4], sc4[:, 2:3]), r=["fsc4c"], w=["fsc4d"])
                        P.dve(lambda: nc.vector.tensor_tensor(sc4[:, 4:5], sc4[:, 1:2], sc4[:, 3:4], ALU.mult),
                              r=["fsc4b", "fsc4d"], w=["fsc4e"])
                        P.dve(lambda: nc.vector.tensor_scalar(c1[:], lg[:], m8[:, 0:1], sc4[:, 3:4], ALU.is_equal, ALU.mult),
                              r=["flg", "fm8", "fsc4d"], w=["fc1"])
                        P.dve(lambda: nc.vector.tensor_scalar(c2[:], lg[:], m8[:, 1:2], sc4[:, 4:5], ALU.is_equal, ALU.mult),
                              r=["flg", "fm8", "fsc4e"], w=["fc2"])
                        P.dve(lambda: nc.vector.tensor_tensor(c1[:], c1[:], c2[:], ALU.add),
                              r=["fc1", "fc2"], w=["fc1"])
                        for e in range(E):
                            d = e % 2
                            P.dve(lambda e=e, d=d: nc.vector.tensor_scalar(
                                dg[d][:], self.ident_b[:], c1[:, e:e + 1], None, ALU.mult),
                                r=["fc1", "identb"], w=[("fdg", d)])
                            P.pe(lambda e=e, d=d: nc.tensor.matmul(
                                pcb[:, (e % 4) * 128:(e % 4 + 1) * 128], self.ones_b[:], dg[d][:],
                                start=True, stop=True, skip_group_check=True),
                                r=[("fdg", d), "onesb"], w=["fpcb"])
                            if e % 4 == 3:
                                for q in range(4):
                                    ee = e - 3 + q
                                    P.dve(lambda ee=ee, q=q, tt=tt: nc.vector.tensor_copy(
                                        cb[:, ee, tt * 128:(tt + 1) * 128], pcb[:, q * 128:(q + 1) * 128]),
                                        r=["fpcb"], w=[("fcb", ee)])
                for e in range(E):
                    for hh in range(NH):
                        wb2 = w2i % 2; w2i += 1
                        for q in range(JH):
                            P.dma("pool", lambda e=e, hh=hh, q=q, wb2=wb2: nc.gpsimd.dma_start(
                                out=w2e[wb2][:, q, :], in_=w2[l // 2 if moe else 0, e, hh * JH + q]),
                                w=[("fw2", wb2)])
                        for jj in range(JH):
                            j = hh * JH + jj
                            wbi = wi % 6; wi += 1
                            for m in range(2):
                                P.dma("pool", lambda e=e, j=j, m=m, wbi=wbi: nc.gpsimd.dma_start(
                                    out=wj[wbi][:, m, :, :], in_=w13[l // 2 if moe else 0, e, j, m]),
                                    w=[("fw13", wbi)])
                            for s in range(2):
                                a = ai % 2; ai += 1
                                cs = slice(s * 512, (s + 1) * 512)
                                for k in range(KT):
                                    P.pe(lambda k=k, a=a, wbi=wbi, cs=cs: nc.tensor.matmul(
                                        pa[a][:], wj[wbi][:, 0, k, :], hb[:, k, cs], start=(k == 0), stop=(k == KT - 1)),
                                        r=["fhb", ("fw13", wbi)], w=[("fpa", a)])
                                for k in range(KT):
                                    P.pe(lambda k=k, a=a, wbi=wbi, cs=cs: nc.tensor.matmul(
                                        pb[a][:], wj[wbi][:, 1, k, :], hb[:, k, cs], start=(k == 0), stop=(k == KT - 1)),
                                        r=["fhb", ("fw13", wbi)], w=[("fpb", a)])
                                if "ffn2" in self.dbg2s:
                                    continue
                                P.act(lambda a=a: nc.scalar.activation(out=sl_t[a][:], in_=pa[a][:], func=AF.Silu),
                                      r=[("fpa", a)], w=[("fsl", a)])
                                if "ffn3" in self.dbg2s:
                                    continue
                                if moe:
                                    P.dve(lambda a=a, e=e, cs=cs: nc.vector.tensor_tensor(
                                        sl_t[a][:], sl_t[a][:], cb[:, e, cs], ALU.mult),
                                        r=[("fsl", a), ("fcb", e)], w=[("fsl", a)])
                                P.dve(lambda a=a, jj=jj, cs=cs: nc.vector.tensor_tensor(
                                    g[:, jj, cs], pb[a][:], sl_t[a][:], ALU.mult),
                                    r=[("fpb", a), ("fsl", a)], w=[("fg", jj, s)])
                        for f in range(KT):
                            if self.dbg2s & {"ffn2", "ffn3", "ffn4"}:
                                break
                            for s in range(2):
                                y = yi % 2; yi += 1
                                cs = slice(s * 512, (s + 1) * 512)
                                for jj in range(JH):
                                    P.pe(lambda jj=jj, f=f, cs=cs, y=y, wb2=wb2: nc.tensor.matmul(
                                        pyy[y][:], w2e[wb2][:, jj, f * 128:(f + 1) * 128], g[:, jj, cs],
                                        start=(jj == 0), stop=(jj == JH - 1)),
                                        r=[("fw2", wb2), ("fg", jj, s)], w=[("fpy", y)])
                                if "ffn5" in self.dbg2s:
                                    continue
                                if True:
                                    P.dve(lambda f=f, y=y: nc.vector.tensor_scalar(
                                        ytmp[y][:], pyy[y][:], self.modv[:, 40 + f:41 + f], None, ALU.mult),
                                        r=[("fpy", y)], w=[("fyt", y)])
                                    P.dve(lambda f=f, cs=cs, y=y: nc.vector.tensor_tensor(
                                        xb[:, f, cs], xb[:, f, cs], ytmp[y][:], ALU.add),
                                        r=[("fyt", y), ("fxb", f, s)], w=[("fxb", f, s)])
                                    continue
                                P.dve(lambda f=f, cs=cs, y=y: nc.vector.scalar_tensor_tensor(
                                    xb[:, f, cs], pyy[y][:], self.modv[:, 40 + f:41 + f], xb[:, f, cs], ALU.mult, ALU.add),
                                    r=[("fpy", y), ("fxb", f, s)], w=[("fxb", f, s)])
                P.dma("sp", lambda sl=sl: nc.sync.dma_start(out=xTv[:, :, sl], in_=xb[:]),
                      r=[("fxb", f, s) for f in range(KT) for s in range(2)], w=[("dram", "xT", tb)])
                P.flush()

    def final_norm(self):
        nc, P = self.nc, self.P
        with ExitStack() as st:
            fg = self.sb(st, "fing", [128, KT], F32)
            xb = [self.sb(st, "ox", [128, KT, 512], F32) for _ in range(2)]
            sq = [self.sb(st, "osq", [128, KT, 512], BF16) for _ in range(2)]
            ms = [self.sb(st, "oms", [128, 512], F32) for _ in range(2)]
            yb = [self.sb(st, "oy", [128, KT, 512], F32) for _ in range(2)]
            ot = [self.sb(st, "oot", [128, D], F32) for _ in range(2)]
            pss = [self.ps(st, "ops", [128, 512]) for _ in range(2)]
            ptp = [self.ps(st, "optp", [128, 512]) for _ in range(2)]
            xTv = self.xT.rearrange("(kt p) t -> p kt t", p=128)
            P.dma("sp", lambda: nc.sync.dma_start(out=fg[:], in_=self.fin_g), w=["fing"])
            ti = 0
            pi = 0
            for tb in range(T // 512):
                b = tb % 2
                sl = slice(tb * 512, (tb + 1) * 512)
                P.dma("sp", lambda b=b, sl=sl: nc.sync.dma_start(out=xb[b][:], in_=xTv[:, :, sl]), w=[("ox", b)])
                P.act(lambda b=b: nc.scalar.activation(out=sq[b][:], in_=xb[b][:], func=AF.Square),
                      r=[("ox", b)], w=[("osq", b)])
                for k in range(KT):
                    P.pe(lambda b=b, k=k: nc.tensor.matmul(pss[b][:], self.ones_b[:], sq[b][:, k, :],
                                                           start=(k == 0), stop=(k == KT - 1)),
                         r=[("osq", b), "onesb"], w=[("ops", b)])
                P.dve(lambda b=b: nc.vector.tensor_scalar(ms[b][:], pss[b][:], 1.0 / D, EPS, ALU.mult, ALU.add),
                      r=[("ops", b)], w=[("oms", b)])
                P.act(lambda b=b: nc.scalar.activation(out=ms[b][:], in_=ms[b][:], func=AF.Sqrt),
                      r=[("oms", b)], w=[("oms", b)])
                P.dve(lambda b=b: nc.vector.reciprocal(ms[b][:], ms[b][:]), r=[("oms", b)], w=[("oms", b)])
                for k in range(KT):
                    P.dve(lambda b=b, k=k: nc.vector.scalar_tensor_tensor(
                        yb[b][:, k, :], xb[b][:, k, :], fg[:, k:k + 1], ms[b][:], ALU.mult, ALU.mult),
                        r=[("ox", b), ("oms", b), "fing"], w=[("oy", b, k)])
                for j in range(4):
                    o = ti % 2; ti += 1
                    for half in range(2):
                        p = pi % 2; pi += 1
                        for kk in range(4):
                            k = half * 4 + kk
                            P.pe(lambda b=b, k=k, kk=kk, j=j, p=p: nc.tensor.transpose(
                                ptp[p][:, kk * 128:(kk + 1) * 128], yb[b][:, k, j * 128:(j + 1) * 128], self.ident_f[:]),
                                r=[("oy", b, k), "identf"], w=[("optp", p)])
                        if half == 0:
                            P.dve(lambda o=o, p=p: nc.vector.tensor_copy(ot[o][:, 0:512], ptp[p][:]),
                                  r=[("optp", p)], w=[("oot", o, 0)])
                        else:
                            P.act(lambda o=o, p=p: nc.scalar.copy(ot[o][:, 512:1024], ptp[p][:]),
                                  r=[("optp", p)], w=[("oot", o, 1)])
                    row0 = tb * 512 + j * 128
                    P.dma("sp", lambda o=o, row0=row0: nc.sync.dma_start(out=self.out[row0:row0 + 128, :], in_=ot[o][:]),
                          r=[("oot", o, 0), ("oot", o, 1)], w=[("dram", "out", row0)])
            P.flush()

    def layer(self, l):
        self.phase_mod(l)
        self.phase_norm(0)
        if "a" in self.mixers:
            self.mixer_a(l)
        if "b" in self.mixers:
            self.mixer_b(l)
        if "c" in self.mixers:
            self.mixer_c(l)
        if "d" in self.mixers:
            self.mixer_d(l)
        if self.stop_after == ("mix", l):
            return "stop"
        self.phase_wout(l)
        if self.stop_after == ("wout", l):
            return "stop"
        self.phase_norm(1)
        if self.stop_after == ("preffn", l):
            return "stop"
        self.phase_ffn(l, moe=(l % 2 == 1))


def host_prep(inputs, b, shared=None):
    f = np.float32
    m = dict(shared) if shared is not None else host_shared(inputs)
    m["x"] = np.ascontiguousarray(inputs["x"][b], dtype=f)
    m["c"] = np.ascontiguousarray(np.asarray(inputs["c"][b], dtype=f).reshape(KT, 128).T)
    return m


def host_shared(inputs):
    f = np.float32
    m = {}
    m["ada_w"] = np.ascontiguousarray(np.asarray(inputs["ada_w"], dtype=f).reshape(DEPTH, KT, 128, 6 * D))
    m["ada_b"] = np.ascontiguousarray(np.asarray(inputs["ada_b"], dtype=f).reshape(DEPTH, 48, 128).transpose(0, 2, 1))
    m["mix_g"] = np.ascontiguousarray(np.asarray(inputs["mix_norm_g"], dtype=f).reshape(DEPTH, KT, 128).transpose(0, 2, 1))
    m["ffn_g"] = np.ascontiguousarray(np.asarray(inputs["ffn_norm_g"], dtype=f).reshape(DEPTH, KT, 128).transpose(0, 2, 1))
    m["fin_g"] = np.ascontiguousarray(np.asarray(inputs["final_norm_g"], dtype=f).reshape(KT, 128).T)
    m["ident"] = np.eye(128, dtype=f)
    w_in = np.asarray(inputs["w_in"], dtype=f)

    def cols(a, b):
        return w_in[:, :, a:b]
    sl_ = np.arange(128)[:, None]
    tl_ = np.arange(128)[None, :]

    def tiles(w):
        return np.ascontiguousarray(w.reshape(DEPTH, KT, 128, w.shape[-1]))
    m["w_out"] = np.ascontiguousarray(np.asarray(inputs["w_out"], dtype=f).reshape(DEPTH, KT, 128, D))

    def w13_layout(w1, w3):
        E_, _, F_ = w1.shape
        a = np.stack([w1, w3], axis=1)
        a = a.reshape(E_, 2, KT, 128, F_ // 128, 128)
        return np.ascontiguousarray(a.transpose(0, 4, 1, 3, 2, 5))

    m["ffn_w13"] = w13_layout(np.asarray(inputs["ffn_w1"], dtype=f), np.asarray(inputs["ffn_w3"], dtype=f))[None]
    m["ffn_w2"] = np.ascontiguousarray(np.asarray(inputs["ffn_w2"], dtype=f).reshape(1, 1, D_FF // 128, 128, D))
    m["moe_w13"] = w13_layout(np.asarray(inputs["moe_w1"], dtype=f)[0], np.asarray(inputs["moe_w3"], dtype=f)[0])[None]
    m["moe_w2"] = np.ascontiguousarray(np.asarray(inputs["moe_w2"], dtype=f).reshape(1, NEXP, D_FFE // 128, 128, D))
    m["router"] = np.ascontiguousarray(np.asarray(inputs["moe_router"], dtype=f)[0].reshape(KT, 128, 8))
    perm = np.concatenate([np.arange(16, 32), np.arange(0, 16)])
    z64 = np.zeros((DEPTH, D, 64), f)
    kr = cols(1152, 1184)
    m["w_b"] = tiles(np.concatenate([cols(768, 1024), cols(1024, 1152), z64, kr, z64, kr[:, :, perm]], axis=-1))
    wuq = np.asarray(inputs["mla_w_uq"], dtype=f).reshape(DEPTH, 256, 4, 96)
    wuqr = np.zeros_like(wuq)
    wuqr[:, :, :, 64:96] = wuq[:, :, :, 64:96][:, :, :, perm]
    m["w_uq"] = np.ascontiguousarray(wuq.reshape(DEPTH, 2, 128, 384))
    m["w_uqr"] = np.ascontiguousarray(wuqr.reshape(DEPTH, 2, 128, 384))
    wukv = np.asarray(inputs["mla_w_ukv"], dtype=f).reshape(DEPTH, 128, 4, 128)
    m["w_ukvk"] = np.ascontiguousarray(wukv[:, :, :, 0:64].reshape(DEPTH, 1, 128, 256))
    m["w_ukvv"] = np.ascontiguousarray(wukv[:, :, :, 64:128].reshape(DEPTH, 1, 128, 256))
    m["gq"] = np.ascontiguousarray(np.asarray(inputs["mla_q_norm_g"], dtype=f).reshape(DEPTH, 2, 128).transpose(0, 2, 1))
    m["gkv"] = np.ascontiguousarray(np.asarray(inputs["mla_kv_norm_g"], dtype=f).reshape(DEPTH, 128, 1))
    inv = (np.float32(10000.0) ** (-np.arange(0, 32, 2, dtype=f) / np.float32(32))).astype(f)
    ang = (np.arange(T, dtype=f)[:, None] * inv[None, :]).astype(f)
    cs_, sn_ = np.cos(ang).astype(f), np.sin(ang).astype(f)
    rope = np.zeros((128, 2, T), f)
    rope[64:80, 0] = cs_.T; rope[80:96, 0] = cs_.T
    rope[64:80, 1] = -sn_.T; rope[80:96, 1] = sn_.T
    m["rope"] = rope
    chunkmask = np.where(sl_ // 64 > tl_ // 64, f(NEG), f(0.0)).astype(f)
    m["diagmask"] = np.ascontiguousarray(np.tile(chunkmask, (1, 4)))
    m["w_a"] = tiles(np.concatenate([cols(0, 256), cols(256, 512), cols(512, 768)], axis=-1))
    slopes = (2.0 ** (-8.0 * np.arange(1, 5, dtype=f) / 4)).astype(f)
    tpos = np.arange(T, dtype=f)
    augq = np.zeros((4, 3, T), f); augk = np.zeros((4, 3, T), f)
    for h in range(4):
        augq[h, 0] = 1.0; augq[h, 1] = 1.0; augq[h, 2] = -slopes[h] * tpos
        augk[h, 0] = slopes[h] * (64.0 * (tpos // 64)); augk[h, 1] = slopes[h] * (tpos % 64); augk[h, 2] = 1.0
    m["augq"] = augq; m["augk"] = augk

    def corr_tile(sig):
        c = np.where(sl_ > tl_, -2.0 * sig * (sl_ - tl_), 0.0).astype(f)
        return np.where(sl_ // 64 > tl_ // 64, f(NEG), c).astype(f)
    ca = np.zeros((2, 128, 4, 128), f)
    for g in range(2):
        for j in range(4):
            ca[g, :, j, :] = corr_tile(slopes[2 * g + j // 2])
    m["corr_a"] = np.ascontiguousarray(ca.reshape(2, 128, 512))
    dl = np.asarray(inputs["diff_lambda"], dtype=f).reshape(DEPTH, 1, 128)
    m["dlam"] = np.ascontiguousarray(np.broadcast_to(dl, (DEPTH, 128, 128)))
    dg_ = np.asarray(inputs["diff_norm_g"], dtype=f).reshape(DEPTH, 1, 64)
    m["dng"] = np.ascontiguousarray(np.broadcast_to(dg_, (DEPTH, 128, 64)))
    ki = cols(2208, 2240)
    m["w_c"] = tiles(np.concatenate([cols(1184, 1440), cols(1440, 1696), cols(1696, 1952), cols(1952, 2208),
                                     ki, ki, ki, ki, cols(2240, 2248)], axis=-1))
    cc = np.zeros((128, 4, 128), f)
    for j in range(4):
        cc[:, j, :] = corr_tile(slopes[j])
    m["corr_c"] = np.ascontiguousarray(cc.reshape(128, 512))
    m["ident4"] = np.ascontiguousarray(np.tile(np.eye(128, dtype=f), (1, 4)))
    m["w_d"] = tiles(np.concatenate([cols(2248, 2504), cols(2504, 2760), cols(2760, 3016)], axis=-1))
    rb = np.asarray(inputs["band_rel_bias"], dtype=f)
    bd = np.empty((DEPTH, 5, 128, 4, 128), f)
    for dd in range(5):
        delta = 128 * dd + tl_ - sl_
        idx = np.clip(delta, -128, 128) + 128
        dc = (128 * dd + tl_) // 64 - sl_ // 64
        ok = (dc >= 0) & (dc <= 8)
        for h in range(4):
            g = rb[:, h, :][:, idx]
            bd[:, dd, :, h, :] = np.where(ok[None], g, f(NEG))
    m["bias_d"] = np.ascontiguousarray(bd.reshape(DEPTH, 5, 128, 512))
    return m


def kernel(**inputs):
    bld = Builder()
    nc = bld.build()
    shared = host_shared(inputs)
    in_maps = []
    for b in range(8):
        m = host_prep(inputs, b, shared)
        in_maps.append({k: v for k, v in m.items() if k in bld.inputs})
    res = run_bass_kernel_spmd(nc, in_maps, core_ids=list(range(8)))
    return np.stack([np.asarray(r["out"]) for r in res.results], axis=0).astype(np.float32)
```

```python
import math
import os
from contextlib import ExitStack
import numpy as np
import concourse.bass as bass
import concourse.mybir as mybir
from concourse.bass_utils import run_bass_kernel_spmd

F32 = mybir.dt.float32
BF16 = mybir.dt.bfloat16
AF = mybir.ActivationFunctionType
ALU = mybir.AluOpType
AX = mybir.AxisListType

D = 1024
T = 4096
DEPTH = 2
NT = T // 128
KT = D // 128
EPS = 1e-6
D_FF = 2816
D_FFE = 3584
NEXP = 8
NEG = -30000.0


class Prog:
    NSLOT = 12

    def __init__(self, nc, es):
        self.nc = nc
        self.ops = []
        self.engs = {"pe": nc.tensor, "act": nc.scalar, "dve": nc.vector,
                     "pool": nc.gpsimd, "sp": nc.sync}
        self.esem = {e: es.enter_context(nc.semaphore("sem_" + e)) for e in self.engs}
        self.dq = ("sp", "act", "pool")
        self.dsem = {q: [es.enter_context(nc.semaphore("dsem_%s%d" % (q, k))) for k in range(self.NSLOT)]
                     for q in self.dq}
        self.ecount = {e: 0 for e in self.engs}
        self.dcount = {q: 0 for q in self.dq}
        self.eclock = {e: {} for e in self.engs}
        self.sig = {}
        self.done_clock = {}
        self.gid = 0
        self.last_comp = {}
        self.dhist = {q: [] for q in self.dq}
        self.n_ops = 0
        self.n_waits = 0
        self.n_sig = 0

    def op(self, eng, fn, reads=(), writes=(), dma=False):
        self.ops.append((eng, fn, tuple(reads), tuple(writes), dma))

    def pe(self, fn, r=(), w=()): self.op("pe", fn, r, w)
    def act(self, fn, r=(), w=()): self.op("act", fn, r, w)
    def dve(self, fn, r=(), w=()): self.op("dve", fn, r, w)
    def pool(self, fn, r=(), w=()): self.op("pool", fn, r, w)
    def dma(self, q, fn, r=(), w=()): self.op(q, fn, r, w, True)

    def _sem(self, sk):
        return self.esem[sk[1]] if sk[0] == "e" else self.dsem[sk[1]][sk[2]]

    def _bar_deps(self):
        d = set(self.last_comp.values())
        for q in self.dq:
            d.update(self.dhist[q][-self.NSLOT:])
        return d

    def flush(self):
        ops = self.ops
        self.ops = []
        n = len(ops)
        base = self.gid
        self.gid += n
        bar = self._bar_deps()
        last_w = {}
        readers = {}
        deps = [None] * n
        seen = set()
        openg = {}
        for i, (eng, fn, rd, wr, is_dma) in enumerate(ops):
            g = base + i
            openg[g] = (eng, is_dma)
            d = set()
            if eng not in seen:
                seen.add(eng)
                d |= bar
            for r in rd:
                lw = last_w.get(r)
                if lw is not None:
                    d.add(lw)
            for r in wr:
                lw = last_w.get(r)
                if lw is not None:
                    d.add(lw)
                for x in readers.get(r, ()):
                    d.add(x)
            for r in rd:
                readers.setdefault(r, []).append(g)
            for r in wr:
                last_w[r] = g
                readers[r] = []
            if is_dma:
                h = self.dhist[eng]
                if len(h) >= self.NSLOT:
                    d.add(h[-self.NSLOT])
                h.append(g)
            d.discard(g)
            if eng == "pe" and not is_dma:
                d = {x for x in d if not (x >= base and openg[x] == ("pe", False))}
            deps[i] = d
            if not is_dma:
                self.last_comp[eng] = g
        signal = set(self.last_comp.values())
        for i in range(n):
            if ops[i][4]:
                signal.add(base + i)
            signal |= deps[i]
        for q in self.dq:
            self.dhist[q] = self.dhist[q][-self.NSLOT:]
        for i, (eng, fn, rd, wr, is_dma) in enumerate(ops):
            g = base + i
            if is_dma:
                k = self.dcount[eng]
                self.dcount[eng] += 1
                self.sig[g] = (("d", eng, k % self.NSLOT), 16 * (k // self.NSLOT + 1))
            elif g in signal:
                self.ecount[eng] += 1
                self.sig[g] = (("e", eng), self.ecount[eng])
        for i, (eng, fn, rd, wr, is_dma) in enumerate(ops):
            g = base + i
            clk = self.eclock[eng]
            e = self.engs[eng]
            need = {}
            for x in deps[i]:
                sk, sv = self.sig[x]
                if clk.get(sk, 0) < sv and need.get(sk, 0) < sv:
                    need[sk] = sv
            for x in deps[i]:
                for k2, v2 in self.done_clock[x].items():
                    if clk.get(k2, 0) < v2:
                        clk[k2] = v2
            for sk, sv in need.items():
                e.wait_ge(self._sem(sk), sv)
                self.n_waits += 1
                if clk.get(sk, 0) < sv:
                    clk[sk] = sv
            inst = fn()
            if g in self.sig:
                sk, sv = self.sig[g]
                inst.then_inc(self._sem(sk), 16 if is_dma else 1)
                dc = dict(clk)
                dc[sk] = sv
                self.done_clock[g] = dc
                self.n_sig += 1
        self.n_ops += n
        keep = self._bar_deps()
        self.sig = {g: v for g, v in self.sig.items() if g in keep}
        self.done_clock = {g: v for g, v in self.done_clock.items() if g in keep}

    def finish(self):
        self.flush()
        e = self.nc.sync
        clk = self.eclock["sp"]
        for x in self._bar_deps():
            sk, sv = self.sig[x]
            if clk.get(sk, 0) < sv:
                e.wait_ge(self._sem(sk), sv)
                clk[sk] = sv
        return dict(n_ops=self.n_ops, n_waits=self.n_waits, n_sig=self.n_sig, ecount=dict(self.ecount), dcount=dict(self.dcount))


class Builder:
    def __init__(self, debug=None, stop_after=None, mixers="abcd"):
        self.mixers = mixers
        import os
        self.dbg = os.environ.get("MK_DBG", "")
        self.dbg2s = set(os.environ.get("MK_DBG2", "").split(","))
        self.dbg2 = ""
        self.debug = debug or []
        self.stop_after = stop_after
        self.nc = bass.Bass("TRN2", target_bir_lowering=False)
        self.es = ExitStack()
        self.P = Prog(self.nc, self.es)
        self.uid = 0
        self.inputs = {}

    def din(self, name, shape, dt=F32):
        t = self.nc.dram_tensor(name, list(shape), dt, kind="ExternalInput").ap()
        self.inputs[name] = t
        return t

    def dscr(self, name, shape, dt):
        kind = "ExternalOutput"
        return self.nc.dram_tensor(name, list(shape), dt, kind=kind).ap()

    def sb(self, stack, name, shape, dt):
        self.uid += 1
        return stack.enter_context(self.nc.sbuf_tensor("%s_%d" % (name, self.uid), list(shape), dt))

    def ps(self, stack, name, shape, dt=F32):
        self.uid += 1
        return stack.enter_context(self.nc.psum_tensor("%s_%d" % (name, self.uid), list(shape), dt))

    def build(self):
        nc, P = self.nc, self.P
        with self.es as es:
            self.declare_io()
            self.consts(es)
            P.flush()
            self.phase_l0()
            for l in range(DEPTH):
                if self.layer(l) == "stop" or self.stop_after == ("layer", l):
                    break
            else:
                self.final_norm()
            st = P.finish()
        self.stats = st
        return nc

    def declare_io(self):
        nc = self.nc
        self.x_in = self.din("x", [T, D])
        self.c_in = self.din("c", [128, KT])
        self.ada_w = self.din("ada_w", [DEPTH, KT, 128, 6 * D])
        self.ada_b = self.din("ada_b", [DEPTH, 128, 48])
        self.mix_g = self.din("mix_g", [DEPTH, 128, KT])
        self.ffn_g = self.din("ffn_g", [DEPTH, 128, KT])
        self.fin_g = self.din("fin_g", [128, KT])
        self.ident_in = self.din("ident", [128, 128])
        self.w_d = self.din("w_d", [DEPTH, KT, 128, 768])
        self.w_out = self.din("w_out", [DEPTH, KT, 128, D])
        self.w_b = self.din("w_b", [DEPTH, KT, 128, 576])
        self.w_uq = self.din("w_uq", [DEPTH, 2, 128, 384])
        self.w_uqr = self.din("w_uqr", [DEPTH, 2, 128, 384])
        self.w_ukvk = self.din("w_ukvk", [DEPTH, 1, 128, 256])
        self.w_ukvv = self.din("w_ukvv", [DEPTH, 1, 128, 256])
        self.gq = self.din("gq", [DEPTH, 128, 2])
        self.gkv = self.din("gkv", [DEPTH, 128, 1])
        self.rope = self.din("rope", [128, 2, T])
        self.diagmask = self.din("diagmask", [128, 512])
        self.w_a = self.din("w_a", [DEPTH, KT, 128, 768])
        self.w_c = self.din("w_c", [DEPTH, KT, 128, 1160])
        self.corr_c = self.din("corr_c", [128, 512])
        self.ident4 = self.din("ident4", [128, 512])
        self.corr_a = self.din("corr_a", [2, 128, 512])
        self.augq = self.din("augq", [4, 3, T])
        self.augk = self.din("augk", [4, 3, T])
        self.dlam = self.din("dlam", [DEPTH, 128, 128])
        self.dng = self.din("dng", [DEPTH, 128, 64])
        self.ffn_w13 = self.din("ffn_w13", [1, 1, D_FF // 128, 2, 128, KT, 128])
        self.ffn_w2 = self.din("ffn_w2", [1, 1, D_FF // 128, 128, D])
        self.moe_w13 = self.din("moe_w13", [1, NEXP, D_FFE // 128, 2, 128, KT, 128])
        self.moe_w2 = self.din("moe_w2", [1, NEXP, D_FFE // 128, 128, D])
        self.router = self.din("router", [KT, 128, 8])
        self.bias_d = self.din("bias_d", [DEPTH, 5, 128, 512])
        self.out = nc.dram_tensor("out", [T, D], F32, kind="ExternalOutput").ap()
        self.xT = self.dscr("xT", [D, T], F32)
        self.hnT = self.dscr("hnT", [D, T], BF16)
        self.oT = self.dscr("oT", [D, T], BF16)

    def consts(self, es):
        nc, P = self.nc, self.P
        self.ident_f = self.sb(es, "identf", [128, 128], F32)
        self.ident_b = self.sb(es, "identb", [128, 128], BF16)
        self.ones_b = self.sb(es, "onesb", [128, 128], BF16)
        self.modv = self.sb(es, "modv", [128, 48], F32)
        self.gvec = self.sb(es, "gvec", [128, 4 * KT], F32)
        P.dma("sp", lambda: nc.sync.dma_start(out=self.ident_f[:], in_=self.ident_in), w=["identf"])
        P.dve(lambda: nc.vector.tensor_copy(self.ident_b[:], self.ident_f[:]), r=["identf"], w=["identb"])
        P.dve(lambda: nc.vector.memset(self.ones_b[:], 1.0), w=["onesb"])

    def phase_l0(self):
        nc, P = self.nc, self.P
        with ExitStack() as st:
            xin = [self.sb(st, "xin", [128, 4, D], F32) for _ in range(2)]
            xo = [self.sb(st, "xo", [128, KT, 512], F32) for _ in range(2)]
            pt = [self.ps(st, "pt", [128, 512]) for _ in range(2)]
            xv = self.x_in.rearrange("(tb j p) f -> tb p j f", j=4, p=128)
            xTv = self.xT.rearrange("(kt p) t -> p kt t", p=128)
            for tb in range(T // 512):
                b = tb % 2
                P.dma("sp", lambda tb=tb, b=b: nc.sync.dma_start(out=xin[b][:], in_=xv[tb]),
                      w=[("xin", b)])
                for kt in range(KT):
                    pb = kt % 2
                    for j in range(4):
                        P.pe(lambda b=b, kt=kt, j=j, pb=pb: nc.tensor.transpose(
                            pt[pb][:, j * 128:(j + 1) * 128], xin[b][:, j, kt * 128:(kt + 1) * 128],
                            self.ident_f[:]),
                            r=[("xin", b), "identf"], w=[("pt", pb)])
                    if kt % 2 == 0:
                        P.dve(lambda b=b, kt=kt, pb=pb: nc.vector.tensor_copy(xo[b][:, kt, :], pt[pb][:]),
                              r=[("pt", pb)], w=[("xo", b, kt)])
                    else:
                        P.act(lambda b=b, kt=kt, pb=pb: nc.scalar.copy(xo[b][:, kt, :], pt[pb][:]),
                              r=[("pt", pb)], w=[("xo", b, kt)])
                P.dma("sp", lambda tb=tb, b=b: nc.sync.dma_start(
                    out=xTv[:, :, tb * 512:(tb + 1) * 512], in_=xo[b][:]),
                    r=[("xo", b, kt) for kt in range(KT)], w=[("dram", "xT", tb)])
            P.flush()

    def phase_mod(self, l):
        nc, P = self.nc, self.P
        with ExitStack() as st:
            cs = self.sb(st, "cs", [128, KT], F32)
            sc = self.sb(st, "sc", [128, KT], BF16)
            wb = [self.sb(st, "adaw", [128, 6 * D], BF16) for _ in range(2)]
            ab = self.sb(st, "adab", [128, 48], F32)
            gg = self.sb(st, "gg", [128, 2 * KT], F32)
            pm = self.ps(st, "pm", [128, 512])
            P.dma("sp", lambda: nc.sync.dma_start(out=cs[:], in_=self.c_in), w=["cs"])
            P.dma("sp", lambda: nc.sync.dma_start(out=ab[:], in_=self.ada_b[l]), w=["adab"])
            P.dma("sp", lambda: nc.sync.dma_start(out=gg[:, 0:KT], in_=self.mix_g[l]), w=["gg0"])
            P.dma("sp", lambda: nc.sync.dma_start(out=gg[:, KT:2 * KT], in_=self.ffn_g[l]), w=["gg1"])
            P.act(lambda: nc.scalar.activation(out=sc[:], in_=cs[:], func=AF.Silu), r=["cs"], w=["sc"])
            P.dve(lambda: nc.vector.memset(pm[:], 0.0), w=["pm"])
            for kt in range(KT):
                b = kt % 2
                P.dma("pool", lambda kt=kt, b=b: nc.gpsimd.dma_start(out=wb[b][:], in_=self.ada_w[l, kt]),
                      w=[("adaw", b)])
                for ft in range(48):
                    P.pe(lambda kt=kt, b=b, ft=ft: nc.tensor.matmul(
                        pm[:, ft:ft + 1], wb[b][:, ft * 128:(ft + 1) * 128], sc[:, kt:kt + 1],
                        start=False, stop=(kt == KT - 1), skip_group_check=True),
                        r=[("adaw", b), "sc"], w=["pm"])
            mv = self.modv
            P.dve(lambda: nc.vector.tensor_tensor(mv[:], pm[:, 0:48], ab[:], ALU.add),
                  r=["pm", "adab"], w=["modv"])
            gv = self.gvec
            P.dve(lambda: nc.vector.scalar_tensor_tensor(
                gv[:, 0:KT], mv[:, 8:16], 1.0, gg[:, 0:KT], ALU.add, ALU.mult),
                r=["modv", "gg0"], w=["gvec0"])
            P.dve(lambda: nc.vector.scalar_tensor_tensor(
                gv[:, KT:2 * KT], mv[:, 32:40], 1.0, gg[:, KT:2 * KT], ALU.add, ALU.mult),
                r=["modv", "gg1"], w=["gvec1"])
            P.flush()

    def phase_norm(self, which):
        nc, P = self.nc, self.P
        goff = which * KT
        shoff = 0 if which == 0 else 24
        with ExitStack() as st:
            xb = [self.sb(st, "nx", [128, KT, 512], F32) for _ in range(2)]
            sq = [self.sb(st, "nsq", [128, KT, 512], BF16) for _ in range(2)]
            ms = [self.sb(st, "nms", [128, 512], F32) for _ in range(2)]
            tmp = [self.sb(st, "ntmp", [128, KT, 512], F32) for _ in range(2)]
            hb = [self.sb(st, "nhb", [128, KT, 512], BF16) for _ in range(2)]
            pss = [self.ps(st, "nps", [128, 512]) for _ in range(2)]
            xTv = self.xT.rearrange("(kt p) t -> p kt t", p=128)
            hTv = self.hnT.rearrange("(kt p) t -> p kt t", p=128)
            for tb in range(T // 512):
                b = tb % 2
                P.dma("sp", lambda tb=tb, b=b: nc.sync.dma_start(
                    out=xb[b][:], in_=xTv[:, :, tb * 512:(tb + 1) * 512]),
                    r=[("dram", "xT", tb)], w=[("nx", b)])
                P.act(lambda b=b: nc.scalar.activation(out=sq[b][:], in_=xb[b][:], func=AF.Square),
                      r=[("nx", b)], w=[("nsq", b)])
                for kt in range(KT):
                    P.pe(lambda b=b, kt=kt: nc.tensor.matmul(
                        pss[b][:], self.ones_b[:], sq[b][:, kt, :], start=(kt == 0), stop=(kt == KT - 1)),
                        r=[("nsq", b), "onesb"], w=[("nps", b)])
                P.dve(lambda b=b: nc.vector.tensor_scalar(
                    ms[b][:], pss[b][:], 1.0 / D, EPS, ALU.mult, ALU.add),
                    r=[("nps", b)], w=[("nms", b)])
                P.act(lambda b=b: nc.scalar.activation(out=ms[b][:], in_=ms[b][:], func=AF.Sqrt),
                      r=[("nms", b)], w=[("nms", b)])
                P.dve(lambda b=b: nc.vector.reciprocal(ms[b][:], ms[b][:]),
                      r=[("nms", b)], w=[("nms", b)])
                for kt in range(KT):
                    P.dve(lambda b=b, kt=kt: nc.vector.scalar_tensor_tensor(
                        tmp[b][:, kt, :], xb[b][:, kt, :], self.gvec[:, goff + kt:goff + kt + 1],
                        ms[b][:], ALU.mult, ALU.mult),
                        r=[("nx", b), ("nms", b), "gvec%d" % which], w=[("ntmp", b, kt)])
                    P.act(lambda b=b, kt=kt: nc.scalar.activation(
                        out=hb[b][:, kt, :], in_=tmp[b][:, kt, :], func=AF.Identity,
                        bias=self.modv[:, shoff + kt:shoff + kt + 1], scale=1.0),
                        r=[("ntmp", b, kt), "modv"], w=[("nhb", b, kt)])
                P.dma("sp", lambda tb=tb, b=b: nc.sync.dma_start(
                    out=hTv[:, :, tb * 512:(tb + 1) * 512], in_=hb[b][:]),
                    r=[("nhb", b, kt) for kt in range(KT)], w=[("dram", "hnT", tb)])
            P.flush()

    def load_w(self, st, name, dram_ap, ncols, nk=KT):
        nc, P = self.nc, self.P
        w = self.sb(st, name, [128, nk, ncols], BF16)
        for k in range(nk):
            P.dma("pool", lambda k=k: nc.gpsimd.dma_start(out=w[:, k, :], in_=dram_ap[k]), w=[(name, k)])
        return w

    def hn_blocks(self, st):
        nc, P = self.nc, self.P
        hb = [self.sb(st, "hb", [128, KT, 512], BF16) for _ in range(2)]
        hTv = self.hnT.rearrange("(kt p) t -> p kt t", p=128)

        def load(tb):
            b = tb % 2
            P.dma("sp", lambda: nc.sync.dma_start(out=hb[b][:], in_=hTv[:, :, tb * 512:(tb + 1) * 512]),
                  w=[("hb", b)])
            return hb[b], ("hb", b)
        return load

    def proj_fm(self, pp, ppr, w, wres, c0, M, hb, hres, nk=KT):
        nc, P = self.nc, self.P
        for k in range(nk):
            P.pe(lambda k=k: nc.tensor.matmul(pp[0:M, :], w[:, k, c0:c0 + M], hb[:, k, :],
                                              start=(k == 0), stop=(k == nk - 1)),
                 r=[hres] + wres, w=[ppr])

    def proj_tm(self, pp, ppr, w, wres, c0, n, hb, hres, j, nk=KT):
        nc, P = self.nc, self.P
        for k in range(nk):
            P.pe(lambda k=k: nc.tensor.matmul(pp[:, 0:n], hb[:, k, j * 128:(j + 1) * 128], w[:, k, c0:c0 + n],
                                              start=(k == 0), stop=(k == nk - 1)),
                 r=[hres] + wres, w=[ppr])

    def attn_core(self, st, name, groups, kts_fn, pre_fn, qk_fn, v_fn, out_fn, before_qt=None, ns=3):
        nc, P = self.nc, self.P
        S = [self.ps(st, "S", [128, 512]) for _ in range(ns)]
        O = [self.ps(st, "O", [128, 512]) for _ in range(2)]
        Pt = [self.sb(st, "Pt", [128, 512], BF16) for _ in range(ns + 1)]
        rl = [self.sb(st, "rl", [128, 4], F32) for _ in range(2)]
        if os.environ.get("MK_NOATTN"):
            return
        tasks = []
        oi = 0
        for qt in range(NT):
            for g in range(groups):
                kts = kts_fn(qt)
                for ki, kt in enumerate(kts):
                    tasks.append((qt, g, ki, kt, len(kts), oi % 2, len(tasks) % ns, len(tasks) % (ns + 1)))
                oi += 1

        def emit_qk(t):
            qt, g, ki, kt, nk, ob, sbk, pb = t
            if ki == 0 and g == 0 and before_qt is not None:
                before_qt(qt)
            pre = pre_fn(g, qt, kt)
            first = True
            for (lh, rh, rr) in pre:
                P.pe(lambda lh=lh, rh=rh, first=first: nc.tensor.matmul(
                    S[sbk][:], lh, rh, start=first, stop=False, skip_group_check=True),
                    r=rr, w=[("S", sbk)])
                first = False
            for j in range(4):
                lh, rh = qk_fn(g, j, qt, kt)
                P.pe(lambda lh=lh, rh=rh, first=first, j=j: nc.tensor.matmul(
                    S[sbk][:, j * 128:(j + 1) * 128], lh, rh, start=first, stop=(j == 3),
                    skip_group_check=True), w=[("S", sbk)])
                first = False

        def emit_exp(t):
            qt, g, ki, kt, nk, ob, sbk, pb = t
            P.act(lambda: nc.scalar.activation(out=Pt[pb][:], in_=S[sbk][:], func=AF.Exp),
                  r=[("S", sbk)], w=[("Pt", pb)])

        def emit_pv(t):
            qt, g, ki, kt, nk, ob, sbk, pb = t
            for j in range(4):
                va = v_fn(g, j, kt)
                P.pe(lambda va=va, j=j: nc.tensor.matmul(
                    O[ob][:, j * 66:j * 66 + 65], Pt[pb][:, j * 128:(j + 1) * 128], va,
                    start=(ki == 0 and j == 0), stop=(ki == nk - 1), skip_group_check=True),
                    r=[("Pt", pb)], w=[("O", ob)])
            if ki == nk - 1:
                P.dve(lambda: nc.vector.reciprocal(
                    rl[ob][:, :], O[ob][:, 0:264].rearrange("p (j c) -> p j c", c=66)[:, :, 64]),
                    r=[("O", ob)], w=[("rl", ob)])
                out_fn(g, qt, O[ob], ("O", ob), rl[ob], ("rl", ob))

        la = ns - 1
        for i in range(min(la, len(tasks))):
            emit_qk(tasks[i])
        for i, t in enumerate(tasks):
            emit_exp(t)
            if i + la < len(tasks):
                emit_qk(tasks[i + la])
            emit_pv(t)

    def std_out(self, st, name, moff, ntp=2):
        nc, P = self.nc, self.P
        on = [self.sb(st, "on", [128, 256], BF16) for _ in range(2)]
        tp = [self.ps(st, "tp", [128, 2, 128], BF16) for _ in range(ntp)]
        stage = [self.sb(st, "ostg", [128, 2, 512], BF16) for _ in range(2)]
        oTv = self.oT[moff:moff + 256, :].rearrange("(i p) t -> p i t", p=128)

        def out_fn(g, qt, O, Or, rl, rlr):
            nb = qt % 2
            sbi = (qt // 4) % 2
            q4 = qt % 4
            tpb = qt % ntp
            for j in range(4):
                if True:
                    P.dve(lambda j=j: nc.vector.tensor_scalar(
                        on[nb][:, j * 64:(j + 1) * 64], O[:, j * 66:j * 66 + 64], rl[:, j:j + 1], None, ALU.mult),
                        r=[Or, rlr], w=[("on", nb, j)])
                else:
                    P.act(lambda j=j: nc.scalar.activation(
                        out=on[nb][:, j * 64:(j + 1) * 64], in_=O[:, j * 66:j * 66 + 64], func=AF.Copy,
                        scale=rl[:, j:j + 1]),
                        r=[Or, rlr], w=[("on", nb, j)])
            if "notp" in self.dbg2s:
                return
            for i in range(2):
                P.pe(lambda i=i: nc.tensor.transpose(tp[tpb][:, i, :], on[nb][:, i * 128:(i + 1) * 128],
                                                     self.ident_b[:]),
                     r=[("on", nb, 2 * i), ("on", nb, 2 * i + 1)], w=[("tp", tpb)])
            if "nostg" in self.dbg2s:
                return
            P.dve(lambda: nc.vector.tensor_copy(stage[sbi][:, 0, q4 * 128:(q4 + 1) * 128], tp[tpb][:, 0, :]),
                  r=[("tp", tpb)], w=[("ostg", sbi, 0)])
            P.dve(lambda: nc.vector.tensor_copy(stage[sbi][:, 1, q4 * 128:(q4 + 1) * 128], tp[tpb][:, 1, :]),
                  r=[("tp", tpb)], w=[("ostg", sbi, 1)])
            if q4 == 3 and "nodma" not in self.dbg2s:
                tb = qt // 4
                P.dma("sp", lambda: nc.sync.dma_start(out=oTv[:, :, tb * 512:(tb + 1) * 512], in_=stage[sbi][:]),
                      r=[("ostg", sbi, 0), ("ostg", sbi, 1)], w=[("dram", "oT", name, tb)])
        return out_fn

    def v_evac(self, V, pp, ppr, tt, eng_i):
        nc, P = self.nc, self.P
        for h in range(4):
            if (eng_i + h) % 2 == 0:
                P.dve(lambda h=h: nc.vector.tensor_copy(V[:, tt, h, 0:64], pp[:, h * 64:(h + 1) * 64]),
                      r=[ppr], w=[("V", tt)])
            else:
                P.act(lambda h=h: nc.scalar.copy(V[:, tt, h, 0:64], pp[:, h * 64:(h + 1) * 64]),
                      r=[ppr], w=[("V", tt)])

    def mixer_d(self, l):
        nc, P = self.nc, self.P
        with ExitStack() as st:
            QT = [self.sb(st, "dQT", [128, T], BF16) for _ in range(2)]
            KTt = [self.sb(st, "dKT", [128, T], BF16) for _ in range(4)]
            for h in range(4):
                P.dve(lambda h=h: nc.vector.memset(KTt[h][:], 0.0), w=[("KT", h)])
            V = self.sb(st, "dV", [128, NT, 4, 66], BF16)
            bias = self.sb(st, "dbias", [128, 5, 512], BF16)
            P.dve(lambda: nc.vector.memset(V[:], 1.0), w=[("V", tt) for tt in range(NT)])
            for dd in range(5):
                P.dma("pool", lambda dd=dd: nc.gpsimd.dma_start(out=bias[:, dd, :], in_=self.bias_d[l, dd]),
                      w=[("dbias", dd)])
            with ExitStack() as s2:
                w = self.load_w(s2, "wd", self.w_d[l], 768)
                wres = [("wd", k) for k in range(KT)]
                load = self.hn_blocks(s2)
                pp = [self.ps(s2, "pp", [128, 512]) for _ in range(4)]
                pi = 0
                for tb in range(T // 512):
                    hb, hres = load(tb)
                    sl = slice(tb * 512, (tb + 1) * 512)
                    for i in range(2):
                        p = pi % 4; pi += 1
                        self.proj_fm(pp[p], ("pp", p), w, wres, i * 128, 128, hb, hres)
                        P.act(lambda p=p, i=i, sl=sl: nc.scalar.mul(QT[i][:, sl], pp[p][:], 0.125),
                              r=[("pp", p)], w=[("QT", i, tb)])
                        p = pi % 4; pi += 1
                        self.proj_fm(pp[p], ("pp", p), w, wres, 256 + i * 128, 128, hb, hres)
                        P.dve(lambda p=p, i=i, sl=sl: nc.vector.tensor_copy(KTt[2 * i][0:64, sl], pp[p][0:64, :]),
                              r=[("pp", p)], w=[("KT", 2 * i)])
                        P.dve(lambda p=p, i=i, sl=sl: nc.vector.tensor_copy(KTt[2 * i + 1][64:128, sl], pp[p][64:128, :]),
                              r=[("pp", p)], w=[("KT", 2 * i + 1)])
                    for j in range(4):
                        if self.dbg2 == "noV":
                            continue
                        p = pi % 4; pi += 1
                        self.proj_tm(pp[p], ("pp", p), w, wres, 512, 256, hb, hres, j)
                        if self.dbg2 == "noVevac":
                            continue
                        self.v_evac(V, pp[p], ("pp", p), tb * 4 + j, j)
                P.flush()
            if self.dbg == "proj":
                return
            with ExitStack() as s3:
                out_fn = self.std_out(s3, "d", 768)

                def kts_fn(qt):
                    return list(range(max(0, qt - 4), qt + 1))

                def pre_fn(g, qt, kt):
                    if self.dbg2 == "nopre":
                        return []
                    return [(self.ident_b[:], bias[:, qt - kt, :], [])]

                def qk_fn(g, j, qt, kt):
                    return (KTt[j][:, kt * 128:(kt + 1) * 128],
                            QT[j // 2][:, qt * 128:(qt + 1) * 128])

                def v_fn(g, j, kt):
                    return V[:, kt, j, 0:65]
                self.attn_core(s3, "d", 1, kts_fn, pre_fn, qk_fn, v_fn, out_fn)
                P.flush()

    def rsqrt_pool(self, out_ap, in_ap, nh_ap, r, w):
        nc, P = self.nc, self.P
        P.pool(lambda: nc.gpsimd.tensor_tensor(out_ap, in_ap, nh_ap, ALU.pow), r=r, w=w)

    def mixer_b(self, l):
        nc, P = self.nc, self.P
        scale = 96.0 ** -0.5
        with ExitStack() as st:
            QTh = [self.sb(st, "bQT", [128, T], BF16) for _ in range(4)]
            KTh = [self.sb(st, "bKT", [128, T], BF16) for _ in range(4)]
            V = self.sb(st, "bV", [128, NT, 4, 66], BF16)
            dmask = self.sb(st, "bdm", [128, 512], BF16)
            P.dve(lambda: nc.vector.memset(V[:], 1.0), w=[("V", tt) for tt in range(NT)])
            for h in range(4):
                P.dve(lambda h=h: nc.vector.memset(QTh[h][:], 0.0), w=[("QT", h), ("QTr", h)])
                P.dve(lambda h=h: nc.vector.memset(KTh[h][:], 0.0), w=[("KT", h), ("KTr", h)])
            P.dma("pool", lambda: nc.gpsimd.dma_start(out=dmask[:], in_=self.diagmask), w=["bdm"])
            with ExitStack() as s2:
                w = self.load_w(s2, "wb", self.w_b[l], 576)
                wres = [("wb", k) for k in range(KT)]
                wuq = self.load_w(s2, "wuq", self.w_uq[l], 384, nk=2)
                wuqr = self.load_w(s2, "wuqr", self.w_uqr[l], 384, nk=2)
                wkk = self.load_w(s2, "wkk", self.w_ukvk[l], 256, nk=1)
                wkv = self.load_w(s2, "wkv", self.w_ukvv[l], 256, nk=1)
                wu_res = [("wuq", 0), ("wuq", 1), ("wuqr", 0), ("wuqr", 1), ("wkk", 0), ("wkv", 0)]
                gq = self.sb(s2, "bgq", [128, 3], F32)
                nh = self.sb(s2, "bnh", [128, 512], F32)
                P.dma("sp", lambda: nc.sync.dma_start(out=gq[:, 0:2], in_=self.gq[l]), w=["bgq"])
                P.dma("sp", lambda: nc.sync.dma_start(out=gq[:, 2:3], in_=self.gkv[l]), w=["bgq"])
                P.dve(lambda: nc.vector.memset(nh[:], -0.5), w=["bnh"])
                load = self.hn_blocks(s2)
                rope = [self.sb(s2, "brope", [128, 2, 512], F32) for _ in range(2)]
                qd = [self.sb(s2, "bqd", [128, 3, 512], F32) for _ in range(2)]
                sq = [self.sb(s2, "bsq", [128, 3, 512], BF16) for _ in range(2)]
                ms = [self.sb(s2, "bms", [128, 2, 512], F32) for _ in range(2)]
                qn = [self.sb(s2, "bqn", [128, 3, 512], BF16) for _ in range(2)]
                t1 = [self.sb(s2, "bt1", [128, 512], F32) for _ in range(2)]
                t2 = [self.sb(s2, "bt2", [128, 512], F32) for _ in range(2)]
                hbs = {}
                pp = [self.ps(s2, "pp", [128, 512]) for _ in range(6)]
                pi = 0
                ri = 0
                pc = [0]
                rc = [0]
                def stage1(tb):
                    b = tb % 2
                    hb, hres = load(tb)
                    sl = slice(tb * 512, (tb + 1) * 512)
                    P.dma("sp", lambda b=b, sl=sl: nc.sync.dma_start(out=rope[b][64:96, :, :], in_=self.rope[64:96, :, sl]),
                          w=[("brope", b)])
                    for i in range(3):
                        p = pc[0] % 6; pc[0] += 1
                        self.proj_fm(pp[p], ("pp", p), w, wres, i * 128, 128, hb, hres)
                        P.act(lambda p=p, i=i, b=b: nc.scalar.copy(qd[b][:, i, :], pp[p][:]),
                              r=[("pp", p)], w=[("bqd", b, i)])
                    P.act(lambda b=b: nc.scalar.activation(out=sq[b][:], in_=qd[b][:], func=AF.Square),
                          r=[("bqd", b, i) for i in range(3)], w=[("bsq", b)])
                    p = pc[0] % 6; pc[0] += 1
                    for i in range(2):
                        P.pe(lambda p=p, i=i, b=b: nc.tensor.matmul(pp[p][:], self.ones_b[:], sq[b][:, i, :],
                                                                   start=(i == 0), stop=(i == 1)),
                             r=[("bsq", b), "onesb"], w=[("pp", p)])
                    P.dve(lambda p=p, b=b: nc.vector.tensor_scalar(ms[b][:, 0, :], pp[p][:], 1.0 / 256, EPS, ALU.mult, ALU.add),
                          r=[("pp", p)], w=[("bms", b, 0)])
                    p = pc[0] % 6; pc[0] += 1
                    P.pe(lambda p=p, b=b: nc.tensor.matmul(pp[p][:], self.ones_b[:], sq[b][:, 2, :], start=True, stop=True),
                         r=[("bsq", b), "onesb"], w=[("pp", p)])
                    P.dve(lambda p=p, b=b: nc.vector.tensor_scalar(ms[b][:, 1, :], pp[p][:], 1.0 / 128, EPS, ALU.mult, ALU.add),
                          r=[("pp", p)], w=[("bms", b, 1)])
                    for i in range(2):
                        P.act(lambda b=b, i=i: nc.scalar.activation(out=ms[b][:, i, :], in_=ms[b][:, i, :], func=AF.Sqrt),
                              r=[("bms", b, i)], w=[("bms", b, i)])
                        P.dve(lambda b=b, i=i: nc.vector.reciprocal(ms[b][:, i, :], ms[b][:, i, :]),
                              r=[("bms", b, i)], w=[("bms", b, i)])
                    for i in range(3):
                        P.dve(lambda b=b, i=i: nc.vector.scalar_tensor_tensor(
                            qn[b][:, i, :], qd[b][:, i, :], gq[:, i:i + 1], ms[b][:, 0 if i < 2 else 1, :],
                            ALU.mult, ALU.mult),
                            r=[("bqd", b, i), ("bms", b, 0 if i < 2 else 1), "bgq"], w=[("bqn", b, i)])
                    p1 = pc[0] % 6; pc[0] += 1
                    self.proj_fm(pp[p1], ("pp", p1), w, wres, 384, 96, hb, hres)
                    p2 = pc[0] % 6; pc[0] += 1
                    self.proj_fm(pp[p2], ("pp", p2), w, wres, 480, 96, hb, hres)
                    r = rc[0] % 2; rc[0] += 1
                    P.dve(lambda p1=p1, b=b, r=r: nc.vector.tensor_tensor(
                        t1[r][64:96, :], pp[p1][64:96, :], rope[b][64:96, 0, :], ALU.mult),
                        r=[("pp", p1), ("brope", b)], w=[("bt1", r)])
                    P.dve(lambda p2=p2, b=b, r=r: nc.vector.tensor_tensor(
                        t2[r][64:96, :], pp[p2][64:96, :], rope[b][64:96, 1, :], ALU.mult),
                        r=[("pp", p2), ("brope", b)], w=[("bt2", r)])
                    for h in range(4):
                        P.dve(lambda h=h, r=r, sl=sl: nc.vector.tensor_tensor(
                            KTh[h][64:96, sl], t1[r][64:96, :], t2[r][64:96, :], ALU.add),
                            r=[("bt1", r), ("bt2", r)], w=[("KTr", h)])

                def stage2(tb):
                    b = tb % 2
                    sl = slice(tb * 512, (tb + 1) * 512)
                    for h in range(4):
                        p1 = pc[0] % 6; pc[0] += 1
                        for i in range(2):
                            P.pe(lambda p1=p1, i=i, h=h, b=b: nc.tensor.matmul(
                                pp[p1][0:96, :], wuq[:, i, h * 96:(h + 1) * 96], qn[b][:, i, :],
                                start=(i == 0), stop=(i == 1)),
                                r=[("bqn", b, i)] + wu_res, w=[("pp", p1)])
                        p2 = pc[0] % 6; pc[0] += 1
                        for i in range(2):
                            P.pe(lambda p2=p2, i=i, h=h, b=b: nc.tensor.matmul(
                                pp[p2][0:96, :], wuqr[:, i, h * 96:(h + 1) * 96], qn[b][:, i, :],
                                start=(i == 0), stop=(i == 1)),
                                r=[("bqn", b, i)] + wu_res, w=[("pp", p2)])
                        P.act(lambda p1=p1, h=h, sl=sl: nc.scalar.mul(QTh[h][0:64, sl], pp[p1][0:64, :], scale),
                              r=[("pp", p1)], w=[("QT", h)])
                        r = rc[0] % 2; rc[0] += 1
                        P.dve(lambda p1=p1, b=b, r=r: nc.vector.scalar_tensor_tensor(
                            t1[r][64:96, :], pp[p1][64:96, :], scale, rope[b][64:96, 0, :], ALU.mult, ALU.mult),
                            r=[("pp", p1), ("brope", b)], w=[("bt1", r)])
                        P.dve(lambda p2=p2, b=b, r=r: nc.vector.scalar_tensor_tensor(
                            t2[r][64:96, :], pp[p2][64:96, :], scale, rope[b][64:96, 1, :], ALU.mult, ALU.mult),
                            r=[("pp", p2), ("brope", b)], w=[("bt2", r)])
                        P.dve(lambda h=h, r=r, sl=sl: nc.vector.tensor_tensor(
                            QTh[h][64:96, sl], t1[r][64:96, :], t2[r][64:96, :], ALU.add),
                            r=[("bt1", r), ("bt2", r)], w=[("QTr", h)])
                        p3 = pc[0] % 6; pc[0] += 1
                        P.pe(lambda p3=p3, h=h, b=b: nc.tensor.matmul(
                            pp[p3][0:64, :], wkk[:, 0, h * 64:(h + 1) * 64], qn[b][:, 2, :], start=True, stop=True),
                            r=[("bqn", b, 2)] + wu_res, w=[("pp", p3)])
                        P.dve(lambda p3=p3, h=h, sl=sl: nc.vector.tensor_copy(KTh[h][0:64, sl], pp[p3][0:64, :]),
                              r=[("pp", p3)], w=[("KT", h)])
                    for j in range(4):
                        p = pc[0] % 6; pc[0] += 1
                        P.pe(lambda p=p, j=j, b=b: nc.tensor.matmul(
                            pp[p][:, 0:256], qn[b][:, 2, j * 128:(j + 1) * 128], wkv[:, 0, :], start=True, stop=True),
                            r=[("bqn", b, 2)] + wu_res, w=[("pp", p)])
                        self.v_evac(V, pp[p], ("pp", p), tb * 4 + j, j)

                stage1(0)
                for tb in range(T // 512):
                    if tb + 1 < T // 512:
                        stage1(tb + 1)
                    stage2(tb)
                P.flush()
            with ExitStack() as s3:
                out_fn = self.std_out(s3, "b", 256)

                def kts_fn(qt):
                    return list(range(qt + 1))

                def pre_fn(g, qt, kt):
                    if kt == qt:
                        return [(self.ident_b[:], dmask[:], [])]
                    return []

                def qk_fn(g, j, qt, kt):
                    return (KTh[j][:, kt * 128:(kt + 1) * 128], QTh[j][:, qt * 128:(qt + 1) * 128])

                def v_fn(g, j, kt):
                    return V[:, kt, j, 0:65]
                self.attn_core(s3, "b", 1, kts_fn, pre_fn, qk_fn, v_fn, out_fn)
                P.flush()

    def mixer_a(self, l):
        nc, P = self.nc, self.P
        scale = 32.0 ** -0.5
        lam_init = 0.8 - 0.6 * math.exp(-0.3 * l)
        with ExitStack() as st:
            QTa = [self.sb(st, "aQT", [128, T], BF16) for _ in range(4)]
            KTa = [[self.sb(st, "aKT", [128, T], BF16) for _ in range(2)] for _ in range(4)]
            V = self.sb(st, "aV", [128, NT, 4, 66], BF16)
            corr = self.sb(st, "acorr", [128, 2, 512], BF16)
            neglam = self.sb(st, "aneglam", [128, 1], F32)
            gAb = self.sb(st, "agAb", [128, 64], F32)
            nh = self.sb(st, "anh", [128, 2], F32)
            P.dve(lambda: nc.vector.memset(V[:], 1.0), w=[("V", tt) for tt in range(NT)])
            P.dve(lambda: nc.vector.memset(nh[:], -0.5), w=["anh"])
            for h in range(4):
                P.dve(lambda h=h: nc.vector.memset(QTa[h][:], 0.0), w=[("QT", h)])
                for c in range(2):
                    P.dve(lambda h=h, c=c: nc.vector.memset(KTa[h][c][:], 0.0), w=[("KT", h, c)])
            for g in range(2):
                P.dma("pool", lambda g=g: nc.gpsimd.dma_start(out=corr[:, g, :], in_=self.corr_a[g]), w=[("acorr", g)])
            for h in range(4):
                P.dma("pool", lambda h=h: nc.gpsimd.dma_start(out=QTa[h][64:67, :], in_=self.augq[h]),
                      r=[("QT", h)], w=[("QTaug", h)])
                for c in range(2):
                    P.dma("pool", lambda h=h, c=c: nc.gpsimd.dma_start(out=KTa[h][c][64:67, :], in_=self.augk[h]),
                          r=[("KT", h, c)], w=[("KTaug", h, c)])
            with ExitStack() as s2:
                lv = self.sb(s2, "alv", [128, 128], F32)
                pr = self.sb(s2, "apr", [128, 64], F32)
                s12 = self.sb(s2, "as12", [128, 2], F32)
                P.dma("sp", lambda: nc.sync.dma_start(out=lv[:], in_=self.dlam[l]), w=["alv"])
                P.dma("sp", lambda: nc.sync.dma_start(out=gAb[:], in_=self.dng[l]), w=["agAb"])
                P.dve(lambda: nc.vector.tensor_tensor(pr[:, 0:32], lv[:, 0:32], lv[:, 32:64], ALU.mult), r=["alv"], w=["apr0"])
                P.dve(lambda: nc.vector.tensor_tensor(pr[:, 32:64], lv[:, 64:96], lv[:, 96:128], ALU.mult), r=["alv"], w=["apr1"])
                P.dve(lambda: nc.vector.reduce_sum(out=s12[:], in_=pr[:, :].rearrange("p (a b) -> p a b", a=2), axis=AX.X),
                      r=["apr0", "apr1"], w=["as12"])
                P.act(lambda: nc.scalar.activation(out=s12[:], in_=s12[:], func=AF.Exp), r=["as12"], w=["as12"])
                P.dve(lambda: nc.vector.tensor_tensor(neglam[:], s12[:, 1:2], s12[:, 0:1], ALU.subtract), r=["as12"], w=["aneglam"])
                P.dve(lambda: nc.vector.tensor_scalar(neglam[:], neglam[:], -lam_init, None, ALU.add), r=["aneglam"], w=["aneglam"])
                P.dve(lambda: nc.vector.tensor_scalar(gAb[:], gAb[:], 1.0 - lam_init, None, ALU.mult), r=["agAb"], w=["agAb"])
                w = self.load_w(s2, "wa", self.w_a[l], 768)
                wres = [("wa", k) for k in range(KT)]
                load = self.hn_blocks(s2)
                pp = [self.ps(s2, "pp", [128, 512]) for _ in range(4)]
                pi = 0
                for tb in range(T // 512):
                    hb, hres = load(tb)
                    sl = slice(tb * 512, (tb + 1) * 512)
                    for h in range(4):
                        p = pi % 4; pi += 1
                        self.proj_fm(pp[p], ("pp", p), w, wres, h * 64, 64, hb, hres)
                        P.act(lambda p=p, h=h, sl=sl: nc.scalar.mul(QTa[h][0:64, sl], pp[p][0:64, :], scale),
                              r=[("pp", p)], w=[("QT", h)])
                        p = pi % 4; pi += 1
                        self.proj_fm(pp[p], ("pp", p), w, wres, 256 + h * 64, 64, hb, hres)
                        P.dve(lambda p=p, h=h, sl=sl: nc.vector.tensor_copy(KTa[h][0][0:32, sl], pp[p][0:32, :]),
                              r=[("pp", p)], w=[("KT", h, 0)])
                        P.dve(lambda p=p, h=h, sl=sl: nc.vector.tensor_copy(KTa[h][1][32:64, sl], pp[p][32:64, :]),
                              r=[("pp", p)], w=[("KT", h, 1)])
                    for j in range(4):
                        p = pi % 4; pi += 1
                        self.proj_tm(pp[p], ("pp", p), w, wres, 512, 256, hb, hres, j)
                        self.v_evac(V, pp[p], ("pp", p), tb * 4 + j, j)
                P.flush()
            with ExitStack() as s3:
                onA = [self.sb(s3, "aon", [128, 4, 64], F32) for _ in range(2)]
                cmb = [self.sb(s3, "acmb", [128, 2, 64], F32) for _ in range(2)]
                sqt = [self.sb(s3, "asq", [128, 2, 64], F32) for _ in range(2)]
                ss = [self.sb(s3, "ass", [128, 2], F32) for _ in range(2)]
                onb = [self.sb(s3, "aonb", [128, 128], BF16) for _ in range(2)]
                tp = [self.ps(s3, "atp", [128, 128], BF16) for _ in range(2)]
                stage = [self.sb(s3, "astg", [128, 2, 512], BF16) for _ in range(2)]
                oTv = self.oT[0:256, :].rearrange("(i p) t -> p i t", p=128)
                cnt = [0]

                def out_fn(g, qt, O, Or, rl, rlr):
                    nb = cnt[0] % 2
                    cnt[0] += 1
                    sbi = (qt // 4) % 2
                    q4 = qt % 4
                    for j in range(4):
                        P.dve(lambda j=j: nc.vector.tensor_scalar(
                            onA[nb][:, j, :], O[:, j * 66:j * 66 + 64], rl[:, j:j + 1], None, ALU.mult),
                            r=[Or, rlr], w=[("aon", nb, j)])
                    for hh in range(2):
                        P.dve(lambda hh=hh: nc.vector.scalar_tensor_tensor(
                            cmb[nb][:, hh, :], onA[nb][:, 2 * hh + 1, :], neglam[:, 0:1], onA[nb][:, 2 * hh, :],
                            ALU.mult, ALU.add),
                            r=[("aon", nb, 2 * hh), ("aon", nb, 2 * hh + 1)], w=[("acmb", nb, hh)])
                    P.dve(lambda: nc.vector.tensor_tensor(sqt[nb][:], cmb[nb][:], cmb[nb][:], ALU.mult),
                          r=[("acmb", nb, 0), ("acmb", nb, 1)], w=[("asq", nb)])
                    P.dve(lambda: nc.vector.reduce_sum(out=ss[nb][:], in_=sqt[nb][:], axis=AX.X),
                          r=[("asq", nb)], w=[("ass", nb)])
                    P.dve(lambda: nc.vector.tensor_scalar(ss[nb][:], ss[nb][:], 1.0 / 64, EPS, ALU.mult, ALU.add),
                          r=[("ass", nb)], w=[("ass", nb)])
                    self.rsqrt_pool(ss[nb][:], ss[nb][:], nh[:], [("ass", nb)], [("ass", nb)])
                    for hh in range(2):
                        P.dve(lambda hh=hh: nc.vector.scalar_tensor_tensor(
                            onb[nb][:, hh * 64:(hh + 1) * 64], cmb[nb][:, hh, :], ss[nb][:, hh:hh + 1], gAb[:],
                            ALU.mult, ALU.mult),
                            r=[("acmb", nb, hh), ("ass", nb)], w=[("aonb", nb, hh)])
                    P.pe(lambda: nc.tensor.transpose(tp[nb][:], onb[nb][:], self.ident_b[:]),
                         r=[("aonb", nb, 0), ("aonb", nb, 1)], w=[("atp", nb)])
                    P.dve(lambda: nc.vector.tensor_copy(stage[sbi][:, g, q4 * 128:(q4 + 1) * 128], tp[nb][:]),
                          r=[("atp", nb)], w=[("astg", sbi, g)])
                    if q4 == 3 and g == 1:
                        tb = qt // 4
                        P.dma("sp", lambda: nc.sync.dma_start(out=oTv[:, :, tb * 512:(tb + 1) * 512], in_=stage[sbi][:]),
                              r=[("astg", sbi, 0), ("astg", sbi, 1)], w=[("dram", "oT", "a", tb)])

                def kts_fn(qt):
                    return list(range(qt + 1))

                def pre_fn(g, qt, kt):
                    if kt == qt:
                        return [(self.ident_b[:], corr[:, g, :], [])]
                    return []

                def qk_fn(g, j, qt, kt):
                    h = 2 * g + j // 2
                    return (KTa[h][j % 2][:, kt * 128:(kt + 1) * 128], QTa[h][:, qt * 128:(qt + 1) * 128])

                def v_fn(g, j, kt):
                    return V[:, kt, 2 * g + j // 2, 0:65]
                self.attn_core(s3, "a", 2, kts_fn, pre_fn, qk_fn, v_fn, out_fn)
                P.flush()

    def mixer_c(self, l):
        nc, P = self.nc, self.P
        NR = int(os.environ.get("MK_NR", "14"))
        with ExitStack() as st:
            QTc = [self.sb(st, "cQT", [128, T], BF16) for _ in range(4)]
            KTc = [self.sb(st, "cKT", [128, T], BF16) for _ in range(4)]
            V = self.sb(st, "cV", [128, NT, 4, 66], BF16)
            qiT = [self.sb(st, "cqi", [128, T], BF16) for _ in range(2)]
            kiT = self.sb(st, "cki", [128, T], BF16)
            wsb = self.sb(st, "cw", [128, NT, 8], F32)
            corr = self.sb(st, "ccorr", [128, 512], BF16)
            id4 = self.sb(st, "cid4", [128, 512], BF16)
            P.dve(lambda: nc.vector.memset(V[:], 1.0), w=[("V", tt) for tt in range(NT)])
            for h in range(4):
                P.dve(lambda h=h: nc.vector.memset(QTc[h][:], 0.0), w=[("QT", h)])
                P.dve(lambda h=h: nc.vector.memset(KTc[h][:], 0.0), w=[("KT", h)])
            P.dma("pool", lambda: nc.gpsimd.dma_start(out=corr[:], in_=self.corr_c), w=["ccorr"])
            P.dma("pool", lambda: nc.gpsimd.dma_start(out=id4[:], in_=self.ident4), w=["cid4"])
            for h in range(4):
                P.dma("pool", lambda h=h: nc.gpsimd.dma_start(out=QTc[h][64:67, :], in_=self.augq[h]),
                      r=[("QT", h)], w=[("QTaug", h)])
                P.dma("pool", lambda h=h: nc.gpsimd.dma_start(out=KTc[h][64:67, :], in_=self.augk[h]),
                      r=[("KT", h)], w=[("KTaug", h)])
            with ExitStack() as s2:
                w = self.load_w(s2, "wc", self.w_c[l], 1160)
                wres = [("wc", k) for k in range(KT)]
                load = self.hn_blocks(s2)
                pp = [self.ps(s2, "pp", [128, 512]) for _ in range(4)]
                pi = 0
                for tb in range(T // 512):
                    hb, hres = load(tb)
                    sl = slice(tb * 512, (tb + 1) * 512)
                    for h in range(4):
                        p = pi % 4; pi += 1
                        self.proj_fm(pp[p], ("pp", p), w, wres, h * 64, 64, hb, hres)
                        P.act(lambda p=p, h=h, sl=sl: nc.scalar.mul(QTc[h][0:64, sl], pp[p][0:64, :], 0.125),
                              r=[("pp", p)], w=[("QT", h)])
                        p = pi % 4; pi += 1
                        self.proj_fm(pp[p], ("pp", p), w, wres, 256 + h * 64, 64, hb, hres)
                        P.dve(lambda p=p, h=h, sl=sl: nc.vector.tensor_copy(KTc[h][0:64, sl], pp[p][0:64, :]),
                              r=[("pp", p)], w=[("KT", h)])
                    for i in range(2):
                        p = pi % 4; pi += 1
                        self.proj_fm(pp[p], ("pp", p), w, wres, 768 + i * 128, 128, hb, hres)
                        P.act(lambda p=p, i=i, sl=sl: nc.scalar.copy(qiT[i][:, sl], pp[p][:]),
                              r=[("pp", p)], w=[("cqi", i)])
                    p = pi % 4; pi += 1
                    self.proj_fm(pp[p], ("pp", p), w, wres, 1024, 128, hb, hres)
                    P.dve(lambda p=p, sl=sl: nc.vector.tensor_copy(kiT[:, sl], pp[p][:]), r=[("pp", p)], w=["cki"])
                    for j in range(4):
                        p = pi % 4; pi += 1
                        self.proj_tm(pp[p], ("pp", p), w, wres, 512, 256, hb, hres, j)
                        self.v_evac(V, pp[p], ("pp", p), tb * 4 + j, j)
                        p = pi % 4; pi += 1
                        self.proj_tm(pp[p], ("pp", p), w, wres, 1152, 8, hb, hres, j)
                        P.dve(lambda p=p, tt=tb * 4 + j: nc.vector.tensor_copy(wsb[:, tt, :], pp[p][:, 0:8]),
                              r=[("pp", p)], w=["cw"])
                P.flush()
            with ExitStack() as s3:
                out_fn = self.std_out(s3, "c", 512, ntp=1)
                scores2 = [self.sb(s3, "cscore", [128, T], F32) for _ in range(2)]
                junk = self.sb(s3, "cjunk", [128, T], BF16)
                Mb = [self.sb(s3, "cMb", [128, T], BF16) for _ in range(2)]
                Dg = [[self.sb(s3, "cDg", [128, 128], BF16) for _ in range(8)] for _ in range(2)]
                Qb = [[self.sb(s3, "cQb", [128, 128], BF16) for _ in range(8)] for _ in range(2)]
                R = [self.sb(s3, "cR", [128, 512], BF16) for _ in range(2)]
                X = [self.ps(s3, "cX", [128, 512]) for _ in range(2)]
                SC = self.ps(s3, "cSC", [128, 512])
                st4 = self.sb(s3, "cst", [128, 4 * (NR + 2)], F32)
                thrc = self.sb(s3, "cthrc", [128, 1], F32)
                steps = self.sb(s3, "csteps", [128, NR + 1], F32)
                cpow = self.sb(s3, "ccpow", [128, NR + 1], F32)
                for r in range(NR + 1):
                    P.pool(lambda r=r: nc.gpsimd.memset(cpow[:, r:r + 1], 2.0 ** -(r + 1)), w=["ccpow"])
                P.dve(lambda: nc.vector.memset(thrc[:], -1.0e30), w=["cthrc"])
                for b in range(2):
                    for ih in range(8):
                        P.pool(lambda b=b, ih=ih: nc.gpsimd.memset(Qb[b][ih][:], 0.0), w=[("cQb", b, ih)])
                xi = [0]
                LO, MID, CNT, STP = 0, NR + 2, 2 * (NR + 2), 3 * (NR + 2)

                def before_qt(qt):
                    if qt == 0:
                        do_scores(0)
                        do_scores(1)
                        do_select(0)
                    if qt + 2 < NT:
                        do_scores(qt + 2)
                    if qt + 1 < NT:
                        do_select(qt + 1)

                def do_scores(qt):
                    n = 128 * (qt + 1)
                    b = qt % 2
                    score = scores2[b]
                    qs = slice(qt * 128, (qt + 1) * 128)
                    for ih in range(8):
                        r0 = 32 * (ih % 4)
                        P.pool(lambda ih=ih, r0=r0: nc.gpsimd.tensor_copy(
                            Qb[b][ih][r0:r0 + 32, :], qiT[ih // 4][r0:r0 + 32, qs]), w=[("cQb", b, ih)])
                        P.pool(lambda ih=ih: nc.gpsimd.tensor_scalar(
                            Dg[b][ih][:], self.ident_b[:], wsb[:, qt, ih:ih + 1], None, ALU.mult),
                            w=[("cDg", b, ih)])
                    nkb = (n + 511) // 512
                    for kb in range(nkb):
                        wd = min(512, n - 512 * kb)
                        ks = slice(512 * kb, 512 * kb + wd)
                        xs = []
                        for ih in range(8):
                            xs.append(xi[0] % 2)
                            xi[0] += 1

                        def emit_x(ih, ks=ks, wd=wd):
                            x = xs[ih]
                            P.pe(lambda x=x, ih=ih, ks=ks, wd=wd: nc.tensor.matmul(
                                X[x][:, 0:wd], Qb[b][ih][:], kiT[:, ks], start=True, stop=True),
                                r=[("cQb", b, ih)], w=[("cX", x)])
                        emit_x(0)
                        for ih in range(8):
                            x = xs[ih]
                            P.act(lambda x=x, wd=wd: nc.scalar.activation(out=R[x][:, 0:wd], in_=X[x][:, 0:wd], func=AF.Relu),
                                  r=[("cX", x)], w=[("cR", x)])
                            if ih + 1 < 8:
                                emit_x(ih + 1)
                            P.pe(lambda ih=ih, x=x, wd=wd: nc.tensor.matmul(
                                SC[:, 0:wd], Dg[b][ih][:], R[x][:, 0:wd], start=(ih == 0), stop=(ih == 7)),
                                r=[("cR", x), ("cDg", b, ih)], w=["cSC"])
                        P.act(lambda ks=ks, wd=wd: nc.scalar.copy(score[:, ks], SC[:, 0:wd]),
                              r=["cSC"], w=[("cscore", b)])

                def do_select(qt):
                    n = 128 * (qt + 1)
                    b = qt % 2
                    score = scores2[b]
                    P.dve(lambda: nc.vector.memset(score[0:64, n - 64:n], -3.0e38), r=[("cscore", b)], w=[("cscore", b)])
                    if qt >= 2:
                        W0 = CNT + NR + 1
                        MX = MID
                        P.dve(lambda: nc.vector.tensor_reduce(out=st4[:, MX:MX + 1], in_=score[:, 0:n], axis=AX.X, op=ALU.max),
                              r=[("cscore", b)], w=["cmx"])
                        P.dve(lambda: nc.vector.tensor_reduce(out=st4[:, STP:STP + 1], in_=score[:, 0:n - 64], axis=AX.X, op=ALU.min),
                              r=[("cscore", b)], w=["cmn"])
                        P.dve(lambda: nc.vector.tensor_tensor(st4[:, W0:W0 + 1], st4[:, MX:MX + 1], st4[:, STP:STP + 1], ALU.subtract),
                              r=["cmx", "cmn"], w=["cw0"])
                        P.dve(lambda: nc.vector.tensor_scalar(steps[:], cpow[:], st4[:, W0:W0 + 1], None, ALU.mult),
                              r=["cw0", "ccpow"], w=["csteps"])
                        P.dve(lambda: nc.vector.tensor_tensor(st4[:, LO:LO + 1], st4[:, STP:STP + 1], steps[:, 0:1], ALU.add),
                              r=["cmn", "csteps"], w=[("clo", 0)])
                        for r in range(NR):
                            P.dve(lambda r=r: nc.vector.tensor_scalar(
                                junk[:, 0:n], score[:, 0:n], st4[:, LO + r:LO + r + 1], 0.0, ALU.is_gt, ALU.add,
                                accum_out=st4[:, CNT + r:CNT + r + 1]),
                                r=[("cscore", b), ("clo", r)], w=[("ccnt", r), "cjunk"])
                            P.dve(lambda r=r: nc.vector.tensor_scalar(
                                st4[:, STP + 1 + r:STP + 2 + r], st4[:, CNT + r:CNT + r + 1], 255.5, 0.5, ALU.is_gt, ALU.subtract),
                                r=[("ccnt", r)], w=[("cstp", r)])
                            P.dve(lambda r=r: nc.vector.scalar_tensor_tensor(
                                st4[:, LO + r + 1:LO + r + 2], st4[:, STP + 1 + r:STP + 2 + r], steps[:, r:r + 1],
                                st4[:, LO + r:LO + r + 1], ALU.mult, ALU.add),
                                r=[("cstp", r), ("clo", r), "csteps"], w=[("clo", r + 1)])
                        P.dve(lambda: nc.vector.tensor_tensor(st4[:, MX:MX + 1], st4[:, LO + NR:LO + NR + 1], steps[:, NR:NR + 1], ALU.subtract),
                              r=[("clo", NR), "csteps"], w=["cthr"])
                        thr = st4[:, MX:MX + 1]
                        thr_r = ["cthr"]
                    else:
                        thr = thrc[:, 0:1]
                        thr_r = ["cthrc"]
                    P.dve(lambda: nc.vector.tensor_scalar(Mb[b][:, 0:n], score[:, 0:n], thr, NEG, ALU.is_le, ALU.mult),
                          r=[("cscore", b)] + thr_r, w=[("cMb", b)])

                def kts_fn(qt):
                    return list(range(qt + 1))

                def pre_fn(g, qt, kt):
                    b = qt % 2
                    pre = [(Mb[b][:, kt * 128:(kt + 1) * 128], id4[:], [("cMb", b)])]
                    if kt == qt:
                        pre.append((self.ident_b[:], corr[:], []))
                    return pre

                def qk_fn(g, j, qt, kt):
                    return (KTc[j][:, kt * 128:(kt + 1) * 128], QTc[j][:, qt * 128:(qt + 1) * 128])

                def v_fn(g, j, kt):
                    return V[:, kt, j, 0:65]
                self.attn_core(s3, "c", 1, kts_fn, pre_fn, qk_fn, v_fn, out_fn, before_qt=before_qt, ns=2)
                P.flush()

    def phase_wout(self, l):
        nc, P = self.nc, self.P
        with ExitStack() as st:
            wo = self.load_w(st, "wo", self.w_out[l], D)
            wres = [("wo", k) for k in range(KT)]
            ob = [self.sb(st, "wob", [128, KT, 512], BF16) for _ in range(2)]
            xb = [self.sb(st, "wxb", [128, KT, 512], F32) for _ in range(2)]
            py = [self.ps(st, "wpy", [128, 512]) for _ in range(2)]
            oTv = self.oT.rearrange("(kt p) t -> p kt t", p=128)
            xTv = self.xT.rearrange("(kt p) t -> p kt t", p=128)
            pi = 0
            for tb in range(T // 512):
                b = tb % 2
                sl = slice(tb * 512, (tb + 1) * 512)
                P.dma("sp", lambda b=b, sl=sl: nc.sync.dma_start(out=ob[b][:], in_=oTv[:, :, sl]), w=[("wob", b)])
                P.dma("sp", lambda b=b, sl=sl: nc.sync.dma_start(out=xb[b][:], in_=xTv[:, :, sl]),
                      r=[("dram", "xT", tb)], w=[("wxb", b, f) for f in range(KT)])
                for f in range(KT):
                    p = pi % 2; pi += 1
                    for k in range(KT):
                        P.pe(lambda b=b, f=f, k=k, p=p: nc.tensor.matmul(
                            py[p][:], wo[:, k, f * 128:(f + 1) * 128], ob[b][:, k, :],
                            start=(k == 0), stop=(k == KT - 1)),
                            r=[("wob", b)] + wres, w=[("wpy", p)])
                    P.dve(lambda b=b, f=f, p=p: nc.vector.scalar_tensor_tensor(
                        xb[b][:, f, :], py[p][:], self.modv[:, 16 + f:17 + f], xb[b][:, f, :], ALU.mult, ALU.add),
                        r=[("wpy", p), ("wxb", b, f)], w=[("wxb", b, f)])
                P.dma("sp", lambda b=b, sl=sl: nc.sync.dma_start(out=xTv[:, :, sl], in_=xb[b][:]),
                      r=[("wxb", b, f) for f in range(KT)], w=[("dram", "xT", tb)])
            P.flush()

    def phase_ffn(self, l, moe):
        nc, P = self.nc, self.P
        E = NEXP if moe else 1
        NJ = (D_FFE if moe else D_FF) // 128
        NH = 2
        JH = NJ // NH
        w13 = self.moe_w13 if moe else self.ffn_w13
        w2 = self.moe_w2 if moe else self.ffn_w2
        NB = 1024
        with ExitStack() as st:
            hb = self.sb(st, "fhb", [128, KT, NB], BF16)
            xb = self.sb(st, "fxb", [128, KT, NB], F32)
            g = self.sb(st, "fg", [128, JH, NB], BF16)
            w2e = [self.sb(st, "fw2", [128, JH, D], BF16) for _ in range(2)]
            wj = [self.sb(st, "fw13", [128, 2, KT, 128], BF16) for _ in range(6)]
            sl_t = [self.sb(st, "fsl", [128, 512], BF16) for _ in range(2)]
            pa = [self.ps(st, "fpa", [128, 512]) for _ in range(2)]
            pb = [self.ps(st, "fpb", [128, 512]) for _ in range(2)]
            pyy = [self.ps(st, "fpy", [128, 512]) for _ in range(2)]
            ytmp = [self.sb(st, "fyt", [128, 512], F32) for _ in range(2)]
            hTv = self.hnT.rearrange("(kt p) t -> p kt t", p=128)
            xTv = self.xT.rearrange("(kt p) t -> p kt t", p=128)
            if moe:
                cb = self.sb(st, "fcb", [128, E, NB], BF16)
                rt = self.sb(st, "frt", [128, KT, 8], BF16)
                lg = self.sb(st, "flg", [128, 8], F32)
                m8 = self.sb(st, "fm8", [128, 8], F32)
                sc4 = self.sb(st, "fsc4", [128, 8], F32)
                c1 = self.sb(st, "fc1", [128, 8], F32)
                c2 = self.sb(st, "fc2", [128, 8], F32)
                dg = [self.sb(st, "fdg", [128, 128], BF16) for _ in range(2)]
                plg = self.ps(st, "fplg", [128, 512])
                pcb = self.ps(st, "fpcb", [128, 512])
                for k in range(KT):
                    P.dma("pool", lambda k=k: nc.gpsimd.dma_start(out=rt[:, k, :], in_=self.router[k]), w=["frt"])
            wi = 0
            ai = 0
            yi = 0
            w2i = 0
            for tb in range(T // NB):
                sl = slice(tb * NB, (tb + 1) * NB)
                P.dma("sp", lambda sl=sl: nc.sync.dma_start(out=hb[:], in_=hTv[:, :, sl]), w=["fhb"])
                P.dma("sp", lambda sl=sl: nc.sync.dma_start(out=xb[:], in_=xTv[:, :, sl]),
                      w=[("fxb", f, s) for f in range(KT) for s in range(2)])
                if moe:
                    for tt in range(NB // 128):
                        for k in range(KT):
                            P.pe(lambda k=k, tt=tt: nc.tensor.matmul(
                                plg[:, 0:8], hb[:, k, tt * 128:(tt + 1) * 128], rt[:, k, :],
                                start=(k == 0), stop=(k == KT - 1)), r=["fhb", "frt"], w=["fplg"])
                        P.dve(lambda: nc.vector.tensor_copy(lg[:], plg[:, 0:8]), r=["fplg"], w=["flg"])
                        P.dve(lambda: nc.vector.max(out=m8[:], in_=lg[:]), r=["flg"], w=["fm8"])
                        P.dve(lambda: nc.vector.tensor_tensor(sc4[:, 0:1], m8[:, 1:2], m8[:, 0:1], ALU.subtract),
                              r=["fm8"], w=["fsc4a"])
                        P.act(lambda: nc.scalar.activation(out=sc4[:, 1:2], in_=sc4[:, 0:1], func=AF.Exp),
                              r=["fsc4a"], w=["fsc4b"])
                        P.dve(lambda: nc.vector.tensor_scalar(sc4[:, 2:3], sc4[:, 1:2], 1.0, None, ALU.add),
                              r=["fsc4b"], w=["fsc4c"])
                        P.dve(lambda: nc.vector.reciprocal(sc4[:, 3:4], sc4[:, 2:3]), r=["fsc4c"], w=["fsc4d"])
                        P.dve(lambda: nc.vector.tensor_tensor(sc4[:, 4:5], sc4[:, 1:2], sc4[:, 3:4], ALU.mult),
                              r=["fsc4b", "fsc4d"], w=["fsc4e"])
                        P.dve(lambda: nc.vector.tensor_scalar(c1[:], lg[:], m8[:, 0:1], sc4[:, 3:4], ALU.is_equal, ALU.mult),
                              r=["flg", "fm8", "fsc4d"], w=["fc1"])
                        P.dve(lambda: nc.vector.tensor_scalar(c2[:], lg[:], m8[:, 1:2], sc4[:, 4:5], ALU.is_equal, ALU.mult),
                              r=["flg", "fm8", "fsc4e"], w=["fc2"])
                        P.dve(lambda: nc.vector.tensor_tensor(c1[:], c1[:], c2[:], ALU.add),
                              r=["fc1", "fc2"], w=["fc1"])
                        for e in range(E):
                            d = e % 2
                            P.dve(lambda e=e, d=d: nc.vector.tensor_scalar(
                                dg[d][:], self.ident_b[:], c1[:, e:e + 1], None, ALU.mult),
                                r=["fc1", "identb"], w=[("fdg", d)])
                            P.pe(lambda e=e, d=d: nc.tensor.matmul(
                                pcb[:, (e % 4) * 128:(e % 4 + 1) * 128], self.ones_b[:], dg[d][:],
                                start=True, stop=True, skip_group_check=True),
                                r=[("fdg", d), "onesb"], w=["fpcb"])
                            if e % 4 == 3:
                                for q in range(4):
                                    ee = e - 3 + q
                                    P.dve(lambda ee=ee, q=q, tt=tt: nc.vector.tensor_copy(
                                        cb[:, ee, tt * 128:(tt + 1) * 128], pcb[:, q * 128:(q + 1) * 128]),
                                        r=["fpcb"], w=[("fcb", ee)])
                for e in range(E):
                    for hh in range(NH):
                        wb2 = w2i % 2; w2i += 1
                        for q in range(JH):
                            P.dma("pool", lambda e=e, hh=hh, q=q, wb2=wb2: nc.gpsimd.dma_start(
                                out=w2e[wb2][:, q, :], in_=w2[l // 2 if moe else 0, e, hh * JH + q]),
                                w=[("fw2", wb2)])
                        for jj in range(JH):
                            j = hh * JH + jj
                            wbi = wi % 6; wi += 1
                            for m in range(2):
                                P.dma("pool", lambda e=e, j=j, m=m, wbi=wbi: nc.gpsimd.dma_start(
                                    out=wj[wbi][:, m, :, :], in_=w13[l // 2 if moe else 0, e, j, m]),
                                    w=[("fw13", wbi)])
                            for s in range(2):
                                a = ai % 2; ai += 1
                                cs = slice(s * 512, (s + 1) * 512)
                                for k in range(KT):
                                    P.pe(lambda k=k, a=a, wbi=wbi, cs=cs: nc.tensor.matmul(
                                        pa[a][:], wj[wbi][:, 0, k, :], hb[:, k, cs], start=(k == 0), stop=(k == KT - 1)),
                                        r=["fhb", ("fw13", wbi)], w=[("fpa", a)])
                                for k in range(KT):
                                    P.pe(lambda k=k, a=a, wbi=wbi, cs=cs: nc.tensor.matmul(
                                        pb[a][:], wj[wbi][:, 1, k, :], hb[:, k, cs], start=(k == 0), stop=(k == KT - 1)),
                                        r=["fhb", ("fw13", wbi)], w=[("fpb", a)])
                                if "ffn2" in self.dbg2s:
                                    continue
                                P.act(lambda a=a: nc.scalar.activation(out=sl_t[a][:], in_=pa[a][:], func=AF.Silu),
                                      r=[("fpa", a)], w=[("fsl", a)])
                                if "ffn3" in self.dbg2s:
                                    continue
                                if moe:
                                    P.dve(lambda a=a, e=e, cs=cs: nc.vector.tensor_tensor(
                                        sl_t[a][:], sl_t[a][:], cb[:, e, cs], ALU.mult),
                                        r=[("fsl", a), ("fcb", e)], w=[("fsl", a)])
                                P.dve(lambda a=a, jj=jj, cs=cs: nc.vector.tensor_tensor(
                                    g[:, jj, cs], pb[a][:], sl_t[a][:], ALU.mult),
                                    r=[("fpb", a), ("fsl", a)], w=[("fg", jj, s)])
                        for f in range(KT):
                            if self.dbg2s & {"ffn2", "ffn3", "ffn4"}:
                                break
                            for s in range(2):
                                y = yi % 2; yi += 1
                                cs = slice(s * 512, (s + 1) * 512)
                                for jj in range(JH):
                                    P.pe(lambda jj=jj, f=f, cs=cs, y=y, wb2=wb2: nc.tensor.matmul(
                                        pyy[y][:], w2e[wb2][:, jj, f * 128:(f + 1) * 128], g[:, jj, cs],
                                        start=(jj == 0), stop=(jj == JH - 1)),
                                        r=[("fw2", wb2), ("fg", jj, s)], w=[("fpy", y)])
                                if "ffn5" in self.dbg2s:
                                    continue
                                if True:
                                    P.dve(lambda f=f, y=y: nc.vector.tensor_scalar(
                                        ytmp[y][:], pyy[y][:], self.modv[:, 40 + f:41 + f], None, ALU.mult),
                                        r=[("fpy", y)], w=[("fyt", y)])
                                    P.dve(lambda f=f, cs=cs, y=y: nc.vector.tensor_tensor(
                                        xb[:, f, cs], xb[:, f, cs], ytmp[y][:], ALU.add),
                                        r=[("fyt", y), ("fxb", f, s)], w=[("fxb", f, s)])
                                    continue
                                P.dve(lambda f=f, cs=cs, y=y: nc.vector.scalar_tensor_tensor(
                                    xb[:, f, cs], pyy[y][:], self.modv[:, 40 + f:41 + f], xb[:, f, cs], ALU.mult, ALU.add),
                                    r=[("fpy", y), ("fxb", f, s)], w=[("fxb", f, s)])
                P.dma("sp", lambda sl=sl: nc.sync.dma_start(out=xTv[:, :, sl], in_=xb[:]),
                      r=[("fxb", f, s) for f in range(KT) for s in range(2)], w=[("dram", "xT", tb)])
                P.flush()

    def final_norm(self):
        nc, P = self.nc, self.P
        with ExitStack() as st:
            fg = self.sb(st, "fing", [128, KT], F32)
            xb = [self.sb(st, "ox", [128, KT, 512], F32) for _ in range(2)]
            sq = [self.sb(st, "osq", [128, KT, 512], BF16) for _ in range(2)]
            ms = [self.sb(st, "oms", [128, 512], F32) for _ in range(2)]
            yb = [self.sb(st, "oy", [128, KT, 512], F32) for _ in range(2)]
            ot = [self.sb(st, "oot", [128, D], F32) for _ in range(2)]
            pss = [self.ps(st, "ops", [128, 512]) for _ in range(2)]
            ptp = [self.ps(st, "optp", [128, 512]) for _ in range(2)]
            xTv = self.xT.rearrange("(kt p) t -> p kt t", p=128)
            P.dma("sp", lambda: nc.sync.dma_start(out=fg[:], in_=self.fin_g), w=["fing"])
            ti = 0
            pi = 0
            for tb in range(T // 512):
                b = tb % 2
                sl = slice(tb * 512, (tb + 1) * 512)
                P.dma("sp", lambda b=b, sl=sl: nc.sync.dma_start(out=xb[b][:], in_=xTv[:, :, sl]), w=[("ox", b)])
                P.act(lambda b=b: nc.scalar.activation(out=sq[b][:], in_=xb[b][:], func=AF.Square),
                      r=[("ox", b)], w=[("osq", b)])
                for k in range(KT):
                    P.pe(lambda b=b, k=k: nc.tensor.matmul(pss[b][:], self.ones_b[:], sq[b][:, k, :],
                                                           start=(k == 0), stop=(k == KT - 1)),
                         r=[("osq", b), "onesb"], w=[("ops", b)])
                P.dve(lambda b=b: nc.vector.tensor_scalar(ms[b][:], pss[b][:], 1.0 / D, EPS, ALU.mult, ALU.add),
                      r=[("ops", b)], w=[("oms", b)])
                P.act(lambda b=b: nc.scalar.activation(out=ms[b][:], in_=ms[b][:], func=AF.Sqrt),
                      r=[("oms", b)], w=[("oms", b)])
                P.dve(lambda b=b: nc.vector.reciprocal(ms[b][:], ms[b][:]), r=[("oms", b)], w=[("oms", b)])
                for k in range(KT):
                    P.dve(lambda b=b, k=k: nc.vector.scalar_tensor_tensor(
                        yb[b][:, k, :], xb[b][:, k, :], fg[:, k:k + 1], ms[b][:], ALU.mult, ALU.mult),
                        r=[("ox", b), ("oms", b), "fing"], w=[("oy", b, k)])
                for j in range(4):
                    o = ti % 2; ti += 1
                    for half in range(2):
                        p = pi % 2; pi += 1
                        for kk in range(4):
                            k = half * 4 + kk
                            P.pe(lambda b=b, k=k, kk=kk, j=j, p=p: nc.tensor.transpose(
                                ptp[p][:, kk * 128:(kk + 1) * 128], yb[b][:, k, j * 128:(j + 1) * 128], self.ident_f[:]),
                                r=[("oy", b, k), "identf"], w=[("optp", p)])
                        if half == 0:
                            P.dve(lambda o=o, p=p: nc.vector.tensor_copy(ot[o][:, 0:512], ptp[p][:]),
                                  r=[("optp", p)], w=[("oot", o, 0)])
                        else:
                            P.act(lambda o=o, p=p: nc.scalar.copy(ot[o][:, 512:1024], ptp[p][:]),
                                  r=[("optp", p)], w=[("oot", o, 1)])
                    row0 = tb * 512 + j * 128
                    P.dma("sp", lambda o=o, row0=row0: nc.sync.dma_start(out=self.out[row0:row0 + 128, :], in_=ot[o][:]),
                          r=[("oot", o, 0), ("oot", o, 1)], w=[("dram", "out", row0)])
            P.flush()

    def layer(self, l):
        self.phase_mod(l)
        self.phase_norm(0)
        if "a" in self.mixers:
            self.mixer_a(l)
        if "b" in self.mixers:
            self.mixer_b(l)
        if "c" in self.mixers:
            self.mixer_c(l)
        if "d" in self.mixers:
            self.mixer_d(l)
        if self.stop_after == ("mix", l):
            return "stop"
        self.phase_wout(l)
        if self.stop_after == ("wout", l):
            return "stop"
        self.phase_norm(1)
        if self.stop_after == ("preffn", l):
            return "stop"
        self.phase_ffn(l, moe=(l % 2 == 1))


def host_prep(inputs, b, shared=None):
    f = np.float32
    m = dict(shared) if shared is not None else host_shared(inputs)
    m["x"] = np.ascontiguousarray(inputs["x"][b], dtype=f)
    m["c"] = np.ascontiguousarray(np.asarray(inputs["c"][b], dtype=f).reshape(KT, 128).T)
    return m


def host_shared(inputs):
    f = np.float32
    m = {}
    m["ada_w"] = np.ascontiguousarray(np.asarray(inputs["ada_w"], dtype=f).reshape(DEPTH, KT, 128, 6 * D))
    m["ada_b"] = np.ascontiguousarray(np.asarray(inputs["ada_b"], dtype=f).reshape(DEPTH, 48, 128).transpose(0, 2, 1))
    m["mix_g"] = np.ascontiguousarray(np.asarray(inputs["mix_norm_g"], dtype=f).reshape(DEPTH, KT, 128).transpose(0, 2, 1))
    m["ffn_g"] = np.ascontiguousarray(np.asarray(inputs["ffn_norm_g"], dtype=f).reshape(DEPTH, KT, 128).transpose(0, 2, 1))
    m["fin_g"] = np.ascontiguousarray(np.asarray(inputs["final_norm_g"], dtype=f).reshape(KT, 128).T)
    m["ident"] = np.eye(128, dtype=f)
    w_in = np.asarray(inputs["w_in"], dtype=f)

    def cols(a, b):
        return w_in[:, :, a:b]
    sl_ = np.arange(128)[:, None]
    tl_ = np.arange(128)[None, :]

    def tiles(w):
        return np.ascontiguousarray(w.reshape(DEPTH, KT, 128, w.shape[-1]))
    m["w_out"] = np.ascontiguousarray(np.asarray(inputs["w_out"], dtype=f).reshape(DEPTH, KT, 128, D))

    def w13_layout(w1, w3):
        E_, _, F_ = w1.shape
        a = np.stack([w1, w3], axis=1)
        a = a.reshape(E_, 2, KT, 128, F_ // 128, 128)
        return np.ascontiguousarray(a.transpose(0, 4, 1, 3, 2, 5))

    m["ffn_w13"] = w13_layout(np.asarray(inputs["ffn_w1"], dtype=f), np.asarray(inputs["ffn_w3"], dtype=f))[None]
    m["ffn_w2"] = np.ascontiguousarray(np.asarray(inputs["ffn_w2"], dtype=f).reshape(1, 1, D_FF // 128, 128, D))
    m["moe_w13"] = w13_layout(np.asarray(inputs["moe_w1"], dtype=f)[0], np.asarray(inputs["moe_w3"], dtype=f)[0])[None]
    m["moe_w2"] = np.ascontiguousarray(np.asarray(inputs["moe_w2"], dtype=f).reshape(1, NEXP, D_FFE // 128, 128, D))
    m["router"] = np.ascontiguousarray(np.asarray(inputs["moe_router"], dtype=f)[0].reshape(KT, 128, 8))
    perm = np.concatenate([np.arange(16, 32), np.arange(0, 16)])
    z64 = np.zeros((DEPTH, D, 64), f)
    kr = cols(1152, 1184)
    m["w_b"] = tiles(np.concatenate([cols(768, 1024), cols(1024, 1152), z64, kr, z64, kr[:, :, perm]], axis=-1))
    wuq = np.asarray(inputs["mla_w_uq"], dtype=f).reshape(DEPTH, 256, 4, 96)
    wuqr = np.zeros_like(wuq)
    wuqr[:, :, :, 64:96] = wuq[:, :, :, 64:96][:, :, :, perm]
    m["w_uq"] = np.ascontiguousarray(wuq.reshape(DEPTH, 2, 128, 384))
    m["w_uqr"] = np.ascontiguousarray(wuqr.reshape(DEPTH, 2, 128, 384))
    wukv = np.asarray(inputs["mla_w_ukv"], dtype=f).reshape(DEPTH, 128, 4, 128)
    m["w_ukvk"] = np.ascontiguousarray(wukv[:, :, :, 0:64].reshape(DEPTH, 1, 128, 256))
    m["w_ukvv"] = np.ascontiguousarray(wukv[:, :, :, 64:128].reshape(DEPTH, 1, 128, 256))
    m["gq"] = np.ascontiguousarray(np.asarray(inputs["mla_q_norm_g"], dtype=f).reshape(DEPTH, 2, 128).transpose(0, 2, 1))
    m["gkv"] = np.ascontiguousarray(np.asarray(inputs["mla_kv_norm_g"], dtype=f).reshape(DEPTH, 128, 1))
    inv = (np.float32(10000.0) ** (-np.arange(0, 32, 2, dtype=f) / np.float32(32))).astype(f)
    ang = (np.arange(T, dtype=f)[:, None] * inv[None, :]).astype(f)
    cs_, sn_ = np.cos(ang).astype(f), np.sin(ang).astype(f)
    rope = np.zeros((128, 2, T), f)
    rope[64:80, 0] = cs_.T; rope[80:96, 0] = cs_.T
    rope[64:80, 1] = -sn_.T; rope[80:96, 1] = sn_.T
    m["rope"] = rope
    chunkmask = np.where(sl_ // 64 > tl_ // 64, f(NEG), f(0.0)).astype(f)
    m["diagmask"] = np.ascontiguousarray(np.tile(chunkmask, (1, 4)))
    m["w_a"] = tiles(np.concatenate([cols(0, 256), cols(256, 512), cols(512, 768)], axis=-1))
    slopes = (2.0 ** (-8.0 * np.arange(1, 5, dtype=f) / 4)).astype(f)
    tpos = np.arange(T, dtype=f)
    augq = np.zeros((4, 3, T), f); augk = np.zeros((4, 3, T), f)
    for h in range(4):
        augq[h, 0] = 1.0; augq[h, 1] = 1.0; augq[h, 2] = -slopes[h] * tpos
        augk[h, 0] = slopes[h] * (64.0 * (tpos // 64)); augk[h, 1] = slopes[h] * (tpos % 64); augk[h, 2] = 1.0
    m["augq"] = augq; m["augk"] = augk

    def corr_tile(sig):
        c = np.where(sl_ > tl_, -2.0 * sig * (sl_ - tl_), 0.0).astype(f)
        return np.where(sl_ // 64 > tl_ // 64, f(NEG), c).astype(f)
    ca = np.zeros((2, 128, 4, 128), f)
    for g in range(2):
        for j in range(4):
            ca[g, :, j, :] = corr_tile(slopes[2 * g + j // 2])
    m["corr_a"] = np.ascontiguousarray(ca.reshape(2, 128, 512))
    dl = np.asarray(inputs["diff_lambda"], dtype=f).reshape(DEPTH, 1, 128)
    m["dlam"] = np.ascontiguousarray(np.broadcast_to(dl, (DEPTH, 128, 128)))
    dg_ = np.asarray(inputs["diff_norm_g"], dtype=f).reshape(DEPTH, 1, 64)
    m["dng"] = np.ascontiguousarray(np.broadcast_to(dg_, (DEPTH, 128, 64)))
    ki = cols(2208, 2240)
    m["w_c"] = tiles(np.concatenate([cols(1184, 1440), cols(1440, 1696), cols(1696, 1952), cols(1952, 2208),
                                     ki, ki, ki, ki, cols(2240, 2248)], axis=-1))
    cc = np.zeros((128, 4, 128), f)
    for j in range(4):
        cc[:, j, :] = corr_tile(slopes[j])
    m["corr_c"] = np.ascontiguousarray(cc.reshape(128, 512))
    m["ident4"] = np.ascontiguousarray(np.tile(np.eye(128, dtype=f), (1, 4)))
    m["w_d"] = tiles(np.concatenate([cols(2248, 2504), cols(2504, 2760), cols(2760, 3016)], axis=-1))
    rb = np.asarray(inputs["band_rel_bias"], dtype=f)
    bd = np.empty((DEPTH, 5, 128, 4, 128), f)
    for dd in range(5):
        delta = 128 * dd + tl_ - sl_
        idx = np.clip(delta, -128, 128) + 128
        dc = (128 * dd + tl_) // 64 - sl_ // 64
        ok = (dc >= 0) & (dc <= 8)
        for h in range(4):
            g = rb[:, h, :][:, idx]
            bd[:, dd, :, h, :] = np.where(ok[None], g, f(NEG))
    m["bias_d"] = np.ascontiguousarray(bd.reshape(DEPTH, 5, 128, 512))
    return m


def kernel(**inputs):
    bld = Builder()
    nc = bld.build()
    shared = host_shared(inputs)
    in_maps = []
    for b in range(8):
        m = host_prep(inputs, b, shared)
        in_maps.append({k: v for k, v in m.items() if k in bld.inputs})
    res = run_bass_kernel_spmd(nc, in_maps, core_ids=list(range(8)))
    return np.stack([np.asarray(r["out"]) for r in res.results], axis=0).astype(np.float32)
```

```python
import math
import os
from contextlib import ExitStack
import numpy as np
import concourse.bass as bass
import concourse.mybir as mybir
from concourse.bass_utils import run_bass_kernel_spmd

F32 = mybir.dt.float32
BF16 = mybir.dt.bfloat16
AF = mybir.ActivationFunctionType
ALU = mybir.AluOpType
AX = mybir.AxisListType

D = 1024
T = 4096
DEPTH = 2
NT = T // 128
KT = D // 128
EPS = 1e-6
D_FF = 2816
D_FFE = 3584
NEXP = 8
NEG = -30000.0


class Prog:
    NSLOT = 12

    def __init__(self, nc, es):
        self.nc = nc
        self.ops = []
        self.engs = {"pe": nc.tensor, "act": nc.scalar, "dve": nc.vector,
                     "pool": nc.gpsimd, "sp": nc.sync}
        self.esem = {e: es.enter_context(nc.semaphore("sem_" + e)) for e in self.engs}
        self.dq = ("sp", "act", "pool")
        self.dsem = {q: [es.enter_context(nc.semaphore("dsem_%s%d" % (q, k))) for k in range(self.NSLOT)]
                     for q in self.dq}
        self.ecount = {e: 0 for e in self.engs}
        self.dcount = {q: 0 for q in self.dq}
        self.eclock = {e: {} for e in self.engs}
        self.sig = {}
        self.done_clock = {}
        self.gid = 0
        self.last_comp = {}
        self.dhist = {q: [] for q in self.dq}
        self.n_ops = 0
        self.n_waits = 0
        self.n_sig = 0

    def op(self, eng, fn, reads=(), writes=(), dma=False):
        self.ops.append((eng, fn, tuple(reads), tuple(writes), dma))

    def pe(self, fn, r=(), w=()): self.op("pe", fn, r, w)
    def act(self, fn, r=(), w=()): self.op("act", fn, r, w)
    def dve(self, fn, r=(), w=()): self.op("dve", fn, r, w)
    def pool(self, fn, r=(), w=()): self.op("pool", fn, r, w)
    def dma(self, q, fn, r=(), w=()): self.op(q, fn, r, w, True)

    def _sem(self, sk):
        return self.esem[sk[1]] if sk[0] == "e" else self.dsem[sk[1]][sk[2]]

    def _bar_deps(self):
        d = set(self.last_comp.values())
        for q in self.dq:
            d.update(self.dhist[q][-self.NSLOT:])
        return d

    def flush(self):
        ops = self.ops
        self.ops = []
        n = len(ops)
        base = self.gid
        self.gid += n
        bar = self._bar_deps()
        last_w = {}
        readers = {}
        deps = [None] * n
        seen = set()
        openg = {}
        for i, (eng, fn, rd, wr, is_dma) in enumerate(ops):
            g = base + i
            openg[g] = (eng, is_dma)
            d = set()
            if eng not in seen:
                seen.add(eng)
                d |= bar
            for r in rd:
                lw = last_w.get(r)
                if lw is not None:
                    d.add(lw)
            for r in wr:
                lw = last_w.get(r)
                if lw is not None:
                    d.add(lw)
                for x in readers.get(r, ()):
                    d.add(x)
            for r in rd:
                readers.setdefault(r, []).append(g)
            for r in wr:
                last_w[r] = g
                readers[r] = []
            if is_dma:
                h = self.dhist[eng]
                if len(h) >= self.NSLOT:
                    d.add(h[-self.NSLOT])
                h.append(g)
            d.discard(g)
            if eng == "pe" and not is_dma:
                d = {x for x in d if not (x >= base and openg[x] == ("pe", False))}
            deps[i] = d
            if not is_dma:
                self.last_comp[eng] = g
        signal = set(self.last_comp.values())
        for i in range(n):
            if ops[i][4]:
                signal.add(base + i)
            signal |= deps[i]
        for q in self.dq:
            self.dhist[q] = self.dhist[q][-self.NSLOT:]
        for i, (eng, fn, rd, wr, is_dma) in enumerate(ops):
            g = base + i
            if is_dma:
                k = self.dcount[eng]
                self.dcount[eng] += 1
                self.sig[g] = (("d", eng, k % self.NSLOT), 16 * (k // self.NSLOT + 1))
            elif g in signal:
                self.ecount[eng] += 1
                self.sig[g] = (("e", eng), self.ecount[eng])
        for i, (eng, fn, rd, wr, is_dma) in enumerate(ops):
            g = base + i
            clk = self.eclock[eng]
            e = self.engs[eng]
            need = {}
            for x in deps[i]:
                sk, sv = self.sig[x]
                if clk.get(sk, 0) < sv and need.get(sk, 0) < sv:
                    need[sk] = sv
            for x in deps[i]:
                for k2, v2 in self.done_clock[x].items():
                    if clk.get(k2, 0) < v2:
                        clk[k2] = v2
            for sk, sv in need.items():
                e.wait_ge(self._sem(sk), sv)
                self.n_waits += 1
                if clk.get(sk, 0) < sv:
                    clk[sk] = sv
            inst = fn()
            if g in self.sig:
                sk, sv = self.sig[g]
                inst.then_inc(self._sem(sk), 16 if is_dma else 1)
                dc = dict(clk)
                dc[sk] = sv
                self.done_clock[g] = dc
                self.n_sig += 1
        self.n_ops += n
        keep = self._bar_deps()
        self.sig = {g: v for g, v in self.sig.items() if g in keep}
        self.done_clock = {g: v for g, v in self.done_clock.items() if g in keep}

    def finish(self):
        self.flush()
        e = self.nc.sync
        clk = self.eclock["sp"]
        for x in self._bar_deps():
            sk, sv = self.sig[x]
            if clk.get(sk, 0) < sv:
                e.wait_ge(self._sem(sk), sv)
                clk[sk] = sv
        return dict(n_ops=self.n_ops, n_waits=self.n_waits, n_sig=self.n_sig, ecount=dict(self.ecount), dcount=dict(self.dcount))


class Builder:
    def __init__(self, debug=None, stop_after=None, mixers="abcd"):
        self.mixers = mixers
        import os
        self.dbg = os.environ.get("MK_DBG", "")
        self.dbg2s = set(os.environ.get("MK_DBG2", "").split(","))
        self.dbg2 = ""
        self.debug = debug or []
        self.stop_after = stop_after
        self.nc = bass.Bass("TRN2", target_bir_lowering=False)
        self.es = ExitStack()
        self.P = Prog(self.nc, self.es)
        self.uid = 0
        self.inputs = {}

    def din(self, name, shape, dt=F32):
        t = self.nc.dram_tensor(name, list(shape), dt, kind="ExternalInput").ap()
        self.inputs[name] = t
        return t

    def dscr(self, name, shape, dt):
        kind = "ExternalOutput"
        return self.nc.dram_tensor(name, list(shape), dt, kind=kind).ap()

    def sb(self, stack, name, shape, dt):
        self.uid += 1
        return stack.enter_context(self.nc.sbuf_tensor("%s_%d" % (name, self.uid), list(shape), dt))

    def ps(self, stack, name, shape, dt=F32):
        self.uid += 1
        return stack.enter_context(self.nc.psum_tensor("%s_%d" % (name, self.uid), list(shape), dt))

    def build(self):
        nc, P = self.nc, self.P
        with self.es as es:
            self.declare_io()
            self.consts(es)
            P.flush()
            self.phase_l0()
            for l in range(DEPTH):
                if self.layer(l) == "stop" or self.stop_after == ("layer", l):
                    break
            else:
                self.final_norm()
            st = P.finish()
        self.stats = st
        return nc

    def declare_io(self):
        nc = self.nc
        self.x_in = self.din("x", [T, D])
        self.c_in = self.din("c", [128, KT])
        self.ada_w = self.din("ada_w", [DEPTH, KT, 128, 6 * D])
        self.ada_b = self.din("ada_b", [DEPTH, 128, 48])
        self.mix_g = self.din("mix_g", [DEPTH, 128, KT])
        self.ffn_g = self.din("ffn_g", [DEPTH, 128, KT])
        self.fin_g = self.din("fin_g", [128, KT])
        self.ident_in = self.din("ident", [128, 128])
        self.w_d = self.din("w_d", [DEPTH, KT, 128, 768])
        self.w_out = self.din("w_out", [DEPTH, KT, 128, D])
        self.w_b = self.din("w_b", [DEPTH, KT, 128, 576])
        self.w_uq = self.din("w_uq", [DEPTH, 2, 128, 384])
        self.w_uqr = self.din("w_uqr", [DEPTH, 2, 128, 384])
        self.w_ukvk = self.din("w_ukvk", [DEPTH, 1, 128, 256])
        self.w_ukvv = self.din("w_ukvv", [DEPTH, 1, 128, 256])
        self.gq = self.din("gq", [DEPTH, 128, 2])
        self.gkv = self.din("gkv", [DEPTH, 128, 1])
        self.rope = self.din("rope", [128, 2, T])
        self.diagmask = self.din("diagmask", [128, 512])
        self.w_a = self.din("w_a", [DEPTH, KT, 128, 768])
        self.w_c = self.din("w_c", [DEPTH, KT, 128, 1160])
        self.corr_c = self.din("corr_c", [128, 512])
        self.ident4 = self.din("ident4", [128, 512])
        self.corr_a = self.din("corr_a", [2, 128, 512])
        self.augq = self.din("augq", [4, 3, T])
        self.augk = self.din("augk", [4, 3, T])
        self.dlam = self.din("dlam", [DEPTH, 128, 128])
        self.dng = self.din("dng", [DEPTH, 128, 64])
        self.ffn_w13 = self.din("ffn_w13", [1, 1, D_FF // 128, 2, 128, KT, 128])
        self.ffn_w2 = self.din("ffn_w2", [1, 1, D_FF // 128, 128, D])
        self.moe_w13 = self.din("moe_w13", [1, NEXP, D_FFE // 128, 2, 128, KT, 128])
        self.moe_w2 = self.din("moe_w2", [1, NEXP, D_FFE // 128, 128, D])
        self.router = self.din("router", [KT, 128, 8])
        self.bias_d = self.din("bias_d", [DEPTH, 5, 128, 512])
        self.out = nc.dram_tensor("out", [T, D], F32, kind="ExternalOutput").ap()
        self.xT = self.dscr("xT", [D, T], F32)
        self.hnT = self.dscr("hnT", [D, T], BF16)
        self.oT = self.dscr("oT", [D, T], BF16)

    def consts(self, es):
        nc, P = self.nc, self.P
        self.ident_f = self.sb(es, "identf", [128, 128], F32)
        self.ident_b = self.sb(es, "identb", [128, 128], BF16)
        self.ones_b = self.sb(es, "onesb", [128, 128], BF16)
        self.modv = self.sb(es, "modv", [128, 48], F32)
        self.gvec = self.sb(es, "gvec", [128, 4 * KT], F32)
        P.dma("sp", lambda: nc.sync.dma_start(out=self.ident_f[:], in_=self.ident_in), w=["identf"])
        P.dve(lambda: nc.vector.tensor_copy(self.ident_b[:], self.ident_f[:]), r=["identf"], w=["identb"])
        P.dve(lambda: nc.vector.memset(self.ones_b[:], 1.0), w=["onesb"])

    def phase_l0(self):
        nc, P = self.nc, self.P
        with ExitStack() as st:
            xin = [self.sb(st, "xin", [128, 4, D], F32) for _ in range(2)]
            xo = [self.sb(st, "xo", [128, KT, 512], F32) for _ in range(2)]
            pt = [self.ps(st, "pt", [128, 512]) for _ in range(2)]
            xv = self.x_in.rearrange("(tb j p) f -> tb p j f", j=4, p=128)
            xTv = self.xT.rearrange("(kt p) t -> p kt t", p=128)
            for tb in range(T // 512):
                b = tb % 2
                P.dma("sp", lambda tb=tb, b=b: nc.sync.dma_start(out=xin[b][:], in_=xv[tb]),
                      w=[("xin", b)])
                for kt in range(KT):
                    pb = kt % 2
                    for j in range(4):
                        P.pe(lambda b=b, kt=kt, j=j, pb=pb: nc.tensor.transpose(
                            pt[pb][:, j * 128:(j + 1) * 128], xin[b][:, j, kt * 128:(kt + 1) * 128],
                            self.ident_f[:]),
                            r=[("xin", b), "identf"], w=[("pt", pb)])
                    if kt % 2 == 0:
                        P.dve(lambda b=b, kt=kt, pb=pb: nc.vector.tensor_copy(xo[b][:, kt, :], pt[pb][:]),
                              r=[("pt", pb)], w=[("xo", b, kt)])
                    else:
                        P.act(lambda b=b, kt=kt, pb=pb: nc.scalar.copy(xo[b][:, kt, :], pt[pb][:]),
                              r=[("pt", pb)], w=[("xo", b, kt)])
                P.dma("sp", lambda tb=tb, b=b: nc.sync.dma_start(
                    out=xTv[:, :, tb * 512:(tb + 1) * 512], in_=xo[b][:]),
                    r=[("xo", b, kt) for kt in range(KT)], w=[("dram", "xT", tb)])
            P.flush()

    def phase_mod(self, l):
        nc, P = self.nc, self.P
        with ExitStack() as st:
            cs = self.sb(st, "cs", [128, KT], F32)
            sc = self.sb(st, "sc", [128, KT], BF16)
            wb = [self.sb(st, "adaw", [128, 6 * D], BF16) for _ in range(2)]
            ab = self.sb(st, "adab", [128, 48], F32)
            gg = self.sb(st, "gg", [128, 2 * KT], F32)
            pm = self.ps(st, "pm", [128, 512])
            P.dma("sp", lambda: nc.sync.dma_start(out=cs[:], in_=self.c_in), w=["cs"])
            P.dma("sp", lambda: nc.sync.dma_start(out=ab[:], in_=self.ada_b[l]), w=["adab"])
            P.dma("sp", lambda: nc.sync.dma_start(out=gg[:, 0:KT], in_=self.mix_g[l]), w=["gg0"])
            P.dma("sp", lambda: nc.sync.dma_start(out=gg[:, KT:2 * KT], in_=self.ffn_g[l]), w=["gg1"])
            P.act(lambda: nc.scalar.activation(out=sc[:], in_=cs[:], func=AF.Silu), r=["cs"], w=["sc"])
            P.dve(lambda: nc.vector.memset(pm[:], 0.0), w=["pm"])
            for kt in range(KT):
                b = kt % 2
                P.dma("pool", lambda kt=kt, b=b: nc.gpsimd.dma_start(out=wb[b][:], in_=self.ada_w[l, kt]),
                      w=[("adaw", b)])
                for ft in range(48):
                    P.pe(lambda kt=kt, b=b, ft=ft: nc.tensor.matmul(
                        pm[:, ft:ft + 1], wb[b][:, ft * 128:(ft + 1) * 128], sc[:, kt:kt + 1],
                        start=False, stop=(kt == KT - 1), skip_group_check=True),
                        r=[("adaw", b), "sc"], w=["pm"])
            mv = self.modv
            P.dve(lambda: nc.vector.tensor_tensor(mv[:], pm[:, 0:48], ab[:], ALU.add),
                  r=["pm", "adab"], w=["modv"])
            gv = self.gvec
            P.dve(lambda: nc.vector.scalar_tensor_tensor(
                gv[:, 0:KT], mv[:, 8:16], 1.0, gg[:, 0:KT], ALU.add, ALU.mult),
                r=["modv", "gg0"], w=["gvec0"])
            P.dve(lambda: nc.vector.scalar_tensor_tensor(
                gv[:, KT:2 * KT], mv[:, 32:40], 1.0, gg[:, KT:2 * KT], ALU.add, ALU.mult),
                r=["modv", "gg1"], w=["gvec1"])
            P.flush()

    def phase_norm(self, which):
        nc, P = self.nc, self.P
        goff = which * KT
        shoff = 0 if which == 0 else 24
        with ExitStack() as st:
            xb = [self.sb(st, "nx", [128, KT, 512], F32) for _ in range(2)]
            sq = [self.sb(st, "nsq", [128, KT, 512], BF16) for _ in range(2)]
            ms = [self.sb(st, "nms", [128, 512], F32) for _ in range(2)]
            tmp = [self.sb(st, "ntmp", [128, KT, 512], F32) for _ in range(2)]
            hb = [self.sb(st, "nhb", [128, KT, 512], BF16) for _ in range(2)]
            pss = [self.ps(st, "nps", [128, 512]) for _ in range(2)]
            xTv = self.xT.rearrange("(kt p) t -> p kt t", p=128)
            hTv = self.hnT.rearrange("(kt p) t -> p kt t", p=128)
            def stage1(tb):
                b = tb % 2
                P.dma("sp", lambda tb=tb, b=b: nc.sync.dma_start(
                    out=xb[b][:], in_=xTv[:, :, tb * 512:(tb + 1) * 512]),
                    r=[("dram", "xT", tb)], w=[("nx", b)])
                P.act(lambda b=b: nc.scalar.activation(out=sq[b][:], in_=xb[b][:], func=AF.Square),
                      r=[("nx", b)], w=[("nsq", b)])
                for kt in range(KT):
                    P.pe(lambda b=b, kt=kt: nc.tensor.matmul(
                        pss[b][:], self.ones_b[:], sq[b][:, kt, :], start=(kt == 0), stop=(kt == KT - 1)),
                        r=[("nsq", b), "onesb"], w=[("nps", b)])
                P.dve(lambda b=b: nc.vector.tensor_scalar(
                    ms[b][:], pss[b][:], 1.0 / D, EPS, ALU.mult, ALU.add),
                    r=[("nps", b)], w=[("nms", b)])
                P.act(lambda b=b: nc.scalar.activation(out=ms[b][:], in_=ms[b][:], func=AF.Sqrt),
                      r=[("nms", b)], w=[("nms", b)])
                P.dve(lambda b=b: nc.vector.reciprocal(ms[b][:], ms[b][:]),
                      r=[("nms", b)], w=[("nms", b)])

            def stage2(tb):
                b = tb % 2
                for kt in range(KT):
                    P.dve(lambda b=b, kt=kt: nc.vector.scalar_tensor_tensor(
                        tmp[b][:, kt, :], xb[b][:, kt, :], self.gvec[:, goff + kt:goff + kt + 1],
                        ms[b][:], ALU.mult, ALU.mult),
                        r=[("nx", b), ("nms", b), "gvec%d" % which], w=[("ntmp", b, kt)])
                    P.act(lambda b=b, kt=kt: nc.scalar.activation(
                        out=hb[b][:, kt, :], in_=tmp[b][:, kt, :], func=AF.Identity,
                        bias=self.modv[:, shoff + kt:shoff + kt + 1], scale=1.0),
                        r=[("ntmp", b, kt), "modv"], w=[("nhb", b, kt)])
                P.dma("sp", lambda tb=tb, b=b: nc.sync.dma_start(
                    out=hTv[:, :, tb * 512:(tb + 1) * 512], in_=hb[b][:]),
                    r=[("nhb", b, kt) for kt in range(KT)], w=[("dram", "hnT", tb)])

            stage1(0)
            for tb in range(T // 512):
                if tb + 1 < T // 512:
                    stage1(tb + 1)
                stage2(tb)
            P.flush()

    def load_w(self, st, name, dram_ap, ncols, nk=KT):
        nc, P = self.nc, self.P
        w = self.sb(st, name, [128, nk, ncols], BF16)
        for k in range(nk):
            P.dma("pool", lambda k=k: nc.gpsimd.dma_start(out=w[:, k, :], in_=dram_ap[k]), w=[(name, k)])
        return w

    def hn_blocks(self, st):
        nc, P = self.nc, self.P
        hb = [self.sb(st, "hb", [128, KT, 512], BF16) for _ in range(2)]
        hTv = self.hnT.rearrange("(kt p) t -> p kt t", p=128)

        def load(tb):
            b = tb % 2
            P.dma("sp", lambda: nc.sync.dma_start(out=hb[b][:], in_=hTv[:, :, tb * 512:(tb + 1) * 512]),
                  w=[("hb", b)])
            return hb[b], ("hb", b)
        return load

    def proj_fm(self, pp, ppr, w, wres, c0, M, hb, hres, nk=KT):
        nc, P = self.nc, self.P
        for k in range(nk):
            P.pe(lambda k=k: nc.tensor.matmul(pp[0:M, :], w[:, k, c0:c0 + M], hb[:, k, :],
                                              start=(k == 0), stop=(k == nk - 1)),
                 r=[hres] + wres, w=[ppr])

    def proj_tm(self, pp, ppr, w, wres, c0, n, hb, hres, j, nk=KT):
        nc, P = self.nc, self.P
        for k in range(nk):
            P.pe(lambda k=k: nc.tensor.matmul(pp[:, 0:n], hb[:, k, j * 128:(j + 1) * 128], w[:, k, c0:c0 + n],
                                              start=(k == 0), stop=(k == nk - 1)),
                 r=[hres] + wres, w=[ppr])

    def attn_core(self, st, name, groups, kts_fn, pre_fn, qk_fn, v_fn, out_fn, before_qt=None, ns=3):
        nc, P = self.nc, self.P
        S = [self.ps(st, "S", [128, 512]) for _ in range(ns)]
        O = [self.ps(st, "O", [128, 512]) for _ in range(2)]
        Pt = [self.sb(st, "Pt", [128, 512], BF16) for _ in range(ns + 1)]
        rl = [self.sb(st, "rl", [128, 4], F32) for _ in range(2)]
        if os.environ.get("MK_NOATTN"):
            return
        tasks = []
        oi = 0
        for qt in range(NT):
            for g in range(groups):
                kts = kts_fn(qt)
                for ki, kt in enumerate(kts):
                    tasks.append((qt, g, ki, kt, len(kts), oi % 2, len(tasks) % ns, len(tasks) % (ns + 1)))
                oi += 1

        def emit_qk(t):
            qt, g, ki, kt, nk, ob, sbk, pb = t
            if ki == 0 and g == 0 and before_qt is not None:
                before_qt(qt)
            pre = pre_fn(g, qt, kt)
            first = True
            for (lh, rh, rr) in pre:
                P.pe(lambda lh=lh, rh=rh, first=first: nc.tensor.matmul(
                    S[sbk][:], lh, rh, start=first, stop=False, skip_group_check=True),
                    r=rr, w=[("S", sbk)])
                first = False
            for j in range(4):
                lh, rh = qk_fn(g, j, qt, kt)
                P.pe(lambda lh=lh, rh=rh, first=first, j=j: nc.tensor.matmul(
                    S[sbk][:, j * 128:(j + 1) * 128], lh, rh, start=first, stop=(j == 3),
                    skip_group_check=True), w=[("S", sbk)])
                first = False

        def emit_exp(t):
            qt, g, ki, kt, nk, ob, sbk, pb = t
            P.act(lambda: nc.scalar.activation(out=Pt[pb][:], in_=S[sbk][:], func=AF.Exp),
                  r=[("S", sbk)], w=[("Pt", pb)])

        def emit_pv(t):
            qt, g, ki, kt, nk, ob, sbk, pb = t
            for j in range(4):
                va = v_fn(g, j, kt)
                P.pe(lambda va=va, j=j: nc.tensor.matmul(
                    O[ob][:, j * 66:j * 66 + 65], Pt[pb][:, j * 128:(j + 1) * 128], va,
                    start=(ki == 0 and j == 0), stop=(ki == nk - 1), skip_group_check=True),
                    r=[("Pt", pb)], w=[("O", ob)])
            if ki == nk - 1:
                P.dve(lambda: nc.vector.reciprocal(
                    rl[ob][:, :], O[ob][:, 0:264].rearrange("p (j c) -> p j c", c=66)[:, :, 64]),
                    r=[("O", ob)], w=[("rl", ob)])
                out_fn(g, qt, O[ob], ("O", ob), rl[ob], ("rl", ob))

        la = ns - 1
        for i in range(min(la, len(tasks))):
            emit_qk(tasks[i])
        for i, t in enumerate(tasks):
            emit_exp(t)
            if i + la < len(tasks):
                emit_qk(tasks[i + la])
            emit_pv(t)

    def std_out(self, st, name, moff, ntp=2):
        nc, P = self.nc, self.P
        on = [self.sb(st, "on", [128, 256], BF16) for _ in range(2)]
        tp = [self.ps(st, "tp", [128, 2, 128], BF16) for _ in range(ntp)]
        stage = [self.sb(st, "ostg", [128, 2, 512], BF16) for _ in range(2)]
        oTv = self.oT[moff:moff + 256, :].rearrange("(i p) t -> p i t", p=128)

        def out_fn(g, qt, O, Or, rl, rlr):
            nb = qt % 2
            sbi = (qt // 4) % 2
            q4 = qt % 4
            tpb = qt % ntp
            for j in range(4):
                if True:
                    P.dve(lambda j=j: nc.vector.tensor_scalar(
                        on[nb][:, j * 64:(j + 1) * 64], O[:, j * 66:j * 66 + 64], rl[:, j:j + 1], None, ALU.mult),
                        r=[Or, rlr], w=[("on", nb, j)])
                else:
                    P.act(lambda j=j: nc.scalar.activation(
                        out=on[nb][:, j * 64:(j + 1) * 64], in_=O[:, j * 66:j * 66 + 64], func=AF.Copy,
                        scale=rl[:, j:j + 1]),
                        r=[Or, rlr], w=[("on", nb, j)])
            if "notp" in self.dbg2s:
                return
            for i in range(2):
                P.pe(lambda i=i: nc.tensor.transpose(tp[tpb][:, i, :], on[nb][:, i * 128:(i + 1) * 128],
                                                     self.ident_b[:]),
                     r=[("on", nb, 2 * i), ("on", nb, 2 * i + 1)], w=[("tp", tpb)])
            if "nostg" in self.dbg2s:
                return
            P.dve(lambda: nc.vector.tensor_copy(stage[sbi][:, 0, q4 * 128:(q4 + 1) * 128], tp[tpb][:, 0, :]),
                  r=[("tp", tpb)], w=[("ostg", sbi, 0)])
            P.dve(lambda: nc.vector.tensor_copy(stage[sbi][:, 1, q4 * 128:(q4 + 1) * 128], tp[tpb][:, 1, :]),
                  r=[("tp", tpb)], w=[("ostg", sbi, 1)])
            if q4 == 3 and "nodma" not in self.dbg2s:
                tb = qt // 4
                P.dma("sp", lambda: nc.sync.dma_start(out=oTv[:, :, tb * 512:(tb + 1) * 512], in_=stage[sbi][:]),
                      r=[("ostg", sbi, 0), ("ostg", sbi, 1)], w=[("dram", "oT", name, tb)])
        return out_fn

    def v_evac(self, V, pp, ppr, tt, eng_i):
        nc, P = self.nc, self.P
        for h in range(4):
            if (eng_i + h) % 2 == 0:
                P.dve(lambda h=h: nc.vector.tensor_copy(V[:, tt, h, 0:64], pp[:, h * 64:(h + 1) * 64]),
                      r=[ppr], w=[("V", tt)])
            else:
                P.act(lambda h=h: nc.scalar.copy(V[:, tt, h, 0:64], pp[:, h * 64:(h + 1) * 64]),
                      r=[ppr], w=[("V", tt)])

    def mixer_d(self, l):
        nc, P = self.nc, self.P
        with ExitStack() as st:
            QT = [self.sb(st, "dQT", [128, T], BF16) for _ in range(2)]
            KTt = [self.sb(st, "dKT", [128, T], BF16) for _ in range(4)]
            for h in range(4):
                P.dve(lambda h=h: nc.vector.memset(KTt[h][:], 0.0), w=[("KT", h)])
            V = self.sb(st, "dV", [128, NT, 4, 66], BF16)
            bias = self.sb(st, "dbias", [128, 5, 512], BF16)
            P.dve(lambda: nc.vector.memset(V[:], 1.0), w=[("V", tt) for tt in range(NT)])
            for dd in range(5):
                P.dma("pool", lambda dd=dd: nc.gpsimd.dma_start(out=bias[:, dd, :], in_=self.bias_d[l, dd]),
                      w=[("dbias", dd)])
            with ExitStack() as s2:
                w = self.load_w(s2, "wd", self.w_d[l], 768)
                wres = [("wd", k) for k in range(KT)]
                load = self.hn_blocks(s2)
                pp = [self.ps(s2, "pp", [128, 512]) for _ in range(4)]
                pi = 0
                for tb in range(T // 512):
                    hb, hres = load(tb)
                    sl = slice(tb * 512, (tb + 1) * 512)
                    for i in range(2):
                        p = pi % 4; pi += 1
                        self.proj_fm(pp[p], ("pp", p), w, wres, i * 128, 128, hb, hres)
                        P.act(lambda p=p, i=i, sl=sl: nc.scalar.mul(QT[i][:, sl], pp[p][:], 0.125),
                              r=[("pp", p)], w=[("QT", i, tb)])
                        p = pi % 4; pi += 1
                        self.proj_fm(pp[p], ("pp", p), w, wres, 256 + i * 128, 128, hb, hres)
                        P.dve(lambda p=p, i=i, sl=sl: nc.vector.tensor_copy(KTt[2 * i][0:64, sl], pp[p][0:64, :]),
                              r=[("pp", p)], w=[("KT", 2 * i)])
                        P.dve(lambda p=p, i=i, sl=sl: nc.vector.tensor_copy(KTt[2 * i + 1][64:128, sl], pp[p][64:128, :]),
                              r=[("pp", p)], w=[("KT", 2 * i + 1)])
                    for j in range(4):
                        if self.dbg2 == "noV":
                            continue
                        p = pi % 4; pi += 1
                        self.proj_tm(pp[p], ("pp", p), w, wres, 512, 256, hb, hres, j)
                        if self.dbg2 == "noVevac":
                            continue
                        self.v_evac(V, pp[p], ("pp", p), tb * 4 + j, j)
                P.flush()
            if self.dbg == "proj":
                return
            with ExitStack() as s3:
                out_fn = self.std_out(s3, "d", 768)

                def kts_fn(qt):
                    return list(range(max(0, qt - 4), qt + 1))

                def pre_fn(g, qt, kt):
                    if self.dbg2 == "nopre":
                        return []
                    return [(self.ident_b[:], bias[:, qt - kt, :], [])]

                def qk_fn(g, j, qt, kt):
                    return (KTt[j][:, kt * 128:(kt + 1) * 128],
                            QT[j // 2][:, qt * 128:(qt + 1) * 128])

                def v_fn(g, j, kt):
                    return V[:, kt, j, 0:65]
                self.attn_core(s3, "d", 1, kts_fn, pre_fn, qk_fn, v_fn, out_fn)
                P.flush()

    def rsqrt_pool(self, out_ap, in_ap, nh_ap, r, w):
        nc, P = self.nc, self.P
        P.pool(lambda: nc.gpsimd.tensor_tensor(out_ap, in_ap, nh_ap, ALU.pow), r=r, w=w)

    def mixer_b(self, l):
        nc, P = self.nc, self.P
        scale = 96.0 ** -0.5
        with ExitStack() as st:
            QTh = [self.sb(st, "bQT", [128, T], BF16) for _ in range(4)]
            KTh = [self.sb(st, "bKT", [128, T], BF16) for _ in range(4)]
            V = self.sb(st, "bV", [128, NT, 4, 66], BF16)
            dmask = self.sb(st, "bdm", [128, 512], BF16)
            P.dve(lambda: nc.vector.memset(V[:], 1.0), w=[("V", tt) for tt in range(NT)])
            for h in range(4):
                P.dve(lambda h=h: nc.vector.memset(QTh[h][:], 0.0), w=[("QT", h), ("QTr", h)])
                P.dve(lambda h=h: nc.vector.memset(KTh[h][:], 0.0), w=[("KT", h), ("KTr", h)])
            P.dma("pool", lambda: nc.gpsimd.dma_start(out=dmask[:], in_=self.diagmask), w=["bdm"])
            with ExitStack() as s2:
                w = self.load_w(s2, "wb", self.w_b[l], 576)
                wres = [("wb", k) for k in range(KT)]
                wuq = self.load_w(s2, "wuq", self.w_uq[l], 384, nk=2)
                wuqr = self.load_w(s2, "wuqr", self.w_uqr[l], 384, nk=2)
                wkk = self.load_w(s2, "wkk", self.w_ukvk[l], 256, nk=1)
                wkv = self.load_w(s2, "wkv", self.w_ukvv[l], 256, nk=1)
                wu_res = [("wuq", 0), ("wuq", 1), ("wuqr", 0), ("wuqr", 1), ("wkk", 0), ("wkv", 0)]
                gq = self.sb(s2, "bgq", [128, 3], F32)
                nh = self.sb(s2, "bnh", [128, 512], F32)
                P.dma("sp", lambda: nc.sync.dma_start(out=gq[:, 0:2], in_=self.gq[l]), w=["bgq"])
                P.dma("sp", lambda: nc.sync.dma_start(out=gq[:, 2:3], in_=self.gkv[l]), w=["bgq"])
                P.dve(lambda: nc.vector.memset(nh[:], -0.5), w=["bnh"])
                load = self.hn_blocks(s2)
                rope = [self.sb(s2, "brope", [128, 2, 512], F32) for _ in range(2)]
                qd = [self.sb(s2, "bqd", [128, 3, 512], F32) for _ in range(2)]
                sq = [self.sb(s2, "bsq", [128, 3, 512], BF16) for _ in range(2)]
                ms = [self.sb(s2, "bms", [128, 2, 512], F32) for _ in range(2)]
                qn = [self.sb(s2, "bqn", [128, 3, 512], BF16) for _ in range(2)]
                t1 = [self.sb(s2, "bt1", [128, 512], F32) for _ in range(2)]
                t2 = [self.sb(s2, "bt2", [128, 512], F32) for _ in range(2)]
                hbs = {}
                pp = [self.ps(s2, "pp", [128, 512]) for _ in range(6)]
                pi = 0
                ri = 0
                pc = [0]
                rc = [0]
                def stage1(tb):
                    b = tb % 2
                    hb, hres = load(tb)
                    sl = slice(tb * 512, (tb + 1) * 512)
                    P.dma("sp", lambda b=b, sl=sl: nc.sync.dma_start(out=rope[b][64:96, :, :], in_=self.rope[64:96, :, sl]),
                          w=[("brope", b)])
                    for i in range(3):
                        p = pc[0] % 6; pc[0] += 1
                        self.proj_fm(pp[p], ("pp", p), w, wres, i * 128, 128, hb, hres)
                        P.act(lambda p=p, i=i, b=b: nc.scalar.copy(qd[b][:, i, :], pp[p][:]),
                              r=[("pp", p)], w=[("bqd", b, i)])
                    P.act(lambda b=b: nc.scalar.activation(out=sq[b][:], in_=qd[b][:], func=AF.Square),
                          r=[("bqd", b, i) for i in range(3)], w=[("bsq", b)])
                    p = pc[0] % 6; pc[0] += 1
                    for i in range(2):
                        P.pe(lambda p=p, i=i, b=b: nc.tensor.matmul(pp[p][:], self.ones_b[:], sq[b][:, i, :],
                                                                   start=(i == 0), stop=(i == 1)),
                             r=[("bsq", b), "onesb"], w=[("pp", p)])
                    P.dve(lambda p=p, b=b: nc.vector.tensor_scalar(ms[b][:, 0, :], pp[p][:], 1.0 / 256, EPS, ALU.mult, ALU.add),
                          r=[("pp", p)], w=[("bms", b, 0)])
                    p = pc[0] % 6; pc[0] += 1
                    P.pe(lambda p=p, b=b: nc.tensor.matmul(pp[p][:], self.ones_b[:], sq[b][:, 2, :], start=True, stop=True),
                         r=[("bsq", b), "onesb"], w=[("pp", p)])
                    P.dve(lambda p=p, b=b: nc.vector.tensor_scalar(ms[b][:, 1, :], pp[p][:], 1.0 / 128, EPS, ALU.mult, ALU.add),
                          r=[("pp", p)], w=[("bms", b, 1)])
                    for i in range(2):
                        P.act(lambda b=b, i=i: nc.scalar.activation(out=ms[b][:, i, :], in_=ms[b][:, i, :], func=AF.Sqrt),
                              r=[("bms", b, i)], w=[("bms", b, i)])
                        P.dve(lambda b=b, i=i: nc.vector.reciprocal(ms[b][:, i, :], ms[b][:, i, :]),
                              r=[("bms", b, i)], w=[("bms", b, i)])
                    for i in range(3):
                        P.dve(lambda b=b, i=i: nc.vector.scalar_tensor_tensor(
                            qn[b][:, i, :], qd[b][:, i, :], gq[:, i:i + 1], ms[b][:, 0 if i < 2 else 1, :],
                            ALU.mult, ALU.mult),
                            r=[("bqd", b, i), ("bms", b, 0 if i < 2 else 1), "bgq"], w=[("bqn", b, i)])
                    p1 = pc[0] % 6; pc[0] += 1
                    self.proj_fm(pp[p1], ("pp", p1), w, wres, 384, 96, hb, hres)
                    p2 = pc[0] % 6; pc[0] += 1
                    self.proj_fm(pp[p2], ("pp", p2), w, wres, 480, 96, hb, hres)
                    r = rc[0] % 2; rc[0] += 1
                    P.dve(lambda p1=p1, b=b, r=r: nc.vector.tensor_tensor(
                        t1[r][64:96, :], pp[p1][64:96, :], rope[b][64:96, 0, :], ALU.mult),
                        r=[("pp", p1), ("brope", b)], w=[("bt1", r)])
                    P.dve(lambda p2=p2, b=b, r=r: nc.vector.tensor_tensor(
                        t2[r][64:96, :], pp[p2][64:96, :], rope[b][64:96, 1, :], ALU.mult),
                        r=[("pp", p2), ("brope", b)], w=[("bt2", r)])
                    for h in range(4):
                        P.dve(lambda h=h, r=r, sl=sl: nc.vector.tensor_tensor(
                            KTh[h][64:96, sl], t1[r][64:96, :], t2[r][64:96, :], ALU.add),
                            r=[("bt1", r), ("bt2", r)], w=[("KTr", h)])

                def stage2(tb):
                    b = tb % 2
                    sl = slice(tb * 512, (tb + 1) * 512)
                    for h in range(4):
                        p1 = pc[0] % 6; pc[0] += 1
                        for i in range(2):
                            P.pe(lambda p1=p1, i=i, h=h, b=b: nc.tensor.matmul(
                                pp[p1][0:96, :], wuq[:, i, h * 96:(h + 1) * 96], qn[b][:, i, :],
                                start=(i == 0), stop=(i == 1)),
                                r=[("bqn", b, i)] + wu_res, w=[("pp", p1)])
                        p2 = pc[0] % 6; pc[0] += 1
                        for i in range(2):
                            P.pe(lambda p2=p2, i=i, h=h, b=b: nc.tensor.matmul(
                                pp[p2][0:96, :], wuqr[:, i, h * 96:(h + 1) * 96], qn[b][:, i, :],
                                start=(i == 0), stop=(i == 1)),
                                r=[("bqn", b, i)] + wu_res, w=[("pp", p2)])
                        P.act(lambda p1=p1, h=h, sl=sl: nc.scalar.mul(QTh[h][0:64, sl], pp[p1][0:64, :], scale),
                              r=[("pp", p1)], w=[("QT", h)])
                        r = rc[0] % 2; rc[0] += 1
                        P.dve(lambda p1=p1, b=b, r=r: nc.vector.scalar_tensor_tensor(
                            t1[r][64:96, :], pp[p1][64:96, :], scale, rope[b][64:96, 0, :], ALU.mult, ALU.mult),
                            r=[("pp", p1), ("brope", b)], w=[("bt1", r)])
                        P.dve(lambda p2=p2, b=b, r=r: nc.vector.scalar_tensor_tensor(
                            t2[r][64:96, :], pp[p2][64:96, :], scale, rope[b][64:96, 1, :], ALU.mult, ALU.mult),
                            r=[("pp", p2), ("brope", b)], w=[("bt2", r)])
                        P.dve(lambda h=h, r=r, sl=sl: nc.vector.tensor_tensor(
                            QTh[h][64:96, sl], t1[r][64:96, :], t2[r][64:96, :], ALU.add),
                            r=[("bt1", r), ("bt2", r)], w=[("QTr", h)])
                        p3 = pc[0] % 6; pc[0] += 1
                        P.pe(lambda p3=p3, h=h, b=b: nc.tensor.matmul(
                            pp[p3][0:64, :], wkk[:, 0, h * 64:(h + 1) * 64], qn[b][:, 2, :], start=True, stop=True),
                            r=[("bqn", b, 2)] + wu_res, w=[("pp", p3)])
                        P.dve(lambda p3=p3, h=h, sl=sl: nc.vector.tensor_copy(KTh[h][0:64, sl], pp[p3][0:64, :]),
                              r=[("pp", p3)], w=[("KT", h)])
                    for j in range(4):
                        p = pc[0] % 6; pc[0] += 1
                        P.pe(lambda p=p, j=j, b=b: nc.tensor.matmul(
                            pp[p][:, 0:256], qn[b][:, 2, j * 128:(j + 1) * 128], wkv[:, 0, :], start=True, stop=True),
                            r=[("bqn", b, 2)] + wu_res, w=[("pp", p)])
                        self.v_evac(V, pp[p], ("pp", p), tb * 4 + j, j)

                stage1(0)
                for tb in range(T // 512):
                    if tb + 1 < T // 512:
                        stage1(tb + 1)
                    stage2(tb)
                P.flush()
            with ExitStack() as s3:
                out_fn = self.std_out(s3, "b", 256)

                def kts_fn(qt):
                    return list(range(qt + 1))

                def pre_fn(g, qt, kt):
                    if kt == qt:
                        return [(self.ident_b[:], dmask[:], [])]
                    return []

                def qk_fn(g, j, qt, kt):
                    return (KTh[j][:, kt * 128:(kt + 1) * 128], QTh[j][:, qt * 128:(qt + 1) * 128])

                def v_fn(g, j, kt):
                    return V[:, kt, j, 0:65]
                self.attn_core(s3, "b", 1, kts_fn, pre_fn, qk_fn, v_fn, out_fn)
                P.flush()

    def mixer_a(self, l):
        nc, P = self.nc, self.P
        scale = 32.0 ** -0.5
        lam_init = 0.8 - 0.6 * math.exp(-0.3 * l)
        with ExitStack() as st:
            QTa = [self.sb(st, "aQT", [128, T], BF16) for _ in range(4)]
            KTa = [[self.sb(st, "aKT", [128, T], BF16) for _ in range(2)] for _ in range(4)]
            V = self.sb(st, "aV", [128, NT, 4, 66], BF16)
            corr = self.sb(st, "acorr", [128, 2, 512], BF16)
            neglam = self.sb(st, "aneglam", [128, 1], F32)
            gAb = self.sb(st, "agAb", [128, 64], F32)
            nh = self.sb(st, "anh", [128, 2], F32)
            P.dve(lambda: nc.vector.memset(V[:], 1.0), w=[("V", tt) for tt in range(NT)])
            P.dve(lambda: nc.vector.memset(nh[:], -0.5), w=["anh"])
            for h in range(4):
                P.dve(lambda h=h: nc.vector.memset(QTa[h][:], 0.0), w=[("QT", h)])
                for c in range(2):
                    P.dve(lambda h=h, c=c: nc.vector.memset(KTa[h][c][:], 0.0), w=[("KT", h, c)])
            for g in range(2):
                P.dma("pool", lambda g=g: nc.gpsimd.dma_start(out=corr[:, g, :], in_=self.corr_a[g]), w=[("acorr", g)])
            for h in range(4):
                P.dma("pool", lambda h=h: nc.gpsimd.dma_start(out=QTa[h][64:67, :], in_=self.augq[h]),
                      r=[("QT", h)], w=[("QTaug", h)])
                for c in range(2):
                    P.dma("pool", lambda h=h, c=c: nc.gpsimd.dma_start(out=KTa[h][c][64:67, :], in_=self.augk[h]),
                          r=[("KT", h, c)], w=[("KTaug", h, c)])
            with ExitStack() as s2:
                lv = self.sb(s2, "alv", [128, 128], F32)
                pr = self.sb(s2, "apr", [128, 64], F32)
                s12 = self.sb(s2, "as12", [128, 2], F32)
                P.dma("sp", lambda: nc.sync.dma_start(out=lv[:], in_=self.dlam[l]), w=["alv"])
                P.dma("sp", lambda: nc.sync.dma_start(out=gAb[:], in_=self.dng[l]), w=["agAb"])
                P.dve(lambda: nc.vector.tensor_tensor(pr[:, 0:32], lv[:, 0:32], lv[:, 32:64], ALU.mult), r=["alv"], w=["apr0"])
                P.dve(lambda: nc.vector.tensor_tensor(pr[:, 32:64], lv[:, 64:96], lv[:, 96:128], ALU.mult), r=["alv"], w=["apr1"])
                P.dve(lambda: nc.vector.reduce_sum(out=s12[:], in_=pr[:, :].rearrange("p (a b) -> p a b", a=2), axis=AX.X),
                      r=["apr0", "apr1"], w=["as12"])
                P.act(lambda: nc.scalar.activation(out=s12[:], in_=s12[:], func=AF.Exp), r=["as12"], w=["as12"])
                P.dve(lambda: nc.vector.tensor_tensor(neglam[:], s12[:, 1:2], s12[:, 0:1], ALU.subtract), r=["as12"], w=["aneglam"])
                P.dve(lambda: nc.vector.tensor_scalar(neglam[:], neglam[:], -lam_init, None, ALU.add), r=["aneglam"], w=["aneglam"])
                P.dve(lambda: nc.vector.tensor_scalar(gAb[:], gAb[:], 1.0 - lam_init, None, ALU.mult), r=["agAb"], w=["agAb"])
                w = self.load_w(s2, "wa", self.w_a[l], 768)
                wres = [("wa", k) for k in range(KT)]
                load = self.hn_blocks(s2)
                pp = [self.ps(s2, "pp", [128, 512]) for _ in range(4)]
                pi = 0
                for tb in range(T // 512):
                    hb, hres = load(tb)
                    sl = slice(tb * 512, (tb + 1) * 512)
                    for h in range(4):
                        p = pi % 4; pi += 1
                        self.proj_fm(pp[p], ("pp", p), w, wres, h * 64, 64, hb, hres)
                        P.act(lambda p=p, h=h, sl=sl: nc.scalar.mul(QTa[h][0:64, sl], pp[p][0:64, :], scale),
                              r=[("pp", p)], w=[("QT", h)])
                        p = pi % 4; pi += 1
                        self.proj_fm(pp[p], ("pp", p), w, wres, 256 + h * 64, 64, hb, hres)
                        P.dve(lambda p=p, h=h, sl=sl: nc.vector.tensor_copy(KTa[h][0][0:32, sl], pp[p][0:32, :]),
                              r=[("pp", p)], w=[("KT", h, 0)])
                        P.dve(lambda p=p, h=h, sl=sl: nc.vector.tensor_copy(KTa[h][1][32:64, sl], pp[p][32:64, :]),
                              r=[("pp", p)], w=[("KT", h, 1)])
                    for j in range(4):
                        p = pi % 4; pi += 1
                        self.proj_tm(pp[p], ("pp", p), w, wres, 512, 256, hb, hres, j)
                        self.v_evac(V, pp[p], ("pp", p), tb * 4 + j, j)
                P.flush()
            with ExitStack() as s3:
                onA = [self.sb(s3, "aon", [128, 4, 64], F32) for _ in range(2)]
                cmb = [self.sb(s3, "acmb", [128, 2, 64], F32) for _ in range(2)]
                sqt = [self.sb(s3, "asq", [128, 2, 64], F32) for _ in range(2)]
                ss = [self.sb(s3, "ass", [128, 2], F32) for _ in range(2)]
                onb = [self.sb(s3, "aonb", [128, 128], BF16) for _ in range(2)]
                tp = [self.ps(s3, "atp", [128, 128], BF16) for _ in range(2)]
                stage = [self.sb(s3, "astg", [128, 2, 512], BF16) for _ in range(2)]
                oTv = self.oT[0:256, :].rearrange("(i p) t -> p i t", p=128)
                cnt = [0]

                def out_fn(g, qt, O, Or, rl, rlr):
                    nb = cnt[0] % 2
                    cnt[0] += 1
                    sbi = (qt // 4) % 2
                    q4 = qt % 4
                    for j in range(4):
                        P.dve(lambda j=j: nc.vector.tensor_scalar(
                            onA[nb][:, j, :], O[:, j * 66:j * 66 + 64], rl[:, j:j + 1], None, ALU.mult),
                            r=[Or, rlr], w=[("aon", nb, j)])
                    for hh in range(2):
                        P.dve(lambda hh=hh: nc.vector.scalar_tensor_tensor(
                            cmb[nb][:, hh, :], onA[nb][:, 2 * hh + 1, :], neglam[:, 0:1], onA[nb][:, 2 * hh, :],
                            ALU.mult, ALU.add),
                            r=[("aon", nb, 2 * hh), ("aon", nb, 2 * hh + 1)], w=[("acmb", nb, hh)])
                    P.dve(lambda: nc.vector.tensor_tensor(sqt[nb][:], cmb[nb][:], cmb[nb][:], ALU.mult),
                          r=[("acmb", nb, 0), ("acmb", nb, 1)], w=[("asq", nb)])
                    P.dve(lambda: nc.vector.reduce_sum(out=ss[nb][:], in_=sqt[nb][:], axis=AX.X),
                          r=[("asq", nb)], w=[("ass", nb)])
                    P.dve(lambda: nc.vector.tensor_scalar(ss[nb][:], ss[nb][:], 1.0 / 64, EPS, ALU.mult, ALU.add),
                          r=[("ass", nb)], w=[("ass", nb)])
                    self.rsqrt_pool(ss[nb][:], ss[nb][:], nh[:], [("ass", nb)], [("ass", nb)])
                    for hh in range(2):
                        P.dve(lambda hh=hh: nc.vector.scalar_tensor_tensor(
                            onb[nb][:, hh * 64:(hh + 1) * 64], cmb[nb][:, hh, :], ss[nb][:, hh:hh + 1], gAb[:],
                            ALU.mult, ALU.mult),
                            r=[("acmb", nb, hh), ("ass", nb)], w=[("aonb", nb, hh)])
                    P.pe(lambda: nc.tensor.transpose(tp[nb][:], onb[nb][:], self.ident_b[:]),
                         r=[("aonb", nb, 0), ("aonb", nb, 1)], w=[("atp", nb)])
                    P.dve(lambda: nc.vector.tensor_copy(stage[sbi][:, g, q4 * 128:(q4 + 1) * 128], tp[nb][:]),
                          r=[("atp", nb)], w=[("astg", sbi, g)])
                    if q4 == 3 and g == 1:
                        tb = qt // 4
                        P.dma("sp", lambda: nc.sync.dma_start(out=oTv[:, :, tb * 512:(tb + 1) * 512], in_=stage[sbi][:]),
                              r=[("astg", sbi, 0), ("astg", sbi, 1)], w=[("dram", "oT", "a", tb)])

                def kts_fn(qt):
                    return list(range(qt + 1))

                def pre_fn(g, qt, kt):
                    if kt == qt:
                        return [(self.ident_b[:], corr[:, g, :], [])]
                    return []

                def qk_fn(g, j, qt, kt):
                    h = 2 * g + j // 2
                    return (KTa[h][j % 2][:, kt * 128:(kt + 1) * 128], QTa[h][:, qt * 128:(qt + 1) * 128])

                def v_fn(g, j, kt):
                    return V[:, kt, 2 * g + j // 2, 0:65]
                self.attn_core(s3, "a", 2, kts_fn, pre_fn, qk_fn, v_fn, out_fn)
                P.flush()

    def mixer_c(self, l):
        nc, P = self.nc, self.P
        NR = int(os.environ.get("MK_NR", "14"))
        with ExitStack() as st:
            QTc = [self.sb(st, "cQT", [128, T], BF16) for _ in range(4)]
            KTc = [self.sb(st, "cKT", [128, T], BF16) for _ in range(4)]
            V = self.sb(st, "cV", [128, NT, 4, 66], BF16)
            qiT = [self.sb(st, "cqi", [128, T], BF16) for _ in range(2)]
            kiT = self.sb(st, "cki", [128, T], BF16)
            wsb = self.sb(st, "cw", [128, NT, 8], F32)
            corr = self.sb(st, "ccorr", [128, 512], BF16)
            id4 = self.sb(st, "cid4", [128, 512], BF16)
            P.dve(lambda: nc.vector.memset(V[:], 1.0), w=[("V", tt) for tt in range(NT)])
            for h in range(4):
                P.dve(lambda h=h: nc.vector.memset(QTc[h][:], 0.0), w=[("QT", h)])
                P.dve(lambda h=h: nc.vector.memset(KTc[h][:], 0.0), w=[("KT", h)])
            P.dma("pool", lambda: nc.gpsimd.dma_start(out=corr[:], in_=self.corr_c), w=["ccorr"])
            P.dma("pool", lambda: nc.gpsimd.dma_start(out=id4[:], in_=self.ident4), w=["cid4"])
            for h in range(4):
                P.dma("pool", lambda h=h: nc.gpsimd.dma_start(out=QTc[h][64:67, :], in_=self.augq[h]),
                      r=[("QT", h)], w=[("QTaug", h)])
                P.dma("pool", lambda h=h: nc.gpsimd.dma_start(out=KTc[h][64:67, :], in_=self.augk[h]),
                      r=[("KT", h)], w=[("KTaug", h)])
            with ExitStack() as s2:
                w = self.load_w(s2, "wc", self.w_c[l], 1160)
                wres = [("wc", k) for k in range(KT)]
                load = self.hn_blocks(s2)
                pp = [self.ps(s2, "pp", [128, 512]) for _ in range(4)]
                pi = 0
                for tb in range(T // 512):
                    hb, hres = load(tb)
                    sl = slice(tb * 512, (tb + 1) * 512)
                    for h in range(4):
                        p = pi % 4; pi += 1
                        self.proj_fm(pp[p], ("pp", p), w, wres, h * 64, 64, hb, hres)
                        P.act(lambda p=p, h=h, sl=sl: nc.scalar.mul(QTc[h][0:64, sl], pp[p][0:64, :], 0.125),
                              r=[("pp", p)], w=[("QT", h)])
                        p = pi % 4; pi += 1
                        self.proj_fm(pp[p], ("pp", p), w, wres, 256 + h * 64, 64, hb, hres)
                        P.dve(lambda p=p, h=h, sl=sl: nc.vector.tensor_copy(KTc[h][0:64, sl], pp[p][0:64, :]),
                              r=[("pp", p)], w=[("KT", h)])
                    for i in range(2):
                        p = pi % 4; pi += 1
                        self.proj_fm(pp[p], ("pp", p), w, wres, 768 + i * 128, 128, hb, hres)
                        P.act(lambda p=p, i=i, sl=sl: nc.scalar.copy(qiT[i][:, sl], pp[p][:]),
                              r=[("pp", p)], w=[("cqi", i)])
                    p = pi % 4; pi += 1
                    self.proj_fm(pp[p], ("pp", p), w, wres, 1024, 128, hb, hres)
                    P.dve(lambda p=p, sl=sl: nc.vector.tensor_copy(kiT[:, sl], pp[p][:]), r=[("pp", p)], w=["cki"])
                    for j in range(4):
                        p = pi % 4; pi += 1
                        self.proj_tm(pp[p], ("pp", p), w, wres, 512, 256, hb, hres, j)
                        self.v_evac(V, pp[p], ("pp", p), tb * 4 + j, j)
                        p = pi % 4; pi += 1
                        self.proj_tm(pp[p], ("pp", p), w, wres, 1152, 8, hb, hres, j)
                        P.dve(lambda p=p, tt=tb * 4 + j: nc.vector.tensor_copy(wsb[:, tt, :], pp[p][:, 0:8]),
                              r=[("pp", p)], w=["cw"])
                P.flush()
            with ExitStack() as s3:
                out_fn = self.std_out(s3, "c", 512, ntp=1)
                scores2 = [self.sb(s3, "cscore", [128, T], F32) for _ in range(2)]
                junk = self.sb(s3, "cjunk", [128, T], BF16)
                Mb = [self.sb(s3, "cMb", [128, T], BF16) for _ in range(2)]
                Dg = [[self.sb(s3, "cDg", [128, 128], BF16) for _ in range(8)] for _ in range(2)]
                Qb = [[self.sb(s3, "cQb", [128, 128], BF16) for _ in range(8)] for _ in range(2)]
                R = [self.sb(s3, "cR", [128, 512], BF16) for _ in range(2)]
                X = [self.ps(s3, "cX", [128, 512]) for _ in range(2)]
                SC = self.ps(s3, "cSC", [128, 512])
                st4 = self.sb(s3, "cst", [128, 4 * (NR + 2)], F32)
                thrc = self.sb(s3, "cthrc", [128, 1], F32)
                steps = self.sb(s3, "csteps", [128, NR + 1], F32)
                cpow = self.sb(s3, "ccpow", [128, NR + 1], F32)
                for r in range(NR + 1):
                    P.pool(lambda r=r: nc.gpsimd.memset(cpow[:, r:r + 1], 2.0 ** -(r + 1)), w=["ccpow"])
                P.dve(lambda: nc.vector.memset(thrc[:], -1.0e30), w=["cthrc"])
                for b in range(2):
                    for ih in range(8):
                        P.pool(lambda b=b, ih=ih: nc.gpsimd.memset(Qb[b][ih][:], 0.0), w=[("cQb", b, ih)])
                xi = [0]
                LO, MID, CNT, STP = 0, NR + 2, 2 * (NR + 2), 3 * (NR + 2)

                def before_qt(qt):
                    if qt == 0:
                        do_scores(0)
                        do_scores(1)
                        do_select(0)
                    if qt + 2 < NT:
                        do_scores(qt + 2)
                    if qt + 1 < NT:
                        do_select(qt + 1)

                def do_scores(qt):
                    n = 128 * (qt + 1)
                    b = qt % 2
                    score = scores2[b]
                    qs = slice(qt * 128, (qt + 1) * 128)
                    for ih in range(8):
                        r0 = 32 * (ih % 4)
                        P.pool(lambda ih=ih, r0=r0: nc.gpsimd.tensor_copy(
                            Qb[b][ih][r0:r0 + 32, :], qiT[ih // 4][r0:r0 + 32, qs]), w=[("cQb", b, ih)])
                        P.pool(lambda ih=ih: nc.gpsimd.tensor_scalar(
                            Dg[b][ih][:], self.ident_b[:], wsb[:, qt, ih:ih + 1], None, ALU.mult),
                            w=[("cDg", b, ih)])
                    nkb = (n + 511) // 512
                    for kb in range(nkb):
                        wd = min(512, n - 512 * kb)
                        ks = slice(512 * kb, 512 * kb + wd)
                        xs = []
                        for ih in range(8):
                            xs.append(xi[0] % 2)
                            xi[0] += 1

                        def emit_x(ih, ks=ks, wd=wd):
                            x = xs[ih]
                            P.pe(lambda x=x, ih=ih, ks=ks, wd=wd: nc.tensor.matmul(
                                X[x][:, 0:wd], Qb[b][ih][:], kiT[:, ks], start=True, stop=True),
                                r=[("cQb", b, ih)], w=[("cX", x)])
                        emit_x(0)
                        for ih in range(8):
                            x = xs[ih]
                            P.act(lambda x=x, wd=wd: nc.scalar.activation(out=R[x][:, 0:wd], in_=X[x][:, 0:wd], func=AF.Relu),
                                  r=[("cX", x)], w=[("cR", x)])
                            if ih + 1 < 8:
                                emit_x(ih + 1)
                            P.pe(lambda ih=ih, x=x, wd=wd: nc.tensor.matmul(
                                SC[:, 0:wd], Dg[b][ih][:], R[x][:, 0:wd], start=(ih == 0), stop=(ih == 7)),
                                r=[("cR", x), ("cDg", b, ih)], w=["cSC"])
                        P.act(lambda ks=ks, wd=wd: nc.scalar.copy(score[:, ks], SC[:, 0:wd]),
                              r=["cSC"], w=[("cscore", b)])

                def do_select(qt):
                    n = 128 * (qt + 1)
                    b = qt % 2
                    score = scores2[b]
                    P.dve(lambda: nc.vector.memset(score[0:64, n - 64:n], -3.0e38), r=[("cscore", b)], w=[("cscore", b)])
                    if qt >= 2:
                        W0 = CNT + NR + 1
                        MX = MID
                        P.dve(lambda: nc.vector.tensor_reduce(out=st4[:, MX:MX + 1], in_=score[:, 0:n], axis=AX.X, op=ALU.max),
                              r=[("cscore", b)], w=["cmx"])
                        P.dve(lambda: nc.vector.tensor_reduce(out=st4[:, STP:STP + 1], in_=score[:, 0:n - 64], axis=AX.X, op=ALU.min),
                              r=[("cscore", b)], w=["cmn"])
                        P.dve(lambda: nc.vector.tensor_tensor(st4[:, W0:W0 + 1], st4[:, MX:MX + 1], st4[:, STP:STP + 1], ALU.subtract),
                              r=["cmx", "cmn"], w=["cw0"])
                        P.dve(lambda: nc.vector.tensor_scalar(steps[:], cpow[:], st4[:, W0:W0 + 1], None, ALU.mult),
                              r=["cw0", "ccpow"], w=["csteps"])
                        P.dve(lambda: nc.vector.tensor_tensor(st4[:, LO:LO + 1], st4[:, STP:STP + 1], steps[:, 0:1], ALU.add),
                              r=["cmn", "csteps"], w=[("clo", 0)])
                        for r in range(NR):
                            P.dve(lambda r=r: nc.vector.tensor_scalar(
                                junk[:, 0:n], score[:, 0:n], st4[:, LO + r:LO + r + 1], 0.0, ALU.is_gt, ALU.add,
                                accum_out=st4[:, CNT + r:CNT + r + 1]),
                                r=[("cscore", b), ("clo", r)], w=[("ccnt", r), "cjunk"])
                            P.dve(lambda r=r: nc.vector.tensor_scalar(
                                st4[:, STP + 1 + r:STP + 2 + r], st4[:, CNT + r:CNT + r + 1], 255.5, 0.5, ALU.is_gt, ALU.subtract),
                                r=[("ccnt", r)], w=[("cstp", r)])
                            P.dve(lambda r=r: nc.vector.scalar_tensor_tensor(
                                st4[:, LO + r + 1:LO + r + 2], st4[:, STP + 1 + r:STP + 2 + r], steps[:, r:r + 1],
                                st4[:, LO + r:LO + r + 1], ALU.mult, ALU.add),
                                r=[("cstp", r), ("clo", r), "csteps"], w=[("clo", r + 1)])
                        P.dve(lambda: nc.vector.tensor_tensor(st4[:, MX:MX + 1], st4[:, LO + NR:LO + NR + 1], steps[:, NR:NR + 1], ALU.subtract),
                              r=[("clo", NR), "csteps"], w=["cthr"])
                        thr = st4[:, MX:MX + 1]
                        thr_r = ["cthr"]
                    else:
                        thr = thrc[:, 0:1]
                        thr_r = ["cthrc"]
                    P.dve(lambda: nc.vector.tensor_scalar(Mb[b][:, 0:n], score[:, 0:n], thr, NEG, ALU.is_le, ALU.mult),
                          r=[("cscore", b)] + thr_r, w=[("cMb", b)])

                def kts_fn(qt):
                    return list(range(qt + 1))

                def pre_fn(g, qt, kt):
                    b = qt % 2
                    pre = [(Mb[b][:, kt * 128:(kt + 1) * 128], id4[:], [("cMb", b)])]
                    if kt == qt:
                        pre.append((self.ident_b[:], corr[:], []))
                    return pre

                def qk_fn(g, j, qt, kt):
                    return (KTc[j][:, kt * 128:(kt + 1) * 128], QTc[j][:, qt * 128:(qt + 1) * 128])

                def v_fn(g, j, kt):
                    return V[:, kt, j, 0:65]
                self.attn_core(s3, "c", 1, kts_fn, pre_fn, qk_fn, v_fn, out_fn, before_qt=before_qt, ns=2)
                P.flush()

    def phase_wout(self, l):
        nc, P = self.nc, self.P
        goff = KT
        shoff = 24
        with ExitStack() as st:
            wo = self.load_w(st, "wo", self.w_out[l], D)
            wres = [("wo", k) for k in range(KT)]
            ob = [self.sb(st, "wob", [128, KT, 512], BF16) for _ in range(2)]
            xb = [self.sb(st, "wxb", [128, KT, 512], F32) for _ in range(2)]
            sq = [self.sb(st, "wsq", [128, KT, 512], BF16) for _ in range(2)]
            ms = [self.sb(st, "wms", [128, 512], F32) for _ in range(2)]
            tmp = [self.sb(st, "wtmp", [128, KT, 512], F32) for _ in range(2)]
            hb = [self.sb(st, "whb", [128, KT, 512], BF16) for _ in range(2)]
            py = [self.ps(st, "wpy", [128, 512]) for _ in range(2)]
            pss = [self.ps(st, "wps", [128, 512]) for _ in range(2)]
            oTv = self.oT.rearrange("(kt p) t -> p kt t", p=128)
            xTv = self.xT.rearrange("(kt p) t -> p kt t", p=128)
            hTv = self.hnT.rearrange("(kt p) t -> p kt t", p=128)
            pc = [0]

            def stage_a(tb):
                b = tb % 2
                sl = slice(tb * 512, (tb + 1) * 512)
                P.dma("sp", lambda: nc.sync.dma_start(out=ob[b][:], in_=oTv[:, :, sl]), w=[("wob", b)])
                P.dma("sp", lambda: nc.sync.dma_start(out=xb[b][:], in_=xTv[:, :, sl]),
                      r=[("dram", "xT", tb)], w=[("wxb", b, f) for f in range(KT)])
                for f in range(KT):
                    p = pc[0] % 2
                    pc[0] += 1
                    for k in range(KT):
                        P.pe(lambda f=f, k=k, p=p: nc.tensor.matmul(
                            py[p][:], wo[:, k, f * 128:(f + 1) * 128], ob[b][:, k, :],
                            start=(k == 0), stop=(k == KT - 1)),
                            r=[("wob", b)] + wres, w=[("wpy", p)])
                    P.dve(lambda f=f, p=p: nc.vector.scalar_tensor_tensor(
                        xb[b][:, f, :], py[p][:], self.modv[:, 16 + f:17 + f], xb[b][:, f, :], ALU.mult, ALU.add),
                        r=[("wpy", p), ("wxb", b, f)], w=[("wxb", b, f)])
                P.dma("sp", lambda: nc.sync.dma_start(out=xTv[:, :, sl], in_=xb[b][:]),
                      r=[("wxb", b, f) for f in range(KT)], w=[("dram", "xT", tb)])

            def stage_b(tb):
                b = tb % 2
                sl = slice(tb * 512, (tb + 1) * 512)
                xres = [("wxb", b, f) for f in range(KT)]
                P.act(lambda: nc.scalar.activation(out=sq[b][:], in_=xb[b][:], func=AF.Square),
                      r=xres, w=[("wsq", b)])
                for kt in range(KT):
                    P.pe(lambda kt=kt: nc.tensor.matmul(
                        pss[b][:], self.ones_b[:], sq[b][:, kt, :], start=(kt == 0), stop=(kt == KT - 1)),
                        r=[("wsq", b), "onesb"], w=[("wps", b)])
                P.dve(lambda: nc.vector.tensor_scalar(ms[b][:], pss[b][:], 1.0 / D, EPS, ALU.mult, ALU.add),
                      r=[("wps", b)], w=[("wms", b)])
                P.act(lambda: nc.scalar.activation(out=ms[b][:], in_=ms[b][:], func=AF.Sqrt),
                      r=[("wms", b)], w=[("wms", b)])
                P.dve(lambda: nc.vector.reciprocal(ms[b][:], ms[b][:]), r=[("wms", b)], w=[("wms", b)])
                for kt in range(KT):
                    P.dve(lambda kt=kt: nc.vector.scalar_tensor_tensor(
                        tmp[b][:, kt, :], xb[b][:, kt, :], self.gvec[:, goff + kt:goff + kt + 1],
                        ms[b][:], ALU.mult, ALU.mult),
                        r=[("wxb", b, kt), ("wms", b), "gvec1"], w=[("wtmp", b, kt)])
                    P.act(lambda kt=kt: nc.scalar.activation(
                        out=hb[b][:, kt, :], in_=tmp[b][:, kt, :], func=AF.Identity,
                        bias=self.modv[:, shoff + kt:shoff + kt + 1], scale=1.0),
                        r=[("wtmp", b, kt), "modv"], w=[("whb", b, kt)])
                P.dma("sp", lambda: nc.sync.dma_start(out=hTv[:, :, sl], in_=hb[b][:]),
                      r=[("whb", b, kt) for kt in range(KT)], w=[("dram", "hnT", tb)])

            stage_a(0)
            for tb in range(T // 512):
                if tb + 1 < T // 512:
                    stage_a(tb + 1)
                stage_b(tb)
            P.flush()

    def phase_ffn(self, l, moe):
        nc, P = self.nc, self.P
        E = NEXP if moe else 1
        NJ = (D_FFE if moe else D_FF) // 128
        NH = 2
        JH = NJ // NH
        w13 = self.moe_w13 if moe else self.ffn_w13
        w2 = self.moe_w2 if moe else self.ffn_w2
        NB = 1024
        with ExitStack() as st:
            hb = self.sb(st, "fhb", [128, KT, NB], BF16)
            xb = self.sb(st, "fxb", [128, KT, NB], F32)
            g = self.sb(st, "fg", [128, JH, NB], BF16)
            w2e = [self.sb(st, "fw2", [128, JH, D], BF16) for _ in range(2)]
            wj = [self.sb(st, "fw13", [128, 2, KT, 128], BF16) for _ in range(6)]
            sl_t = [self.sb(st, "fsl", [128, 512], BF16) for _ in range(2)]
            pa = [self.ps(st, "fpa", [128, 512]) for _ in range(2)]
            pb = [self.ps(st, "fpb", [128, 512]) for _ in range(2)]
            pyy = [self.ps(st, "fpy", [128, 512]) for _ in range(2)]
            ytmp = [self.sb(st, "fyt", [128, 512], F32) for _ in range(2)]
            hTv = self.hnT.rearrange("(kt p) t -> p kt t", p=128)
            xTv = self.xT.rearrange("(kt p) t -> p kt t", p=128)
            if moe:
                cb = self.sb(st, "fcb", [128, E, NB], BF16)
                rt = self.sb(st, "frt", [128, KT, 8], BF16)
                lg = self.sb(st, "flg", [128, 8], F32)
                m8 = self.sb(st, "fm8", [128, 8], F32)
                sc4 = self.sb(st, "fsc4", [128, 8], F32)
                c1 = self.sb(st, "fc1", [128, 8], F32)
                c2 = self.sb(st, "fc2", [128, 8], F32)
                dg = [self.sb(st, "fdg", [128, 128], BF16) for _ in range(2)]
                plg = self.ps(st, "fplg", [128, 512])
                pcb = self.ps(st, "fpcb", [128, 512])
                for k in range(KT):
                    P.dma("pool", lambda k=k: nc.gpsimd.dma_start(out=rt[:, k, :], in_=self.router[k]), w=["frt"])
            wi = 0
            ai = 0
            yi = 0
            w2i = 0
            for tb in range(T // NB):
                sl = slice(tb * NB, (tb + 1) * NB)
                P.dma("sp", lambda sl=sl: nc.sync.dma_start(out=hb[:], in_=hTv[:, :, sl]), w=["fhb"])
                P.dma("sp", lambda sl=sl: nc.sync.dma_start(out=xb[:], in_=xTv[:, :, sl]),
                      w=[("fxb", f, s) for f in range(KT) for s in range(2)])
                if moe:
                    for tt in range(NB // 128):
                        for k in range(KT):
                            P.pe(lambda k=k, tt=tt: nc.tensor.matmul(
                                plg[:, 0:8], hb[:, k, tt * 128:(tt + 1) * 128], rt[:, k, :],
                                start=(k == 0), stop=(k == KT - 1)), r=["fhb", "frt"], w=["fplg"])
                        P.dve(lambda: nc.vector.tensor_copy(lg[:], plg[:, 0:8]), r=["fplg"], w=["flg"])
                        P.dve(lambda: nc.vector.max(out=m8[:], in_=lg[:]), r=["flg"], w=["fm8"])
                        P.dve(lambda: nc.vector.tensor_tensor(sc4[:, 0:1], m8[:, 1:2], m8[:, 0:1], ALU.subtract),
                              r=["fm8"], w=["fsc4a"])
                        P.act(lambda: nc.scalar.activation(out=sc4[:, 1:2], in_=sc4[:, 0:1], func=AF.Exp),
                              r=["fsc4a"], w=["fsc4b"])
                        P.dve(lambda: nc.vector.tensor_scalar(sc4[:, 2:3], sc4[:, 1:2], 1.0, None, ALU.add),
                              r=["fsc4b"], w=["fsc4c"])
                        P.dve(lambda: nc.vector.reciprocal(sc4[:, 3:4], sc4[:, 2:3]), r=["fsc4c"], w=["fsc4d"])
                        P.dve(lambda: nc.vector.tensor_tensor(sc4[:, 4:5], sc4[:, 1:2], sc4[:, 3:4], ALU.mult),
                              r=["fsc4b", "fsc4d"], w=["fsc4e"])
                        P.dve(lambda: nc.vector.tensor_scalar(c1[:], lg[:], m8[:, 0:1], sc4[:, 3:4], ALU.is_equal, ALU.mult),
                              r=["flg", "fm8", "fsc4d"], w=["fc1"])
                        P.dve(lambda: nc.vector.tensor_scalar(c2[:], lg[:], m8[:, 1:2], sc4[:, 4:5], ALU.is_equal, ALU.mult),
                              r=["flg", "fm8", "fsc4e"], w=["fc2"])
                        P.dve(lambda: nc.vector.tensor_tensor(c1[:], c1[:], c2[:], ALU.add),
                              r=["fc1", "fc2"], w=["fc1"])
                        for e in range(E):
                            d = e % 2
                            P.dve(lambda e=e, d=d: nc.vector.tensor_scalar(
                                dg[d][:], self.ident_b[:], c1[:, e:e + 1], None, ALU.mult),
                                r=["fc1", "identb"], w=[("fdg", d)])
                            P.pe(lambda e=e, d=d: nc.tensor.matmul(
                                pcb[:, (e % 4) * 128:(e % 4 + 1) * 128], self.ones_b[:], dg[d][:],
                                start=True, stop=True, skip_group_check=True),
                                r=[("fdg", d), "onesb"], w=["fpcb"])
                            if e % 4 == 3:
                                for q in range(4):
                                    ee = e - 3 + q
                                    P.dve(lambda ee=ee, q=q, tt=tt: nc.vector.tensor_copy(
                                        cb[:, ee, tt * 128:(tt + 1) * 128], pcb[:, q * 128:(q + 1) * 128]),
                                        r=["fpcb"], w=[("fcb", ee)])
                for e in range(E):
                    for hh in range(NH):
                        wb2 = w2i % 2; w2i += 1
                        for q in range(JH):
                            P.dma("pool", lambda e=e, hh=hh, q=q, wb2=wb2: nc.gpsimd.dma_start(
                                out=w2e[wb2][:, q, :], in_=w2[l // 2 if moe else 0, e, hh * JH + q]),
                                w=[("fw2", wb2)])
                        for jj in range(JH):
                            j = hh * JH + jj
                            wbi = wi % 6; wi += 1
                            for m in range(2):
                                P.dma("pool", lambda e=e, j=j, m=m, wbi=wbi: nc.gpsimd.dma_start(
                                    out=wj[wbi][:, m, :, :], in_=w13[l // 2 if moe else 0, e, j, m]),
                                    w=[("fw13", wbi)])
                            for s in range(2):
                                a = ai % 2; ai += 1
                                cs = slice(s * 512, (s + 1) * 512)
                                for k in range(KT):
                                    P.pe(lambda k=k, a=a, wbi=wbi, cs=cs: nc.tensor.matmul(
                                        pa[a][:], wj[wbi][:, 0, k, :], hb[:, k, cs], start=(k == 0), stop=(k == KT - 1)),
                                        r=["fhb", ("fw13", wbi)], w=[("fpa", a)])
                                for k in range(KT):
                                    P.pe(lambda k=k, a=a, wbi=wbi, cs=cs: nc.tensor.matmul(
                                        pb[a][:], wj[wbi][:, 1, k, :], hb[:, k, cs], start=(k == 0), stop=(k == KT - 1)),
                                        r=["fhb", ("fw13", wbi)], w=[("fpb", a)])
                                if "ffn2" in self.dbg2s:
                                    continue
                                P.act(lambda a=a: nc.scalar.activation(out=sl_t[a][:], in_=pa[a][:], func=AF.Silu),
                                      r=[("fpa", a)], w=[("fsl", a)])
                                if "ffn3" in self.dbg2s:
                                    continue
                                if moe:
                                    P.dve(lambda a=a, e=e, cs=cs: nc.vector.tensor_tensor(
                                        sl_t[a][:], sl_t[a][:], cb[:, e, cs], ALU.mult),
                                        r=[("fsl", a), ("fcb", e)], w=[("fsl", a)])
                                P.dve(lambda a=a, jj=jj, cs=cs: nc.vector.tensor_tensor(
                                    g[:, jj, cs], pb[a][:], sl_t[a][:], ALU.mult),
                                    r=[("fpb", a), ("fsl", a)], w=[("fg", jj, s)])
                        for f in range(KT):
                            if self.dbg2s & {"ffn2", "ffn3", "ffn4"}:
                                break
                            for s in range(2):
                                y = yi % 2; yi += 1
                                cs = slice(s * 512, (s + 1) * 512)
                                for jj in range(JH):
                                    P.pe(lambda jj=jj, f=f, cs=cs, y=y, wb2=wb2: nc.tensor.matmul(
                                        pyy[y][:], w2e[wb2][:, jj, f * 128:(f + 1) * 128], g[:, jj, cs],
                                        start=(jj == 0), stop=(jj == JH - 1)),
                                        r=[("fw2", wb2), ("fg", jj, s)], w=[("fpy", y)])
                                if "ffn5" in self.dbg2s:
                                    continue
                                if True:
                                    P.dve(lambda f=f, y=y: nc.vector.tensor_scalar(
                                        ytmp[y][:], pyy[y][:], self.modv[:, 40 + f:41 + f], None, ALU.mult),
                                        r=[("fpy", y)], w=[("fyt", y)])
                                    P.dve(lambda f=f, cs=cs, y=y: nc.vector.tensor_tensor(
                                        xb[:, f, cs], xb[:, f, cs], ytmp[y][:], ALU.add),
                                        r=[("fyt", y), ("fxb", f, s)], w=[("fxb", f, s)])
                                    continue
                                P.dve(lambda f=f, cs=cs, y=y: nc.vector.scalar_tensor_tensor(
                                    xb[:, f, cs], pyy[y][:], self.modv[:, 40 + f:41 + f], xb[:, f, cs], ALU.mult, ALU.add),
                                    r=[("fpy", y), ("fxb", f, s)], w=[("fxb", f, s)])
                P.dma("sp", lambda sl=sl: nc.sync.dma_start(out=xTv[:, :, sl], in_=xb[:]),
                      r=[("fxb", f, s) for f in range(KT) for s in range(2)], w=[("dram", "xT", tb)])
                P.flush()

    def final_norm(self):
        nc, P = self.nc, self.P
        with ExitStack() as st:
            fg = self.sb(st, "fing", [128, KT], F32)
            xb = [self.sb(st, "ox", [128, KT, 512], F32) for _ in range(2)]
            sq = [self.sb(st, "osq", [128, KT, 512], BF16) for _ in range(2)]
            ms = [self.sb(st, "oms", [128, 512], F32) for _ in range(2)]
            yb = [self.sb(st, "oy", [128, KT, 512], F32) for _ in range(2)]
            ot = [self.sb(st, "oot", [128, D], F32) for _ in range(2)]
            pss = [self.ps(st, "ops", [128, 512]) for _ in range(2)]
            ptp = [self.ps(st, "optp", [128, 512]) for _ in range(2)]
            xTv = self.xT.rearrange("(kt p) t -> p kt t", p=128)
            P.dma("sp", lambda: nc.sync.dma_start(out=fg[:], in_=self.fin_g), w=["fing"])
            ti = 0
            pi = 0
            for tb in range(T // 512):
                b = tb % 2
                sl = slice(tb * 512, (tb + 1) * 512)
                P.dma("sp", lambda b=b, sl=sl: nc.sync.dma_start(out=xb[b][:], in_=xTv[:, :, sl]), w=[("ox", b)])
                P.act(lambda b=b: nc.scalar.activation(out=sq[b][:], in_=xb[b][:], func=AF.Square),
                      r=[("ox", b)], w=[("osq", b)])
                for k in range(KT):
                    P.pe(lambda b=b, k=k: nc.tensor.matmul(pss[b][:], self.ones_b[:], sq[b][:, k, :],
                                                           start=(k == 0), stop=(k == KT - 1)),
                         r=[("osq", b), "onesb"], w=[("ops", b)])
                P.dve(lambda b=b: nc.vector.tensor_scalar(ms[b][:], pss[b][:], 1.0 / D, EPS, ALU.mult, ALU.add),
                      r=[("ops", b)], w=[("oms", b)])
                P.act(lambda b=b: nc.scalar.activation(out=ms[b][:], in_=ms[b][:], func=AF.Sqrt),
                      r=[("oms", b)], w=[("oms", b)])
                P.dve(lambda b=b: nc.vector.reciprocal(ms[b][:], ms[b][:]), r=[("oms", b)], w=[("oms", b)])
                for k in range(KT):
                    P.dve(lambda b=b, k=k: nc.vector.scalar_tensor_tensor(
                        yb[b][:, k, :], xb[b][:, k, :], fg[:, k:k + 1], ms[b][:], ALU.mult, ALU.mult),
                        r=[("ox", b), ("oms", b), "fing"], w=[("oy", b, k)])
                for j in range(4):
                    o = ti % 2; ti += 1
                    for half in range(2):
                        p = pi % 2; pi += 1
                        for kk in range(4):
                            k = half * 4 + kk
                            P.pe(lambda b=b, k=k, kk=kk, j=j, p=p: nc.tensor.transpose(
                                ptp[p][:, kk * 128:(kk + 1) * 128], yb[b][:, k, j * 128:(j + 1) * 128], self.ident_f[:]),
                                r=[("oy", b, k), "identf"], w=[("optp", p)])
                        if half == 0:
                            P.dve(lambda o=o, p=p: nc.vector.tensor_copy(ot[o][:, 0:512], ptp[p][:]),
                                  r=[("optp", p)], w=[("oot", o, 0)])
                        else:
                            P.act(lambda o=o, p=p: nc.scalar.copy(ot[o][:, 512:1024], ptp[p][:]),
                                  r=[("optp", p)], w=[("oot", o, 1)])
                    row0 = tb * 512 + j * 128
                    P.dma("sp", lambda o=o, row0=row0: nc.sync.dma_start(out=self.out[row0:row0 + 128, :], in_=ot[o][:]),
                          r=[("oot", o, 0), ("oot", o, 1)], w=[("dram", "out", row0)])
            P.flush()

    def layer(self, l):
        self.phase_mod(l)
        self.phase_norm(0)
        if "a" in self.mixers:
            self.mixer_a(l)
        if "b" in self.mixers:
            self.mixer_b(l)
        if "c" in self.mixers:
            self.mixer_c(l)
        if "d" in self.mixers:
            self.mixer_d(l)
        if self.stop_after == ("mix", l):
            return "stop"
        self.phase_wout(l)
        if self.stop_after == ("wout", l):
            return "stop"
        if self.stop_after == ("preffn", l):
            return "stop"
        self.phase_ffn(l, moe=(l % 2 == 1))


def host_prep(inputs, b, shared=None):
    f = np.float32
    m = dict(shared) if shared is not None else host_shared(inputs)
    m["x"] = np.ascontiguousarray(inputs["x"][b], dtype=f)
    m["c"] = np.ascontiguousarray(np.asarray(inputs["c"][b], dtype=f).reshape(KT, 128).T)
    return m


def host_shared(inputs):
    f = np.float32
    m = {}
    m["ada_w"] = np.ascontiguousarray(np.asarray(inputs["ada_w"], dtype=f).reshape(DEPTH, KT, 128, 6 * D))
    m["ada_b"] = np.ascontiguousarray(np.asarray(inputs["ada_b"], dtype=f).reshape(DEPTH, 48, 128).transpose(0, 2, 1))
    m["mix_g"] = np.ascontiguousarray(np.asarray(inputs["mix_norm_g"], dtype=f).reshape(DEPTH, KT, 128).transpose(0, 2, 1))
    m["ffn_g"] = np.ascontiguousarray(np.asarray(inputs["ffn_norm_g"], dtype=f).reshape(DEPTH, KT, 128).transpose(0, 2, 1))
    m["fin_g"] = np.ascontiguousarray(np.asarray(inputs["final_norm_g"], dtype=f).reshape(KT, 128).T)
    m["ident"] = np.eye(128, dtype=f)
    w_in = np.asarray(inputs["w_in"], dtype=f)

    def cols(a, b):
        return w_in[:, :, a:b]
    sl_ = np.arange(128)[:, None]
    tl_ = np.arange(128)[None, :]

    def tiles(w):
        return np.ascontiguousarray(w.reshape(DEPTH, KT, 128, w.shape[-1]))
    m["w_out"] = np.ascontiguousarray(np.asarray(inputs["w_out"], dtype=f).reshape(DEPTH, KT, 128, D))

    def w13_layout(w1, w3):
        E_, _, F_ = w1.shape
        a = np.stack([w1, w3], axis=1)
        a = a.reshape(E_, 2, KT, 128, F_ // 128, 128)
        return np.ascontiguousarray(a.transpose(0, 4, 1, 3, 2, 5))

    m["ffn_w13"] = w13_layout(np.asarray(inputs["ffn_w1"], dtype=f), np.asarray(inputs["ffn_w3"], dtype=f))[None]
    m["ffn_w2"] = np.ascontiguousarray(np.asarray(inputs["ffn_w2"], dtype=f).reshape(1, 1, D_FF // 128, 128, D))
    m["moe_w13"] = w13_layout(np.asarray(inputs["moe_w1"], dtype=f)[0], np.asarray(inputs["moe_w3"], dtype=f)[0])[None]
    m["moe_w2"] = np.ascontiguousarray(np.asarray(inputs["moe_w2"], dtype=f).reshape(1, NEXP, D_FFE // 128, 128, D))
    m["router"] = np.ascontiguousarray(np.asarray(inputs["moe_router"], dtype=f)[0].reshape(KT, 128, 8))
    perm = np.concatenate([np.arange(16, 32), np.arange(0, 16)])
    z64 = np.zeros((DEPTH, D, 64), f)
    kr = cols(1152, 1184)
    m["w_b"] = tiles(np.concatenate([cols(768, 1024), cols(1024, 1152), z64, kr, z64, kr[:, :, perm]], axis=-1))
    wuq = np.asarray(inputs["mla_w_uq"], dtype=f).reshape(DEPTH, 256, 4, 96)
    wuqr = np.zeros_like(wuq)
    wuqr[:, :, :, 64:96] = wuq[:, :, :, 64:96][:, :, :, perm]
    m["w_uq"] = np.ascontiguousarray(wuq.reshape(DEPTH, 2, 128, 384))
    m["w_uqr"] = np.ascontiguousarray(wuqr.reshape(DEPTH, 2, 128, 384))
    wukv = np.asarray(inputs["mla_w_ukv"], dtype=f).reshape(DEPTH, 128, 4, 128)
    m["w_ukvk"] = np.ascontiguousarray(wukv[:, :, :, 0:64].reshape(DEPTH, 1, 128, 256))
    m["w_ukvv"] = np.ascontiguousarray(wukv[:, :, :, 64:128].reshape(DEPTH, 1, 128, 256))
    m["gq"] = np.ascontiguousarray(np.asarray(inputs["mla_q_norm_g"], dtype=f).reshape(DEPTH, 2, 128).transpose(0, 2, 1))
    m["gkv"] = np.ascontiguousarray(np.asarray(inputs["mla_kv_norm_g"], dtype=f).reshape(DEPTH, 128, 1))
    inv = (np.float32(10000.0) ** (-np.arange(0, 32, 2, dtype=f) / np.float32(32))).astype(f)
    ang = (np.arange(T, dtype=f)[:, None] * inv[None, :]).astype(f)
    cs_, sn_ = np.cos(ang).astype(f), np.sin(ang).astype(f)
    rope = np.zeros((128, 2, T), f)
    rope[64:80, 0] = cs_.T; rope[80:96, 0] = cs_.T
    rope[64:80, 1] = -sn_.T; rope[80:96, 1] = sn_.T
    m["rope"] = rope
    chunkmask = np.where(sl_ // 64 > tl_ // 64, f(NEG), f(0.0)).astype(f)
    m["diagmask"] = np.ascontiguousarray(np.tile(chunkmask, (1, 4)))
    m["w_a"] = tiles(np.concatenate([cols(0, 256), cols(256, 512), cols(512, 768)], axis=-1))
    slopes = (2.0 ** (-8.0 * np.arange(1, 5, dtype=f) / 4)).astype(f)
    tpos = np.arange(T, dtype=f)
    augq = np.zeros((4, 3, T), f); augk = np.zeros((4, 3, T), f)
    for h in range(4):
        augq[h, 0] = 1.0; augq[h, 1] = 1.0; augq[h, 2] = -slopes[h] * tpos
        augk[h, 0] = slopes[h] * (64.0 * (tpos // 64)); augk[h, 1] = slopes[h] * (tpos % 64); augk[h, 2] = 1.0
    m["augq"] = augq; m["augk"] = augk

    def corr_tile(sig):
        c = np.where(sl_ > tl_, -2.0 * sig * (sl_ - tl_), 0.0).astype(f)
        return np.where(sl_ // 64 > tl_ // 64, f(NEG), c).astype(f)
    ca = np.zeros((2, 128, 4, 128), f)
    for g in range(2):
        for j in range(4):
            ca[g, :, j, :] = corr_tile(slopes[2 * g + j // 2])
    m["corr_a"] = np.ascontiguousarray(ca.reshape(2, 128, 512))
    dl = np.asarray(inputs["diff_lambda"], dtype=f).reshape(DEPTH, 1, 128)
    m["dlam"] = np.ascontiguousarray(np.broadcast_to(dl, (DEPTH, 128, 128)))
    dg_ = np.asarray(inputs["diff_norm_g"], dtype=f).reshape(DEPTH, 1, 64)
    m["dng"] = np.ascontiguousarray(np.broadcast_to(dg_, (DEPTH, 128, 64)))
    ki = cols(2208, 2240)
    m["w_c"] = tiles(np.concatenate([cols(1184, 1440), cols(1440, 1696), cols(1696, 1952), cols(1952, 2208),
                                     ki, ki, ki, ki, cols(2240, 2248)], axis=-1))
    cc = np.zeros((128, 4, 128), f)
    for j in range(4):
        cc[:, j, :] = corr_tile(slopes[j])
    m["corr_c"] = np.ascontiguousarray(cc.reshape(128, 512))
    m["ident4"] = np.ascontiguousarray(np.tile(np.eye(128, dtype=f), (1, 4)))
    m["w_d"] = tiles(np.concatenate([cols(2248, 2504), cols(2504, 2760), cols(2760, 3016)], axis=-1))
    rb = np.asarray(inputs["band_rel_bias"], dtype=f)
    bd = np.empty((DEPTH, 5, 128, 4, 128), f)
    for dd in range(5):
        delta = 128 * dd + tl_ - sl_
        idx = np.clip(delta, -128, 128) + 128
        dc = (128 * dd + tl_) // 64 - sl_ // 64
        ok = (dc >= 0) & (dc <= 8)
        for h in range(4):
            g = rb[:, h, :][:, idx]
            bd[:, dd, :, h, :] = np.where(ok[None], g, f(NEG))
    m["bias_d"] = np.ascontiguousarray(bd.reshape(DEPTH, 5, 128, 512))
    return m


def kernel(**inputs):
    bld = Builder()
    nc = bld.build()
    shared = host_shared(inputs)
    in_maps = []
    for b in range(8):
        m = host_prep(inputs, b, shared)
        in_maps.append({k: v for k, v in m.items() if k in bld.inputs})
    res = run_bass_kernel_spmd(nc, in_maps, core_ids=list(range(8)))
    return np.stack([np.asarray(r["out"]) for r in res.results], axis=0).astype(np.float32)
```

```python
import math
import os
from contextlib import ExitStack
import numpy as np
import concourse.bass as bass
import concourse.mybir as mybir
from concourse.bass_utils import run_bass_kernel_spmd

F32 = mybir.dt.float32
BF16 = mybir.dt.bfloat16
AF = mybir.ActivationFunctionType
ALU = mybir.AluOpType
AX = mybir.AxisListType

D = 1024
T = 4096
DEPTH = 2
NT = T // 128
KT = D // 128
EPS = 1e-6
D_FF = 2816
D_FFE = 3584
NEXP = 8
NEG = -30000.0


class Prog:
    NSLOT = 12

    def __init__(self, nc, es):
        self.nc = nc
        self.ops = []
        self.engs = {"pe": nc.tensor, "act": nc.scalar, "dve": nc.vector,
                     "pool": nc.gpsimd, "sp": nc.sync}
        self.esem = {e: es.enter_context(nc.semaphore("sem_" + e)) for e in self.engs}
        self.dq = ("sp", "act", "pool")
        self.dsem = {q: [es.enter_context(nc.semaphore("dsem_%s%d" % (q, k))) for k in range(self.NSLOT)]
                     for q in self.dq}
        self.ecount = {e: 0 for e in self.engs}
        self.dcount = {q: 0 for q in self.dq}
        self.eclock = {e: {} for e in self.engs}
        self.sig = {}
        self.done_clock = {}
        self.gid = 0
        self.last_comp = {}
        self.dhist = {q: [] for q in self.dq}
        self.n_ops = 0
        self.n_waits = 0
        self.n_sig = 0

    def op(self, eng, fn, reads=(), writes=(), dma=False):
        self.ops.append((eng, fn, tuple(reads), tuple(writes), dma))

    def pe(self, fn, r=(), w=()): self.op("pe", fn, r, w)
    def act(self, fn, r=(), w=()): self.op("act", fn, r, w)
    def dve(self, fn, r=(), w=()): self.op("dve", fn, r, w)
    def pool(self, fn, r=(), w=()): self.op("pool", fn, r, w)
    def dma(self, q, fn, r=(), w=()): self.op(q, fn, r, w, True)

    def _sem(self, sk):
        return self.esem[sk[1]] if sk[0] == "e" else self.dsem[sk[1]][sk[2]]

    def _bar_deps(self):
        d = set(self.last_comp.values())
        for q in self.dq:
            d.update(self.dhist[q][-self.NSLOT:])
        return d

    def flush(self):
        ops = self.ops
        self.ops = []
        n = len(ops)
        base = self.gid
        self.gid += n
        bar = self._bar_deps()
        last_w = {}
        readers = {}
        deps = [None] * n
        seen = set()
        openg = {}
        for i, (eng, fn, rd, wr, is_dma) in enumerate(ops):
            g = base + i
            openg[g] = (eng, is_dma)
            d = set()
            if eng not in seen:
                seen.add(eng)
                d |= bar
            for r in rd:
                lw = last_w.get(r)
                if lw is not None:
                    d.add(lw)
            for r in wr:
                lw = last_w.get(r)
                if lw is not None:
                    d.add(lw)
                for x in readers.get(r, ()):
                    d.add(x)
            for r in rd:
                readers.setdefault(r, []).append(g)
            for r in wr:
                last_w[r] = g
                readers[r] = []
            if is_dma:
                h = self.dhist[eng]
                if len(h) >= self.NSLOT:
                    d.add(h[-self.NSLOT])
                h.append(g)
            d.discard(g)
            if eng == "pe" and not is_dma:
                d = {x for x in d if not (x >= base and openg[x] == ("pe", False))}
            deps[i] = d
            if not is_dma:
                self.last_comp[eng] = g
        signal = set(self.last_comp.values())
        for i in range(n):
            if ops[i][4]:
                signal.add(base + i)
            signal |= deps[i]
        for q in self.dq:
            self.dhist[q] = self.dhist[q][-self.NSLOT:]
        for i, (eng, fn, rd, wr, is_dma) in enumerate(ops):
            g = base + i
            if is_dma:
                k = self.dcount[eng]
                self.dcount[eng] += 1
                self.sig[g] = (("d", eng, k % self.NSLOT), 16 * (k // self.NSLOT + 1))
            elif g in signal:
                self.ecount[eng] += 1
                self.sig[g] = (("e", eng), self.ecount[eng])
        for i, (eng, fn, rd, wr, is_dma) in enumerate(ops):
            g = base + i
            clk = self.eclock[eng]
            e = self.engs[eng]
            need = {}
            for x in deps[i]:
                sk, sv = self.sig[x]
                if clk.get(sk, 0) < sv and need.get(sk, 0) < sv:
                    need[sk] = sv
            for x in deps[i]:
                for k2, v2 in self.done_clock[x].items():
                    if clk.get(k2, 0) < v2:
                        clk[k2] = v2
            for sk, sv in need.items():
                e.wait_ge(self._sem(sk), sv)
                self.n_waits += 1
                if clk.get(sk, 0) < sv:
                    clk[sk] = sv
            inst = fn()
            if g in self.sig:
                sk, sv = self.sig[g]
                inst.then_inc(self._sem(sk), 16 if is_dma else 1)
                dc = dict(clk)
                dc[sk] = sv
                self.done_clock[g] = dc
                self.n_sig += 1
        self.n_ops += n
        keep = self._bar_deps()
        self.sig = {g: v for g, v in self.sig.items() if g in keep}
        self.done_clock = {g: v for g, v in self.done_clock.items() if g in keep}

    def finish(self):
        self.flush()
        e = self.nc.sync
        clk = self.eclock["sp"]
        for x in self._bar_deps():
            sk, sv = self.sig[x]
            if clk.get(sk, 0) < sv:
                e.wait_ge(self._sem(sk), sv)
                clk[sk] = sv
        return dict(n_ops=self.n_ops, n_waits=self.n_waits, n_sig=self.n_sig, ecount=dict(self.ecount), dcount=dict(self.dcount))


class Builder:
    def __init__(self, debug=None, stop_after=None, mixers="abcd"):
        self.mixers = mixers
        import os
        self.dbg = os.environ.get("MK_DBG", "")
        self.dbg2s = set(os.environ.get("MK_DBG2", "").split(","))
        self.dbg2 = ""
        self.debug = debug or []
        self.stop_after = stop_after
        self.nc = bass.Bass("TRN2", target_bir_lowering=False)
        self.es = ExitStack()
        self.P = Prog(self.nc, self.es)
        self.uid = 0
        self.inputs = {}

    def din(self, name, shape, dt=F32):
        t = self.nc.dram_tensor(name, list(shape), dt, kind="ExternalInput").ap()
        self.inputs[name] = t
        return t

    def dscr(self, name, shape, dt):
        kind = "ExternalOutput"
        return self.nc.dram_tensor(name, list(shape), dt, kind=kind).ap()

    def sb(self, stack, name, shape, dt):
        self.uid += 1
        return stack.enter_context(self.nc.sbuf_tensor("%s_%d" % (name, self.uid), list(shape), dt))

    def ps(self, stack, name, shape, dt=F32):
        self.uid += 1
        return stack.enter_context(self.nc.psum_tensor("%s_%d" % (name, self.uid), list(shape), dt))

    def build(self):
        nc, P = self.nc, self.P
        with self.es as es:
            self.declare_io()
            self.consts(es)
            P.flush()
            self.phase_l0()
            for l in range(DEPTH):
                if self.stop_after == ("final", 0):
                    continue
                if self.layer(l) == "stop" or self.stop_after == ("layer", l):
                    break
            else:
                self.final_norm()
            st = P.finish()
        self.stats = st
        return nc

    def declare_io(self):
        nc = self.nc
        self.x_in = self.din("x", [T, D])
        self.c_in = self.din("c", [128, KT])
        self.ada_w = self.din("ada_w", [DEPTH, KT, 128, 6 * D])
        self.ada_b = self.din("ada_b", [DEPTH, 128, 48])
        self.mix_g = self.din("mix_g", [DEPTH, 128, KT])
        self.ffn_g = self.din("ffn_g", [DEPTH, 128, KT])
        self.fin_g = self.din("fin_g", [128, KT])
        self.ident_in = self.din("ident", [128, 128])
        self.w_d = self.din("w_d", [DEPTH, KT, 128, 768])
        self.w_out = self.din("w_out", [DEPTH, KT, 128, D])
        self.w_b = self.din("w_b", [DEPTH, KT, 128, 576])
        self.w_uq = self.din("w_uq", [DEPTH, 2, 128, 384])
        self.w_uqr = self.din("w_uqr", [DEPTH, 2, 128, 384])
        self.w_ukvk = self.din("w_ukvk", [DEPTH, 1, 128, 256])
        self.w_ukvv = self.din("w_ukvv", [DEPTH, 1, 128, 256])
        self.gq = self.din("gq", [DEPTH, 128, 2])
        self.gkv = self.din("gkv", [DEPTH, 128, 1])
        self.rope = self.din("rope", [128, 2, T])
        self.diagmask = self.din("diagmask", [128, 512])
        self.w_a = self.din("w_a", [DEPTH, KT, 128, 768])
        self.w_c = self.din("w_c", [DEPTH, KT, 128, 1160])
        self.corr_c = self.din("corr_c", [128, 512])
        self.ident4 = self.din("ident4", [128, 512])
        self.corr_a = self.din("corr_a", [2, 128, 512])
        self.augq = self.din("augq", [4, 3, T])
        self.augk = self.din("augk", [4, 3, T])
        self.dlam = self.din("dlam", [DEPTH, 128, 128])
        self.dng = self.din("dng", [DEPTH, 128, 64])
        self.ffn_w13 = self.din("ffn_w13", [1, 1, D_FF // 128, 2, 128, KT, 128])
        self.ffn_w2 = self.din("ffn_w2", [1, 1, D_FF // 128, 128, D])
        self.moe_w13 = self.din("moe_w13", [1, NEXP, D_FFE // 128, 2, 128, KT, 128])
        self.moe_w2 = self.din("moe_w2", [1, NEXP, D_FFE // 128, 128, D])
        self.router = self.din("router", [KT, 128, 8])
        self.bias_d = self.din("bias_d", [DEPTH, 5, 128, 512])
        self.out = nc.dram_tensor("out", [T, D], F32, kind="ExternalOutput").ap()
        self.xT = self.dscr("xT", [D, T], F32)
        self.hnT = self.dscr("hnT", [D, T], BF16)
        self.oT = self.dscr("oT", [D, T], BF16)

    def consts(self, es):
        nc, P = self.nc, self.P
        self.ident_f = self.sb(es, "identf", [128, 128], F32)
        self.ident_b = self.sb(es, "identb", [128, 128], BF16)
        self.ones_b = self.sb(es, "onesb", [128, 128], BF16)
        self.modv = self.sb(es, "modv", [128, 48], F32)
        self.gvec = self.sb(es, "gvec", [128, 4 * KT], F32)
        P.dma("sp", lambda: nc.sync.dma_start(out=self.ident_f[:], in_=self.ident_in), w=["identf"])
        P.dve(lambda: nc.vector.tensor_copy(self.ident_b[:], self.ident_f[:]), r=["identf"], w=["identb"])
        P.dve(lambda: nc.vector.memset(self.ones_b[:], 1.0), w=["onesb"])

    def phase_l0(self):
        nc, P = self.nc, self.P
        with ExitStack() as st:
            xin = [self.sb(st, "xin", [128, 4, D], F32) for _ in range(2)]
            xo = [self.sb(st, "xo", [128, KT, 512], F32) for _ in range(2)]
            pt = [self.ps(st, "pt", [128, 512]) for _ in range(2)]
            xv = self.x_in.rearrange("(tb j p) f -> tb p j f", j=4, p=128)
            xTv = self.xT.rearrange("(kt p) t -> p kt t", p=128)
            for tb in range(T // 512):
                b = tb % 2
                P.dma("sp", lambda tb=tb, b=b: nc.sync.dma_start(out=xin[b][:], in_=xv[tb]),
                      w=[("xin", b)])
                for kt in range(KT):
                    pb = kt % 2
                    for j in range(4):
                        P.pe(lambda b=b, kt=kt, j=j, pb=pb: nc.tensor.transpose(
                            pt[pb][:, j * 128:(j + 1) * 128], xin[b][:, j, kt * 128:(kt + 1) * 128],
                            self.ident_f[:]),
                            r=[("xin", b), "identf"], w=[("pt", pb)])
                    if kt % 2 == 0:
                        P.dve(lambda b=b, kt=kt, pb=pb: nc.vector.tensor_copy(xo[b][:, kt, :], pt[pb][:]),
                              r=[("pt", pb)], w=[("xo", b, kt)])
                    else:
                        P.act(lambda b=b, kt=kt, pb=pb: nc.scalar.copy(xo[b][:, kt, :], pt[pb][:]),
                              r=[("pt", pb)], w=[("xo", b, kt)])
                P.dma("sp", lambda tb=tb, b=b: nc.sync.dma_start(
                    out=xTv[:, :, tb * 512:(tb + 1) * 512], in_=xo[b][:]),
                    r=[("xo", b, kt) for kt in range(KT)], w=[("dram", "xT", tb)])
            P.flush()

    def phase_mod(self, l):
        nc, P = self.nc, self.P
        with ExitStack() as st:
            cs = self.sb(st, "cs", [128, KT], F32)
            sc = self.sb(st, "sc", [128, KT], BF16)
            wb = [self.sb(st, "adaw", [128, 6 * D], BF16) for _ in range(4)]
            ab = self.sb(st, "adab", [128, 48], F32)
            gg = self.sb(st, "gg", [128, 2 * KT], F32)
            pm = self.ps(st, "pm", [128, 512])
            P.dma("sp", lambda: nc.sync.dma_start(out=cs[:], in_=self.c_in), w=["cs"])
            P.dma("sp", lambda: nc.sync.dma_start(out=ab[:], in_=self.ada_b[l]), w=["adab"])
            P.dma("sp", lambda: nc.sync.dma_start(out=gg[:, 0:KT], in_=self.mix_g[l]), w=["gg0"])
            P.dma("sp", lambda: nc.sync.dma_start(out=gg[:, KT:2 * KT], in_=self.ffn_g[l]), w=["gg1"])
            P.act(lambda: nc.scalar.activation(out=sc[:], in_=cs[:], func=AF.Silu), r=["cs"], w=["sc"])
            P.dve(lambda: nc.vector.memset(pm[:], 0.0), w=["pm"])
            for kt in range(KT):
                b = kt % 4
                P.dma("pool", lambda kt=kt, b=b: nc.gpsimd.dma_start(out=wb[b][:], in_=self.ada_w[l, kt]),
                      w=[("adaw", b)])
                for ft in range(48):
                    P.pe(lambda kt=kt, b=b, ft=ft: nc.tensor.matmul(
                        pm[:, ft:ft + 1], wb[b][:, ft * 128:(ft + 1) * 128], sc[:, kt:kt + 1],
                        start=False, stop=(kt == KT - 1), skip_group_check=True),
                        r=[("adaw", b), "sc"], w=["pm"])
            mv = self.modv
            P.dve(lambda: nc.vector.tensor_tensor(mv[:], pm[:, 0:48], ab[:], ALU.add),
                  r=["pm", "adab"], w=["modv"])
            gv = self.gvec
            P.dve(lambda: nc.vector.scalar_tensor_tensor(
                gv[:, 0:KT], mv[:, 8:16], 1.0, gg[:, 0:KT], ALU.add, ALU.mult),
                r=["modv", "gg0"], w=["gvec0"])
            P.dve(lambda: nc.vector.scalar_tensor_tensor(
                gv[:, KT:2 * KT], mv[:, 32:40], 1.0, gg[:, KT:2 * KT], ALU.add, ALU.mult),
                r=["modv", "gg1"], w=["gvec1"])
            P.flush()

    def phase_norm(self, which):
        nc, P = self.nc, self.P
        goff = which * KT
        shoff = 0 if which == 0 else 24
        with ExitStack() as st:
            xb = [self.sb(st, "nx", [128, KT, 512], F32) for _ in range(2)]
            sq = [self.sb(st, "nsq", [128, KT, 512], BF16) for _ in range(2)]
            ms = [self.sb(st, "nms", [128, 512], F32) for _ in range(2)]
            tmp = [self.sb(st, "ntmp", [128, KT, 512], F32) for _ in range(2)]
            hb = [self.sb(st, "nhb", [128, KT, 512], BF16) for _ in range(2)]
            pss = [self.ps(st, "nps", [128, 512]) for _ in range(2)]
            xTv = self.xT.rearrange("(kt p) t -> p kt t", p=128)
            hTv = self.hnT.rearrange("(kt p) t -> p kt t", p=128)
            def stage1(tb):
                b = tb % 2
                P.dma("sp", lambda tb=tb, b=b: nc.sync.dma_start(
                    out=xb[b][:], in_=xTv[:, :, tb * 512:(tb + 1) * 512]),
                    r=[("dram", "xT", tb)], w=[("nx", b)])
                P.act(lambda b=b: nc.scalar.activation(out=sq[b][:], in_=xb[b][:], func=AF.Square),
                      r=[("nx", b)], w=[("nsq", b)])
                for kt in range(KT):
                    P.pe(lambda b=b, kt=kt: nc.tensor.matmul(
                        pss[b][:], self.ones_b[:], sq[b][:, kt, :], start=(kt == 0), stop=(kt == KT - 1)),
                        r=[("nsq", b), "onesb"], w=[("nps", b)])
                P.dve(lambda b=b: nc.vector.tensor_scalar(
                    ms[b][:], pss[b][:], 1.0 / D, EPS, ALU.mult, ALU.add),
                    r=[("nps", b)], w=[("nms", b)])
                P.act(lambda b=b: nc.scalar.activation(out=ms[b][:], in_=ms[b][:], func=AF.Sqrt),
                      r=[("nms", b)], w=[("nms", b)])
                P.dve(lambda b=b: nc.vector.reciprocal(ms[b][:], ms[b][:]),
                      r=[("nms", b)], w=[("nms", b)])

            def stage2(tb):
                b = tb % 2
                for kt in range(KT):
                    P.dve(lambda b=b, kt=kt: nc.vector.scalar_tensor_tensor(
                        tmp[b][:, kt, :], xb[b][:, kt, :], self.gvec[:, goff + kt:goff + kt + 1],
                        ms[b][:], ALU.mult, ALU.mult),
                        r=[("nx", b), ("nms", b), "gvec%d" % which], w=[("ntmp", b, kt)])
                    P.act(lambda b=b, kt=kt: nc.scalar.activation(
                        out=hb[b][:, kt, :], in_=tmp[b][:, kt, :], func=AF.Identity,
                        bias=self.modv[:, shoff + kt:shoff + kt + 1], scale=1.0),
                        r=[("ntmp", b, kt), "modv"], w=[("nhb", b, kt)])
                P.dma("sp", lambda tb=tb, b=b: nc.sync.dma_start(
                    out=hTv[:, :, tb * 512:(tb + 1) * 512], in_=hb[b][:]),
                    r=[("nhb", b, kt) for kt in range(KT)], w=[("dram", "hnT", tb)])

            stage1(0)
            for tb in range(T // 512):
                if tb + 1 < T // 512:
                    stage1(tb + 1)
                stage2(tb)
            P.flush()

    def load_w(self, st, name, dram_ap, ncols, nk=KT):
        nc, P = self.nc, self.P
        w = self.sb(st, name, [128, nk, ncols], BF16)
        for k in range(nk):
            P.dma("pool", lambda k=k: nc.gpsimd.dma_start(out=w[:, k, :], in_=dram_ap[k]), w=[(name, k)])
        return w

    def hn_blocks(self, st):
        nc, P = self.nc, self.P
        hb = [self.sb(st, "hb", [128, KT, 512], BF16) for _ in range(2)]
        hTv = self.hnT.rearrange("(kt p) t -> p kt t", p=128)

        def load(tb):
            b = tb % 2
            P.dma("sp", lambda: nc.sync.dma_start(out=hb[b][:], in_=hTv[:, :, tb * 512:(tb + 1) * 512]),
                  w=[("hb", b)])
            return hb[b], ("hb", b)
        return load

    def proj_fm(self, pp, ppr, w, wres, c0, M, hb, hres, nk=KT):
        nc, P = self.nc, self.P
        for k in range(nk):
            P.pe(lambda k=k: nc.tensor.matmul(pp[0:M, :], w[:, k, c0:c0 + M], hb[:, k, :],
                                              start=(k == 0), stop=(k == nk - 1)),
                 r=[hres] + wres, w=[ppr])

    def proj_tm(self, pp, ppr, w, wres, c0, n, hb, hres, j, nk=KT):
        nc, P = self.nc, self.P
        for k in range(nk):
            P.pe(lambda k=k: nc.tensor.matmul(pp[:, 0:n], hb[:, k, j * 128:(j + 1) * 128], w[:, k, c0:c0 + n],
                                              start=(k == 0), stop=(k == nk - 1)),
                 r=[hres] + wres, w=[ppr])

    def attn_core(self, st, name, groups, kts_fn, pre_fn, qk_fn, v_fn, out_fn, before_qt=None, ns=3):
        nc, P = self.nc, self.P
        S = [self.ps(st, "S", [128, 512]) for _ in range(ns)]
        O = [self.ps(st, "O", [128, 512]) for _ in range(2)]
        Pt = [self.sb(st, "Pt", [128, 512], BF16) for _ in range(ns + 1)]
        rl = [self.sb(st, "rl", [128, 4], F32) for _ in range(2)]
        if os.environ.get("MK_NOATTN"):
            return
        tasks = []
        oi = 0
        for qt in range(NT):
            for g in range(groups):
                kts = kts_fn(qt)
                for ki, kt in enumerate(kts):
                    tasks.append((qt, g, ki, kt, len(kts), oi % 2, len(tasks) % ns, len(tasks) % (ns + 1)))
                oi += 1

        def emit_qk(t):
            qt, g, ki, kt, nk, ob, sbk, pb = t
            if ki == 0 and g == 0 and before_qt is not None:
                before_qt(qt)
            pre = pre_fn(g, qt, kt)
            first = True
            for (lh, rh, rr) in pre:
                P.pe(lambda lh=lh, rh=rh, first=first: nc.tensor.matmul(
                    S[sbk][:], lh, rh, start=first, stop=False, skip_group_check=True),
                    r=rr, w=[("S", sbk)])
                first = False
            for j in range(4):
                lh, rh = qk_fn(g, j, qt, kt)
                P.pe(lambda lh=lh, rh=rh, first=first, j=j: nc.tensor.matmul(
                    S[sbk][:, j * 128:(j + 1) * 128], lh, rh, start=first, stop=(j == 3),
                    skip_group_check=True), w=[("S", sbk)])
                first = False

        def emit_exp(t):
            qt, g, ki, kt, nk, ob, sbk, pb = t
            P.act(lambda: nc.scalar.activation(out=Pt[pb][:], in_=S[sbk][:], func=AF.Exp),
                  r=[("S", sbk)], w=[("Pt", pb)])

        def emit_pv(t):
            qt, g, ki, kt, nk, ob, sbk, pb = t
            for j in range(4):
                va = v_fn(g, j, kt)
                P.pe(lambda va=va, j=j: nc.tensor.matmul(
                    O[ob][:, j * 66:j * 66 + 65], Pt[pb][:, j * 128:(j + 1) * 128], va,
                    start=(ki == 0 and j == 0), stop=(ki == nk - 1), skip_group_check=True),
                    r=[("Pt", pb)], w=[("O", ob)])
            if ki == nk - 1:
                P.dve(lambda: nc.vector.reciprocal(
                    rl[ob][:, :], O[ob][:, 0:264].rearrange("p (j c) -> p j c", c=66)[:, :, 64]),
                    r=[("O", ob)], w=[("rl", ob)])
                out_fn(g, qt, O[ob], ("O", ob), rl[ob], ("rl", ob))

        la = ns - 1
        for i in range(min(la, len(tasks))):
            emit_qk(tasks[i])
        for i, t in enumerate(tasks):
            emit_exp(t)
            if i + la < len(tasks):
                emit_qk(tasks[i + la])
            emit_pv(t)

    def std_out(self, st, name, moff, ntp=2):
        nc, P = self.nc, self.P
        on = [self.sb(st, "on", [128, 256], BF16) for _ in range(2)]
        tp = [self.ps(st, "tp", [128, 2, 128], BF16) for _ in range(ntp)]
        stage = [self.sb(st, "ostg", [128, 2, 512], BF16) for _ in range(2)]
        oTv = self.oT[moff:moff + 256, :].rearrange("(i p) t -> p i t", p=128)

        def out_fn(g, qt, O, Or, rl, rlr):
            nb = qt % 2
            sbi = (qt // 4) % 2
            q4 = qt % 4
            tpb = qt % ntp
            for j in range(4):
                if True:
                    P.dve(lambda j=j: nc.vector.tensor_scalar(
                        on[nb][:, j * 64:(j + 1) * 64], O[:, j * 66:j * 66 + 64], rl[:, j:j + 1], None, ALU.mult),
                        r=[Or, rlr], w=[("on", nb, j)])
                else:
                    P.act(lambda j=j: nc.scalar.activation(
                        out=on[nb][:, j * 64:(j + 1) * 64], in_=O[:, j * 66:j * 66 + 64], func=AF.Copy,
                        scale=rl[:, j:j + 1]),
                        r=[Or, rlr], w=[("on", nb, j)])
            if "notp" in self.dbg2s:
                return
            for i in range(2):
                P.pe(lambda i=i: nc.tensor.transpose(tp[tpb][:, i, :], on[nb][:, i * 128:(i + 1) * 128],
                                                     self.ident_b[:]),
                     r=[("on", nb, 2 * i), ("on", nb, 2 * i + 1)], w=[("tp", tpb)])
            if "nostg" in self.dbg2s:
                return
            P.dve(lambda: nc.vector.tensor_copy(stage[sbi][:, 0, q4 * 128:(q4 + 1) * 128], tp[tpb][:, 0, :]),
                  r=[("tp", tpb)], w=[("ostg", sbi, 0)])
            P.dve(lambda: nc.vector.tensor_copy(stage[sbi][:, 1, q4 * 128:(q4 + 1) * 128], tp[tpb][:, 1, :]),
                  r=[("tp", tpb)], w=[("ostg", sbi, 1)])
            if q4 == 3 and "nodma" not in self.dbg2s:
                tb = qt // 4
                P.dma("sp", lambda: nc.sync.dma_start(out=oTv[:, :, tb * 512:(tb + 1) * 512], in_=stage[sbi][:]),
                      r=[("ostg", sbi, 0), ("ostg", sbi, 1)], w=[("dram", "oT", name, tb)])
        return out_fn

    def v_evac(self, V, pp, ppr, tt, eng_i):
        nc, P = self.nc, self.P
        for h in range(4):
            if (eng_i + h) % 2 == 0:
                P.dve(lambda h=h: nc.vector.tensor_copy(V[:, tt, h, 0:64], pp[:, h * 64:(h + 1) * 64]),
                      r=[ppr], w=[("V", tt)])
            else:
                P.act(lambda h=h: nc.scalar.copy(V[:, tt, h, 0:64], pp[:, h * 64:(h + 1) * 64]),
                      r=[ppr], w=[("V", tt)])

    def mixer_d(self, l):
        nc, P = self.nc, self.P
        with ExitStack() as st:
            QT = [self.sb(st, "dQT", [128, T], BF16) for _ in range(2)]
            KTt = [self.sb(st, "dKT", [128, T], BF16) for _ in range(4)]
            for h in range(4):
                P.dve(lambda h=h: nc.vector.memset(KTt[h][:], 0.0), w=[("KT", h)])
            V = self.sb(st, "dV", [128, NT, 4, 66], BF16)
            bias = self.sb(st, "dbias", [128, 5, 512], BF16)
            P.dve(lambda: nc.vector.memset(V[:], 1.0), w=[("V", tt) for tt in range(NT)])
            for dd in range(5):
                P.dma("pool", lambda dd=dd: nc.gpsimd.dma_start(out=bias[:, dd, :], in_=self.bias_d[l, dd]),
                      w=[("dbias", dd)])
            with ExitStack() as s2:
                w = self.load_w(s2, "wd", self.w_d[l], 768)
                wres = [("wd", k) for k in range(KT)]
                load = self.hn_blocks(s2)
                pp = [self.ps(s2, "pp", [128, 512]) for _ in range(4)]
                pi = 0
                for tb in range(T // 512):
                    hb, hres = load(tb)
                    sl = slice(tb * 512, (tb + 1) * 512)
                    for i in range(2):
                        p = pi % 4; pi += 1
                        self.proj_fm(pp[p], ("pp", p), w, wres, i * 128, 128, hb, hres)
                        P.act(lambda p=p, i=i, sl=sl: nc.scalar.mul(QT[i][:, sl], pp[p][:], 0.125),
                              r=[("pp", p)], w=[("QT", i, tb)])
                        p = pi % 4; pi += 1
                        self.proj_fm(pp[p], ("pp", p), w, wres, 256 + i * 128, 128, hb, hres)
                        P.dve(lambda p=p, i=i, sl=sl: nc.vector.tensor_copy(KTt[2 * i][0:64, sl], pp[p][0:64, :]),
                              r=[("pp", p)], w=[("KT", 2 * i)])
                        P.dve(lambda p=p, i=i, sl=sl: nc.vector.tensor_copy(KTt[2 * i + 1][64:128, sl], pp[p][64:128, :]),
                              r=[("pp", p)], w=[("KT", 2 * i + 1)])
                    for j in range(4):
                        if self.dbg2 == "noV":
                            continue
                        p = pi % 4; pi += 1
                        self.proj_tm(pp[p], ("pp", p), w, wres, 512, 256, hb, hres, j)
                        if self.dbg2 == "noVevac":
                            continue
                        self.v_evac(V, pp[p], ("pp", p), tb * 4 + j, j)
                P.flush()
            if self.dbg == "proj":
                return
            with ExitStack() as s3:
                out_fn = self.std_out(s3, "d", 768)

                def kts_fn(qt):
                    return list(range(max(0, qt - 4), qt + 1))

                def pre_fn(g, qt, kt):
                    if self.dbg2 == "nopre":
                        return []
                    return [(self.ident_b[:], bias[:, qt - kt, :], [])]

                def qk_fn(g, j, qt, kt):
                    return (KTt[j][:, kt * 128:(kt + 1) * 128],
                            QT[j // 2][:, qt * 128:(qt + 1) * 128])

                def v_fn(g, j, kt):
                    return V[:, kt, j, 0:65]
                self.attn_core(s3, "d", 1, kts_fn, pre_fn, qk_fn, v_fn, out_fn)
                P.flush()

    def rsqrt_pool(self, out_ap, in_ap, nh_ap, r, w):
        nc, P = self.nc, self.P
        P.pool(lambda: nc.gpsimd.tensor_tensor(out_ap, in_ap, nh_ap, ALU.pow), r=r, w=w)

    def mixer_b(self, l):
        nc, P = self.nc, self.P
        scale = 96.0 ** -0.5
        with ExitStack() as st:
            QTh = [self.sb(st, "bQT", [128, T], BF16) for _ in range(4)]
            KTh = [self.sb(st, "bKT", [128, T], BF16) for _ in range(4)]
            V = self.sb(st, "bV", [128, NT, 4, 66], BF16)
            dmask = self.sb(st, "bdm", [128, 512], BF16)
            P.dve(lambda: nc.vector.memset(V[:], 1.0), w=[("V", tt) for tt in range(NT)])
            for h in range(4):
                P.dve(lambda h=h: nc.vector.memset(QTh[h][:], 0.0), w=[("QT", h), ("QTr", h)])
                P.dve(lambda h=h: nc.vector.memset(KTh[h][:], 0.0), w=[("KT", h), ("KTr", h)])
            P.dma("pool", lambda: nc.gpsimd.dma_start(out=dmask[:], in_=self.diagmask), w=["bdm"])
            with ExitStack() as s2:
                w = self.load_w(s2, "wb", self.w_b[l], 576)
                wres = [("wb", k) for k in range(KT)]
                wuq = self.load_w(s2, "wuq", self.w_uq[l], 384, nk=2)
                wuqr = self.load_w(s2, "wuqr", self.w_uqr[l], 384, nk=2)
                wkk = self.load_w(s2, "wkk", self.w_ukvk[l], 256, nk=1)
                wkv = self.load_w(s2, "wkv", self.w_ukvv[l], 256, nk=1)
                wu_res = [("wuq", 0), ("wuq", 1), ("wuqr", 0), ("wuqr", 1), ("wkk", 0), ("wkv", 0)]
                gq = self.sb(s2, "bgq", [128, 3], F32)
                nh = self.sb(s2, "bnh", [128, 512], F32)
                P.dma("sp", lambda: nc.sync.dma_start(out=gq[:, 0:2], in_=self.gq[l]), w=["bgq"])
                P.dma("sp", lambda: nc.sync.dma_start(out=gq[:, 2:3], in_=self.gkv[l]), w=["bgq"])
                P.dve(lambda: nc.vector.memset(nh[:], -0.5), w=["bnh"])
                load = self.hn_blocks(s2)
                rope = [self.sb(s2, "brope", [128, 2, 512], F32) for _ in range(2)]
                qd = [self.sb(s2, "bqd", [128, 3, 512], F32) for _ in range(2)]
                sq = [self.sb(s2, "bsq", [128, 3, 512], BF16) for _ in range(2)]
                ms = [self.sb(s2, "bms", [128, 2, 512], F32) for _ in range(2)]
                qn = [self.sb(s2, "bqn", [128, 3, 512], BF16) for _ in range(2)]
                t1 = [self.sb(s2, "bt1", [128, 512], F32) for _ in range(2)]
                t2 = [self.sb(s2, "bt2", [128, 512], F32) for _ in range(2)]
                hbs = {}
                pp = [self.ps(s2, "pp", [128, 512]) for _ in range(6)]
                pi = 0
                ri = 0
                pc = [0]
                rc = [0]
                def stage1(tb):
                    b = tb % 2
                    hb, hres = load(tb)
                    sl = slice(tb * 512, (tb + 1) * 512)
                    P.dma("sp", lambda b=b, sl=sl: nc.sync.dma_start(out=rope[b][64:96, :, :], in_=self.rope[64:96, :, sl]),
                          w=[("brope", b)])
                    for i in range(3):
                        p = pc[0] % 6; pc[0] += 1
                        self.proj_fm(pp[p], ("pp", p), w, wres, i * 128, 128, hb, hres)
                        P.act(lambda p=p, i=i, b=b: nc.scalar.copy(qd[b][:, i, :], pp[p][:]),
                              r=[("pp", p)], w=[("bqd", b, i)])
                    P.act(lambda b=b: nc.scalar.activation(out=sq[b][:], in_=qd[b][:], func=AF.Square),
                          r=[("bqd", b, i) for i in range(3)], w=[("bsq", b)])
                    p = pc[0] % 6; pc[0] += 1
                    for i in range(2):
                        P.pe(lambda p=p, i=i, b=b: nc.tensor.matmul(pp[p][:], self.ones_b[:], sq[b][:, i, :],
                                                                   start=(i == 0), stop=(i == 1)),
                             r=[("bsq", b), "onesb"], w=[("pp", p)])
                    P.dve(lambda p=p, b=b: nc.vector.tensor_scalar(ms[b][:, 0, :], pp[p][:], 1.0 / 256, EPS, ALU.mult, ALU.add),
                          r=[("pp", p)], w=[("bms", b, 0)])
                    p = pc[0] % 6; pc[0] += 1
                    P.pe(lambda p=p, b=b: nc.tensor.matmul(pp[p][:], self.ones_b[:], sq[b][:, 2, :], start=True, stop=True),
                         r=[("bsq", b), "onesb"], w=[("pp", p)])
                    P.dve(lambda p=p, b=b: nc.vector.tensor_scalar(ms[b][:, 1, :], pp[p][:], 1.0 / 128, EPS, ALU.mult, ALU.add),
                          r=[("pp", p)], w=[("bms", b, 1)])
                    for i in range(2):
                        P.act(lambda b=b, i=i: nc.scalar.activation(out=ms[b][:, i, :], in_=ms[b][:, i, :], func=AF.Sqrt),
                              r=[("bms", b, i)], w=[("bms", b, i)])
                        P.dve(lambda b=b, i=i: nc.vector.reciprocal(ms[b][:, i, :], ms[b][:, i, :]),
                              r=[("bms", b, i)], w=[("bms", b, i)])
                    for i in range(3):
                        P.dve(lambda b=b, i=i: nc.vector.scalar_tensor_tensor(
                            qn[b][:, i, :], qd[b][:, i, :], gq[:, i:i + 1], ms[b][:, 0 if i < 2 else 1, :],
                            ALU.mult, ALU.mult),
                            r=[("bqd", b, i), ("bms", b, 0 if i < 2 else 1), "bgq"], w=[("bqn", b, i)])
                    p1 = pc[0] % 6; pc[0] += 1
                    self.proj_fm(pp[p1], ("pp", p1), w, wres, 384, 96, hb, hres)
                    p2 = pc[0] % 6; pc[0] += 1
                    self.proj_fm(pp[p2], ("pp", p2), w, wres, 480, 96, hb, hres)
                    r = rc[0] % 2; rc[0] += 1
                    P.dve(lambda p1=p1, b=b, r=r: nc.vector.tensor_tensor(
                        t1[r][64:96, :], pp[p1][64:96, :], rope[b][64:96, 0, :], ALU.mult),
                        r=[("pp", p1), ("brope", b)], w=[("bt1", r)])
                    P.dve(lambda p2=p2, b=b, r=r: nc.vector.tensor_tensor(
                        t2[r][64:96, :], pp[p2][64:96, :], rope[b][64:96, 1, :], ALU.mult),
                        r=[("pp", p2), ("brope", b)], w=[("bt2", r)])
                    for h in range(4):
                        P.dve(lambda h=h, r=r, sl=sl: nc.vector.tensor_tensor(
                            KTh[h][64:96, sl], t1[r][64:96, :], t2[r][64:96, :], ALU.add),
                            r=[("bt1", r), ("bt2", r)], w=[("KTr", h)])

                def stage2(tb):
                    b = tb % 2
                    sl = slice(tb * 512, (tb + 1) * 512)
                    for h in range(4):
                        p1 = pc[0] % 6; pc[0] += 1
                        for i in range(2):
                            P.pe(lambda p1=p1, i=i, h=h, b=b: nc.tensor.matmul(
                                pp[p1][0:96, :], wuq[:, i, h * 96:(h + 1) * 96], qn[b][:, i, :],
                                start=(i == 0), stop=(i == 1)),
                                r=[("bqn", b, i)] + wu_res, w=[("pp", p1)])
                        p2 = pc[0] % 6; pc[0] += 1
                        for i in range(2):
                            P.pe(lambda p2=p2, i=i, h=h, b=b: nc.tensor.matmul(
                                pp[p2][0:96, :], wuqr[:, i, h * 96:(h + 1) * 96], qn[b][:, i, :],
                                start=(i == 0), stop=(i == 1)),
                                r=[("bqn", b, i)] + wu_res, w=[("pp", p2)])
                        P.act(lambda p1=p1, h=h, sl=sl: nc.scalar.mul(QTh[h][0:64, sl], pp[p1][0:64, :], scale),
                              r=[("pp", p1)], w=[("QT", h)])
                        r = rc[0] % 2; rc[0] += 1
                        P.dve(lambda p1=p1, b=b, r=r: nc.vector.scalar_tensor_tensor(
                            t1[r][64:96, :], pp[p1][64:96, :], scale, rope[b][64:96, 0, :], ALU.mult, ALU.mult),
                            r=[("pp", p1), ("brope", b)], w=[("bt1", r)])
                        P.dve(lambda p2=p2, b=b, r=r: nc.vector.scalar_tensor_tensor(
                            t2[r][64:96, :], pp[p2][64:96, :], scale, rope[b][64:96, 1, :], ALU.mult, ALU.mult),
                            r=[("pp", p2), ("brope", b)], w=[("bt2", r)])
                        P.dve(lambda h=h, r=r, sl=sl: nc.vector.tensor_tensor(
                            QTh[h][64:96, sl], t1[r][64:96, :], t2[r][64:96, :], ALU.add),
                            r=[("bt1", r), ("bt2", r)], w=[("QTr", h)])
                        p3 = pc[0] % 6; pc[0] += 1
                        P.pe(lambda p3=p3, h=h, b=b: nc.tensor.matmul(
                            pp[p3][0:64, :], wkk[:, 0, h * 64:(h + 1) * 64], qn[b][:, 2, :], start=True, stop=True),
                            r=[("bqn", b, 2)] + wu_res, w=[("pp", p3)])
                        P.dve(lambda p3=p3, h=h, sl=sl: nc.vector.tensor_copy(KTh[h][0:64, sl], pp[p3][0:64, :]),
                              r=[("pp", p3)], w=[("KT", h)])
                    for j in range(4):
                        p = pc[0] % 6; pc[0] += 1
                        P.pe(lambda p=p, j=j, b=b: nc.tensor.matmul(
                            pp[p][:, 0:256], qn[b][:, 2, j * 128:(j + 1) * 128], wkv[:, 0, :], start=True, stop=True),
                            r=[("bqn", b, 2)] + wu_res, w=[("pp", p)])
                        self.v_evac(V, pp[p], ("pp", p), tb * 4 + j, j)

                stage1(0)
                for tb in range(T // 512):
                    if tb + 1 < T // 512:
                        stage1(tb + 1)
                    stage2(tb)
                P.flush()
            with ExitStack() as s3:
                out_fn = self.std_out(s3, "b", 256)

                def kts_fn(qt):
                    return list(range(qt + 1))

                def pre_fn(g, qt, kt):
                    if kt == qt:
                        return [(self.ident_b[:], dmask[:], [])]
                    return []

                def qk_fn(g, j, qt, kt):
                    return (KTh[j][:, kt * 128:(kt + 1) * 128], QTh[j][:, qt * 128:(qt + 1) * 128])

                def v_fn(g, j, kt):
                    return V[:, kt, j, 0:65]
                self.attn_core(s3, "b", 1, kts_fn, pre_fn, qk_fn, v_fn, out_fn)
                P.flush()

    def mixer_a(self, l):
        nc, P = self.nc, self.P
        scale = 32.0 ** -0.5
        lam_init = 0.8 - 0.6 * math.exp(-0.3 * l)
        with ExitStack() as st:
            QTa = [self.sb(st, "aQT", [128, T], BF16) for _ in range(4)]
            KTa = [[self.sb(st, "aKT", [128, T], BF16) for _ in range(2)] for _ in range(4)]
            V = self.sb(st, "aV", [128, NT, 4, 66], BF16)
            corr = self.sb(st, "acorr", [128, 2, 512], BF16)
            neglam = self.sb(st, "aneglam", [128, 1], F32)
            gAb = self.sb(st, "agAb", [128, 64], F32)
            nh = self.sb(st, "anh", [128, 2], F32)
            P.dve(lambda: nc.vector.memset(V[:], 1.0), w=[("V", tt) for tt in range(NT)])
            P.dve(lambda: nc.vector.memset(nh[:], -0.5), w=["anh"])
            for h in range(4):
                P.dve(lambda h=h: nc.vector.memset(QTa[h][:], 0.0), w=[("QT", h)])
                for c in range(2):
                    P.dve(lambda h=h, c=c: nc.vector.memset(KTa[h][c][:], 0.0), w=[("KT", h, c)])
            for g in range(2):
                P.dma("pool", lambda g=g: nc.gpsimd.dma_start(out=corr[:, g, :], in_=self.corr_a[g]), w=[("acorr", g)])
            for h in range(4):
                P.dma("pool", lambda h=h: nc.gpsimd.dma_start(out=QTa[h][64:67, :], in_=self.augq[h]),
                      r=[("QT", h)], w=[("QTaug", h)])
                for c in range(2):
                    P.dma("pool", lambda h=h, c=c: nc.gpsimd.dma_start(out=KTa[h][c][64:67, :], in_=self.augk[h]),
                          r=[("KT", h, c)], w=[("KTaug", h, c)])
            with ExitStack() as s2:
                lv = self.sb(s2, "alv", [128, 128], F32)
                pr = self.sb(s2, "apr", [128, 64], F32)
                s12 = self.sb(s2, "as12", [128, 2], F32)
                P.dma("sp", lambda: nc.sync.dma_start(out=lv[:], in_=self.dlam[l]), w=["alv"])
                P.dma("sp", lambda: nc.sync.dma_start(out=gAb[:], in_=self.dng[l]), w=["agAb"])
                P.dve(lambda: nc.vector.tensor_tensor(pr[:, 0:32], lv[:, 0:32], lv[:, 32:64], ALU.mult), r=["alv"], w=["apr0"])
                P.dve(lambda: nc.vector.tensor_tensor(pr[:, 32:64], lv[:, 64:96], lv[:, 96:128], ALU.mult), r=["alv"], w=["apr1"])
                P.dve(lambda: nc.vector.reduce_sum(out=s12[:], in_=pr[:, :].rearrange("p (a b) -> p a b", a=2), axis=AX.X),
                      r=["apr0", "apr1"], w=["as12"])
                P.act(lambda: nc.scalar.activation(out=s12[:], in_=s12[:], func=AF.Exp), r=["as12"], w=["as12"])
                P.dve(lambda: nc.vector.tensor_tensor(neglam[:], s12[:, 1:2], s12[:, 0:1], ALU.subtract), r=["as12"], w=["aneglam"])
                P.dve(lambda: nc.vector.tensor_scalar(neglam[:], neglam[:], -lam_init, None, ALU.add), r=["aneglam"], w=["aneglam"])
                P.dve(lambda: nc.vector.tensor_scalar(gAb[:], gAb[:], 1.0 - lam_init, None, ALU.mult), r=["agAb"], w=["agAb"])
                w = self.load_w(s2, "wa", self.w_a[l], 768)
                wres = [("wa", k) for k in range(KT)]
                load = self.hn_blocks(s2)
                pp = [self.ps(s2, "pp", [128, 512]) for _ in range(4)]
                pi = 0
                for tb in range(T // 512):
                    hb, hres = load(tb)
                    sl = slice(tb * 512, (tb + 1) * 512)
                    for h in range(4):
                        p = pi % 4; pi += 1
                        self.proj_fm(pp[p], ("pp", p), w, wres, h * 64, 64, hb, hres)
                        P.act(lambda p=p, h=h, sl=sl: nc.scalar.mul(QTa[h][0:64, sl], pp[p][0:64, :], scale),
                              r=[("pp", p)], w=[("QT", h)])
                        p = pi % 4; pi += 1
                        self.proj_fm(pp[p], ("pp", p), w, wres, 256 + h * 64, 64, hb, hres)
                        P.dve(lambda p=p, h=h, sl=sl: nc.vector.tensor_copy(KTa[h][0][0:32, sl], pp[p][0:32, :]),
                              r=[("pp", p)], w=[("KT", h, 0)])
                        P.dve(lambda p=p, h=h, sl=sl: nc.vector.tensor_copy(KTa[h][1][32:64, sl], pp[p][32:64, :]),
                              r=[("pp", p)], w=[("KT", h, 1)])
                    for j in range(4):
                        p = pi % 4; pi += 1
                        self.proj_tm(pp[p], ("pp", p), w, wres, 512, 256, hb, hres, j)
                        self.v_evac(V, pp[p], ("pp", p), tb * 4 + j, j)
                P.flush()
            with ExitStack() as s3:
                onA = [self.sb(s3, "aon", [128, 4, 64], F32) for _ in range(2)]
                cmb = [self.sb(s3, "acmb", [128, 2, 64], F32) for _ in range(2)]
                sqt = [self.sb(s3, "asq", [128, 2, 64], F32) for _ in range(2)]
                ss = [self.sb(s3, "ass", [128, 2], F32) for _ in range(2)]
                onb = [self.sb(s3, "aonb", [128, 128], BF16) for _ in range(2)]
                tp = [self.ps(s3, "atp", [128, 128], BF16) for _ in range(2)]
                stage = [self.sb(s3, "astg", [128, 2, 512], BF16) for _ in range(2)]
                oTv = self.oT[0:256, :].rearrange("(i p) t -> p i t", p=128)
                cnt = [0]

                def out_fn(g, qt, O, Or, rl, rlr):
                    nb = cnt[0] % 2
                    cnt[0] += 1
                    sbi = (qt // 4) % 2
                    q4 = qt % 4
                    for j in range(4):
                        P.dve(lambda j=j: nc.vector.tensor_scalar(
                            onA[nb][:, j, :], O[:, j * 66:j * 66 + 64], rl[:, j:j + 1], None, ALU.mult),
                            r=[Or, rlr], w=[("aon", nb, j)])
                    for hh in range(2):
                        P.dve(lambda hh=hh: nc.vector.scalar_tensor_tensor(
                            cmb[nb][:, hh, :], onA[nb][:, 2 * hh + 1, :], neglam[:, 0:1], onA[nb][:, 2 * hh, :],
                            ALU.mult, ALU.add),
                            r=[("aon", nb, 2 * hh), ("aon", nb, 2 * hh + 1)], w=[("acmb", nb, hh)])
                    P.dve(lambda: nc.vector.tensor_tensor(sqt[nb][:], cmb[nb][:], cmb[nb][:], ALU.mult),
                          r=[("acmb", nb, 0), ("acmb", nb, 1)], w=[("asq", nb)])
                    P.dve(lambda: nc.vector.reduce_sum(out=ss[nb][:], in_=sqt[nb][:], axis=AX.X),
                          r=[("asq", nb)], w=[("ass", nb)])
                    P.dve(lambda: nc.vector.tensor_scalar(ss[nb][:], ss[nb][:], 1.0 / 64, EPS, ALU.mult, ALU.add),
                          r=[("ass", nb)], w=[("ass", nb)])
                    self.rsqrt_pool(ss[nb][:], ss[nb][:], nh[:], [("ass", nb)], [("ass", nb)])
                    for hh in range(2):
                        P.dve(lambda hh=hh: nc.vector.scalar_tensor_tensor(
                            onb[nb][:, hh * 64:(hh + 1) * 64], cmb[nb][:, hh, :], ss[nb][:, hh:hh + 1], gAb[:],
                            ALU.mult, ALU.mult),
                            r=[("acmb", nb, hh), ("ass", nb)], w=[("aonb", nb, hh)])
                    P.pe(lambda: nc.tensor.transpose(tp[nb][:], onb[nb][:], self.ident_b[:]),
                         r=[("aonb", nb, 0), ("aonb", nb, 1)], w=[("atp", nb)])
                    P.dve(lambda: nc.vector.tensor_copy(stage[sbi][:, g, q4 * 128:(q4 + 1) * 128], tp[nb][:]),
                          r=[("atp", nb)], w=[("astg", sbi, g)])
                    if q4 == 3 and g == 1:
                        tb = qt // 4
                        P.dma("sp", lambda: nc.sync.dma_start(out=oTv[:, :, tb * 512:(tb + 1) * 512], in_=stage[sbi][:]),
                              r=[("astg", sbi, 0), ("astg", sbi, 1)], w=[("dram", "oT", "a", tb)])

                def kts_fn(qt):
                    return list(range(qt + 1))

                def pre_fn(g, qt, kt):
                    if kt == qt:
                        return [(self.ident_b[:], corr[:, g, :], [])]
                    return []

                def qk_fn(g, j, qt, kt):
                    h = 2 * g + j // 2
                    return (KTa[h][j % 2][:, kt * 128:(kt + 1) * 128], QTa[h][:, qt * 128:(qt + 1) * 128])

                def v_fn(g, j, kt):
                    return V[:, kt, 2 * g + j // 2, 0:65]
                self.attn_core(s3, "a", 2, kts_fn, pre_fn, qk_fn, v_fn, out_fn)
                P.flush()

    def mixer_c(self, l):
        nc, P = self.nc, self.P
        NR = int(os.environ.get("MK_NR", "14"))
        with ExitStack() as st:
            QTc = [self.sb(st, "cQT", [128, T], BF16) for _ in range(4)]
            KTc = [self.sb(st, "cKT", [128, T], BF16) for _ in range(4)]
            V = self.sb(st, "cV", [128, NT, 4, 66], BF16)
            qiT = [self.sb(st, "cqi", [128, T], BF16) for _ in range(2)]
            kiT = self.sb(st, "cki", [128, T], BF16)
            wsb = self.sb(st, "cw", [128, NT, 8], F32)
            corr = self.sb(st, "ccorr", [128, 512], BF16)
            id4 = self.sb(st, "cid4", [128, 512], BF16)
            P.dve(lambda: nc.vector.memset(V[:], 1.0), w=[("V", tt) for tt in range(NT)])
            for h in range(4):
                P.dve(lambda h=h: nc.vector.memset(QTc[h][:], 0.0), w=[("QT", h)])
                P.dve(lambda h=h: nc.vector.memset(KTc[h][:], 0.0), w=[("KT", h)])
            P.dma("pool", lambda: nc.gpsimd.dma_start(out=corr[:], in_=self.corr_c), w=["ccorr"])
            P.dma("pool", lambda: nc.gpsimd.dma_start(out=id4[:], in_=self.ident4), w=["cid4"])
            for h in range(4):
                P.dma("pool", lambda h=h: nc.gpsimd.dma_start(out=QTc[h][64:67, :], in_=self.augq[h]),
                      r=[("QT", h)], w=[("QTaug", h)])
                P.dma("pool", lambda h=h: nc.gpsimd.dma_start(out=KTc[h][64:67, :], in_=self.augk[h]),
                      r=[("KT", h)], w=[("KTaug", h)])
            with ExitStack() as s2:
                w = self.load_w(s2, "wc", self.w_c[l], 1160)
                wres = [("wc", k) for k in range(KT)]
                load = self.hn_blocks(s2)
                pp = [self.ps(s2, "pp", [128, 512]) for _ in range(4)]
                pi = 0
                for tb in range(T // 512):
                    hb, hres = load(tb)
                    sl = slice(tb * 512, (tb + 1) * 512)
                    for h in range(4):
                        p = pi % 4; pi += 1
                        self.proj_fm(pp[p], ("pp", p), w, wres, h * 64, 64, hb, hres)
                        P.act(lambda p=p, h=h, sl=sl: nc.scalar.mul(QTc[h][0:64, sl], pp[p][0:64, :], 0.125),
                              r=[("pp", p)], w=[("QT", h)])
                        p = pi % 4; pi += 1
                        self.proj_fm(pp[p], ("pp", p), w, wres, 256 + h * 64, 64, hb, hres)
                        P.dve(lambda p=p, h=h, sl=sl: nc.vector.tensor_copy(KTc[h][0:64, sl], pp[p][0:64, :]),
                              r=[("pp", p)], w=[("KT", h)])
                    for i in range(2):
                        p = pi % 4; pi += 1
                        self.proj_fm(pp[p], ("pp", p), w, wres, 768 + i * 128, 128, hb, hres)
                        P.act(lambda p=p, i=i, sl=sl: nc.scalar.copy(qiT[i][:, sl], pp[p][:]),
                              r=[("pp", p)], w=[("cqi", i)])
                    p = pi % 4; pi += 1
                    self.proj_fm(pp[p], ("pp", p), w, wres, 1024, 128, hb, hres)
                    P.dve(lambda p=p, sl=sl: nc.vector.tensor_copy(kiT[:, sl], pp[p][:]), r=[("pp", p)], w=["cki"])
                    for j in range(4):
                        p = pi % 4; pi += 1
                        self.proj_tm(pp[p], ("pp", p), w, wres, 512, 256, hb, hres, j)
                        self.v_evac(V, pp[p], ("pp", p), tb * 4 + j, j)
                        p = pi % 4; pi += 1
                        self.proj_tm(pp[p], ("pp", p), w, wres, 1152, 8, hb, hres, j)
                        P.dve(lambda p=p, tt=tb * 4 + j: nc.vector.tensor_copy(wsb[:, tt, :], pp[p][:, 0:8]),
                              r=[("pp", p)], w=["cw"])
                P.flush()
            with ExitStack() as s3:
                out_fn = self.std_out(s3, "c", 512, ntp=1)
                scores2 = [self.sb(s3, "cscore", [128, T], F32) for _ in range(2)]
                junk = self.sb(s3, "cjunk", [128, T], BF16)
                Mb = [self.sb(s3, "cMb", [128, T], BF16) for _ in range(2)]
                Dg = [[self.sb(s3, "cDg", [128, 128], BF16) for _ in range(8)] for _ in range(2)]
                Qb = [[self.sb(s3, "cQb", [128, 128], BF16) for _ in range(8)] for _ in range(2)]
                R = [self.sb(s3, "cR", [128, 512], BF16) for _ in range(2)]
                X = [self.ps(s3, "cX", [128, 512]) for _ in range(2)]
                SC = self.ps(s3, "cSC", [128, 512])
                st4 = self.sb(s3, "cst", [128, 4 * (NR + 2)], F32)
                thrc = self.sb(s3, "cthrc", [128, 1], F32)
                steps = self.sb(s3, "csteps", [128, NR + 1], F32)
                cpow = self.sb(s3, "ccpow", [128, NR + 1], F32)
                for r in range(NR + 1):
                    P.pool(lambda r=r: nc.gpsimd.memset(cpow[:, r:r + 1], 2.0 ** -(r + 1)), w=["ccpow"])
                P.dve(lambda: nc.vector.memset(thrc[:], -1.0e30), w=["cthrc"])
                for b in range(2):
                    for ih in range(8):
                        P.pool(lambda b=b, ih=ih: nc.gpsimd.memset(Qb[b][ih][:], 0.0), w=[("cQb", b, ih)])
                xi = [0]
                LO, MID, CNT, STP = 0, NR + 2, 2 * (NR + 2), 3 * (NR + 2)

                def before_qt(qt):
                    if qt == 0:
                        do_scores(0)
                        do_scores(1)
                        do_select(0)
                    if qt + 2 < NT:
                        do_scores(qt + 2)
                    if qt + 1 < NT:
                        do_select(qt + 1)

                def do_scores(qt):
                    n = 128 * (qt + 1)
                    b = qt % 2
                    score = scores2[b]
                    qs = slice(qt * 128, (qt + 1) * 128)
                    for ih in range(8):
                        r0 = 32 * (ih % 4)
                        P.pool(lambda ih=ih, r0=r0: nc.gpsimd.tensor_copy(
                            Qb[b][ih][r0:r0 + 32, :], qiT[ih // 4][r0:r0 + 32, qs]), w=[("cQb", b, ih)])
                        P.pool(lambda ih=ih: nc.gpsimd.tensor_scalar(
                            Dg[b][ih][:], self.ident_b[:], wsb[:, qt, ih:ih + 1], None, ALU.mult),
                            w=[("cDg", b, ih)])
                    nkb = (n + 511) // 512
                    for kb in range(nkb):
                        wd = min(512, n - 512 * kb)
                        ks = slice(512 * kb, 512 * kb + wd)
                        xs = []
                        for ih in range(8):
                            xs.append(xi[0] % 2)
                            xi[0] += 1

                        def emit_x(ih, ks=ks, wd=wd):
                            x = xs[ih]
                            P.pe(lambda x=x, ih=ih, ks=ks, wd=wd: nc.tensor.matmul(
                                X[x][:, 0:wd], Qb[b][ih][:], kiT[:, ks], start=True, stop=True),
                                r=[("cQb", b, ih)], w=[("cX", x)])
                        emit_x(0)
                        for ih in range(8):
                            x = xs[ih]
                            P.act(lambda x=x, wd=wd: nc.scalar.activation(out=R[x][:, 0:wd], in_=X[x][:, 0:wd], func=AF.Relu),
                                  r=[("cX", x)], w=[("cR", x)])
                            if ih + 1 < 8:
                                emit_x(ih + 1)
                            P.pe(lambda ih=ih, x=x, wd=wd: nc.tensor.matmul(
                                SC[:, 0:wd], Dg[b][ih][:], R[x][:, 0:wd], start=(ih == 0), stop=(ih == 7)),
                                r=[("cR", x), ("cDg", b, ih)], w=["cSC"])
                        P.act(lambda ks=ks, wd=wd: nc.scalar.copy(score[:, ks], SC[:, 0:wd]),
                              r=["cSC"], w=[("cscore", b)])

                def do_select(qt):
                    n = 128 * (qt + 1)
                    b = qt % 2
                    score = scores2[b]
                    P.dve(lambda: nc.vector.memset(score[0:64, n - 64:n], -3.0e38), r=[("cscore", b)], w=[("cscore", b)])
                    if qt >= 2:
                        W0 = CNT + NR + 1
                        MX = MID
                        P.dve(lambda: nc.vector.tensor_reduce(out=st4[:, MX:MX + 1], in_=score[:, 0:n], axis=AX.X, op=ALU.max),
                              r=[("cscore", b)], w=["cmx"])
                        P.dve(lambda: nc.vector.tensor_reduce(out=st4[:, STP:STP + 1], in_=score[:, 0:320], axis=AX.X, op=ALU.min),
                              r=[("cscore", b)], w=["cmn"])
                        P.dve(lambda: nc.vector.tensor_tensor(st4[:, W0:W0 + 1], st4[:, MX:MX + 1], st4[:, STP:STP + 1], ALU.subtract),
                              r=["cmx", "cmn"], w=["cw0"])
                        P.dve(lambda: nc.vector.tensor_scalar(steps[:], cpow[:], st4[:, W0:W0 + 1], None, ALU.mult),
                              r=["cw0", "ccpow"], w=["csteps"])
                        P.dve(lambda: nc.vector.tensor_tensor(st4[:, LO:LO + 1], st4[:, STP:STP + 1], steps[:, 0:1], ALU.add),
                              r=["cmn", "csteps"], w=[("clo", 0)])
                        for r in range(NR):
                            P.dve(lambda r=r: nc.vector.tensor_scalar(
                                junk[:, 0:n], score[:, 0:n], st4[:, LO + r:LO + r + 1], 0.0, ALU.is_gt, ALU.add,
                                accum_out=st4[:, CNT + r:CNT + r + 1]),
                                r=[("cscore", b), ("clo", r)], w=[("ccnt", r), "cjunk"])
                            P.dve(lambda r=r: nc.vector.tensor_scalar(
                                st4[:, STP + 1 + r:STP + 2 + r], st4[:, CNT + r:CNT + r + 1], 255.5, 0.5, ALU.is_gt, ALU.subtract),
                                r=[("ccnt", r)], w=[("cstp", r)])
                            P.dve(lambda r=r: nc.vector.scalar_tensor_tensor(
                                st4[:, LO + r + 1:LO + r + 2], st4[:, STP + 1 + r:STP + 2 + r], steps[:, r:r + 1],
                                st4[:, LO + r:LO + r + 1], ALU.mult, ALU.add),
                                r=[("cstp", r), ("clo", r), "csteps"], w=[("clo", r + 1)])
                        P.dve(lambda: nc.vector.tensor_tensor(st4[:, MX:MX + 1], st4[:, LO + NR:LO + NR + 1], steps[:, NR:NR + 1], ALU.subtract),
                              r=[("clo", NR), "csteps"], w=["cthr"])
                        thr = st4[:, MX:MX + 1]
                        thr_r = ["cthr"]
                    else:
                        thr = thrc[:, 0:1]
                        thr_r = ["cthrc"]
                    P.dve(lambda: nc.vector.tensor_scalar(Mb[b][:, 0:n], score[:, 0:n], thr, NEG, ALU.is_le, ALU.mult),
                          r=[("cscore", b)] + thr_r, w=[("cMb", b)])

                def kts_fn(qt):
                    return list(range(qt + 1))

                def pre_fn(g, qt, kt):
                    b = qt % 2
                    pre = [(Mb[b][:, kt * 128:(kt + 1) * 128], id4[:], [("cMb", b)])]
                    if kt == qt:
                        pre.append((self.ident_b[:], corr[:], []))
                    return pre

                def qk_fn(g, j, qt, kt):
                    return (KTc[j][:, kt * 128:(kt + 1) * 128], QTc[j][:, qt * 128:(qt + 1) * 128])

                def v_fn(g, j, kt):
                    return V[:, kt, j, 0:65]
                self.attn_core(s3, "c", 1, kts_fn, pre_fn, qk_fn, v_fn, out_fn, before_qt=before_qt, ns=2)
                P.flush()

    def phase_wout(self, l):
        nc, P = self.nc, self.P
        goff = KT
        shoff = 24
        with ExitStack() as st:
            wo = self.load_w(st, "wo", self.w_out[l], D)
            wres = [("wo", k) for k in range(KT)]
            ob = [self.sb(st, "wob", [128, KT, 512], BF16) for _ in range(2)]
            xb = [self.sb(st, "wxb", [128, KT, 512], F32) for _ in range(2)]
            sq = [self.sb(st, "wsq", [128, KT, 512], BF16) for _ in range(2)]
            ms = [self.sb(st, "wms", [128, 512], F32) for _ in range(2)]
            tmp = [self.sb(st, "wtmp", [128, KT, 512], F32) for _ in range(2)]
            hb = [self.sb(st, "whb", [128, KT, 512], BF16) for _ in range(2)]
            py = [self.ps(st, "wpy", [128, 512]) for _ in range(2)]
            pss = [self.ps(st, "wps", [128, 512]) for _ in range(2)]
            oTv = self.oT.rearrange("(kt p) t -> p kt t", p=128)
            xTv = self.xT.rearrange("(kt p) t -> p kt t", p=128)
            hTv = self.hnT.rearrange("(kt p) t -> p kt t", p=128)
            pc = [0]

            def stage_a(tb):
                b = tb % 2
                sl = slice(tb * 512, (tb + 1) * 512)
                P.dma("sp", lambda: nc.sync.dma_start(out=ob[b][:], in_=oTv[:, :, sl]), w=[("wob", b)])
                P.dma("sp", lambda: nc.sync.dma_start(out=xb[b][:], in_=xTv[:, :, sl]),
                      r=[("dram", "xT", tb)], w=[("wxb", b, f) for f in range(KT)])
                for f in range(KT):
                    p = pc[0] % 2
                    pc[0] += 1
                    for k in range(KT):
                        P.pe(lambda f=f, k=k, p=p: nc.tensor.matmul(
                            py[p][:], wo[:, k, f * 128:(f + 1) * 128], ob[b][:, k, :],
                            start=(k == 0), stop=(k == KT - 1)),
                            r=[("wob", b)] + wres, w=[("wpy", p)])
                    P.dve(lambda f=f, p=p: nc.vector.scalar_tensor_tensor(
                        xb[b][:, f, :], py[p][:], self.modv[:, 16 + f:17 + f], xb[b][:, f, :], ALU.mult, ALU.add),
                        r=[("wpy", p), ("wxb", b, f)], w=[("wxb", b, f)])
                P.dma("sp", lambda: nc.sync.dma_start(out=xTv[:, :, sl], in_=xb[b][:]),
                      r=[("wxb", b, f) for f in range(KT)], w=[("dram", "xT", tb)])

            def stage_b(tb):
                b = tb % 2
                sl = slice(tb * 512, (tb + 1) * 512)
                xres = [("wxb", b, f) for f in range(KT)]
                P.act(lambda: nc.scalar.activation(out=sq[b][:], in_=xb[b][:], func=AF.Square),
                      r=xres, w=[("wsq", b)])
                for kt in range(KT):
                    P.pe(lambda kt=kt: nc.tensor.matmul(
                        pss[b][:], self.ones_b[:], sq[b][:, kt, :], start=(kt == 0), stop=(kt == KT - 1)),
                        r=[("wsq", b), "onesb"], w=[("wps", b)])
                P.dve(lambda: nc.vector.tensor_scalar(ms[b][:], pss[b][:], 1.0 / D, EPS, ALU.mult, ALU.add),
                      r=[("wps", b)], w=[("wms", b)])
                P.act(lambda: nc.scalar.activation(out=ms[b][:], in_=ms[b][:], func=AF.Sqrt),
                      r=[("wms", b)], w=[("wms", b)])
                P.dve(lambda: nc.vector.reciprocal(ms[b][:], ms[b][:]), r=[("wms", b)], w=[("wms", b)])
                for kt in range(KT):
                    P.dve(lambda kt=kt: nc.vector.scalar_tensor_tensor(
                        tmp[b][:, kt, :], xb[b][:, kt, :], self.gvec[:, goff + kt:goff + kt + 1],
                        ms[b][:], ALU.mult, ALU.mult),
                        r=[("wxb", b, kt), ("wms", b), "gvec1"], w=[("wtmp", b, kt)])
                    P.act(lambda kt=kt: nc.scalar.activation(
                        out=hb[b][:, kt, :], in_=tmp[b][:, kt, :], func=AF.Identity,
                        bias=self.modv[:, shoff + kt:shoff + kt + 1], scale=1.0),
                        r=[("wtmp", b, kt), "modv"], w=[("whb", b, kt)])
                P.dma("sp", lambda: nc.sync.dma_start(out=hTv[:, :, sl], in_=hb[b][:]),
                      r=[("whb", b, kt) for kt in range(KT)], w=[("dram", "hnT", tb)])

            stage_a(0)
            for tb in range(T // 512):
                if tb + 1 < T // 512:
                    stage_a(tb + 1)
                stage_b(tb)
            P.flush()

    def phase_ffn(self, l, moe):
        nc, P = self.nc, self.P
        E = NEXP if moe else 1
        NJ = (D_FFE if moe else D_FF) // 128
        NH = 2
        JH = NJ // NH
        w13 = self.moe_w13 if moe else self.ffn_w13
        w2 = self.moe_w2 if moe else self.ffn_w2
        NB = 1024
        with ExitStack() as st:
            hb = self.sb(st, "fhb", [128, KT, NB], BF16)
            xb = self.sb(st, "fxb", [128, KT, NB], F32)
            g = self.sb(st, "fg", [128, JH, NB], BF16)
            w2e = [self.sb(st, "fw2", [128, JH, D], BF16) for _ in range(2)]
            wj = [self.sb(st, "fw13", [128, 2, KT, 128], BF16) for _ in range(6)]
            sl_t = [self.sb(st, "fsl", [128, 512], BF16) for _ in range(2)]
            pa = [self.ps(st, "fpa", [128, 512]) for _ in range(2)]
            pb = [self.ps(st, "fpb", [128, 512]) for _ in range(2)]
            pyy = [self.ps(st, "fpy", [128, 512]) for _ in range(2)]
            ytmp = [self.sb(st, "fyt", [128, 512], F32) for _ in range(2)]
            hTv = self.hnT.rearrange("(kt p) t -> p kt t", p=128)
            xTv = self.xT.rearrange("(kt p) t -> p kt t", p=128)
            if moe:
                cb = self.sb(st, "fcb", [128, E, NB], BF16)
                rt = self.sb(st, "frt", [128, KT, 8], BF16)
                lg = self.sb(st, "flg", [128, 8], F32)
                m8 = self.sb(st, "fm8", [128, 8], F32)
                sc4 = self.sb(st, "fsc4", [128, 8], F32)
                c1 = self.sb(st, "fc1", [128, 8], F32)
                c2 = self.sb(st, "fc2", [128, 8], F32)
                dg = [self.sb(st, "fdg", [128, 128], BF16) for _ in range(2)]
                plg = self.ps(st, "fplg", [128, 512])
                pcb = self.ps(st, "fpcb", [128, 512])
                for k in range(KT):
                    P.dma("pool", lambda k=k: nc.gpsimd.dma_start(out=rt[:, k, :], in_=self.router[k]), w=["frt"])
            wi = 0
            ai = 0
            yi = 0
            w2i = 0
            for tb in range(T // NB):
                sl = slice(tb * NB, (tb + 1) * NB)
                P.dma("sp", lambda sl=sl: nc.sync.dma_start(out=hb[:], in_=hTv[:, :, sl]), w=["fhb"])
                P.dma("sp", lambda sl=sl: nc.sync.dma_start(out=xb[:], in_=xTv[:, :, sl]),
                      w=[("fxb", f, s) for f in range(KT) for s in range(2)])
                if moe:
                    for tt in range(NB // 128):
                        for k in range(KT):
                            P.pe(lambda k=k, tt=tt: nc.tensor.matmul(
                                plg[:, 0:8], hb[:, k, tt * 128:(tt + 1) * 128], rt[:, k, :],
                                start=(k == 0), stop=(k == KT - 1)), r=["fhb", "frt"], w=["fplg"])
                        P.dve(lambda: nc.vector.tensor_copy(lg[:], plg[:, 0:8]), r=["fplg"], w=["flg"])
                        P.dve(lambda: nc.vector.max(out=m8[:], in_=lg[:]), r=["flg"], w=["fm8"])
                        P.dve(lambda: nc.vector.tensor_tensor(sc4[:, 0:1], m8[:, 1:2], m8[:, 0:1], ALU.subtract),
                              r=["fm8"], w=["fsc4a"])
                        P.act(lambda: nc.scalar.activation(out=sc4[:, 1:2], in_=sc4[:, 0:1], func=AF.Exp),
                              r=["fsc4a"], w=["fsc4b"])
                        P.dve(lambda: nc.vector.tensor_scalar(sc4[:, 2:3], sc4[:, 1:2], 1.0, None, ALU.add),
                              r=["fsc4b"], w=["fsc4c"])
                        P.dve(lambda: nc.vector.reciprocal(sc4[:, 3:4], sc4[:, 2:3]), r=["fsc4c"], w=["fsc4d"])
                        P.dve(lambda: nc.vector.tensor_tensor(sc4[:, 4:5], sc4[:, 1:2], sc4[:, 3:4], ALU.mult),
                              r=["fsc4b", "fsc4d"], w=["fsc4e"])
                        P.dve(lambda: nc.vector.tensor_scalar(c1[:], lg[:], m8[:, 0:1], sc4[:, 3:4], ALU.is_equal, ALU.mult),
                              r=["flg", "fm8", "fsc4d"], w=["fc1"])
                        P.dve(lambda: nc.vector.tensor_scalar(c2[:], lg[:], m8[:, 1:2], sc4[:, 4:5], ALU.is_equal, ALU.mult),
                              r=["flg", "fm8", "fsc4e"], w=["fc2"])
                        P.dve(lambda: nc.vector.tensor_tensor(c1[:], c1[:], c2[:], ALU.add),
                              r=["fc1", "fc2"], w=["fc1"])
                        for e in range(E):
                            d = e % 2
                            P.dve(lambda e=e, d=d: nc.vector.tensor_scalar(
                                dg[d][:], self.ident_b[:], c1[:, e:e + 1], None, ALU.mult),
                                r=["fc1", "identb"], w=[("fdg", d)])
                            P.pe(lambda e=e, d=d: nc.tensor.matmul(
                                pcb[:, (e % 4) * 128:(e % 4 + 1) * 128], self.ones_b[:], dg[d][:],
                                start=True, stop=True, skip_group_check=True),
                                r=[("fdg", d), "onesb"], w=["fpcb"])
                            if e % 4 == 3:
                                for q in range(4):
                                    ee = e - 3 + q
                                    P.dve(lambda ee=ee, q=q, tt=tt: nc.vector.tensor_copy(
                                        cb[:, ee, tt * 128:(tt + 1) * 128], pcb[:, q * 128:(q + 1) * 128]),
                                        r=["fpcb"], w=[("fcb", ee)])
                for e in range(E):
                    for hh in range(NH):
                        wb2 = w2i % 2; w2i += 1
                        for q in range(JH):
                            P.dma("pool", lambda e=e, hh=hh, q=q, wb2=wb2: nc.gpsimd.dma_start(
                                out=w2e[wb2][:, q, :], in_=w2[l // 2 if moe else 0, e, hh * JH + q]),
                                w=[("fw2", wb2)])
                        for jj in range(JH):
                            j = hh * JH + jj
                            wbi = wi % 6; wi += 1
                            for m in range(2):
                                P.dma("pool", lambda e=e, j=j, m=m, wbi=wbi: nc.gpsimd.dma_start(
                                    out=wj[wbi][:, m, :, :], in_=w13[l // 2 if moe else 0, e, j, m]),
                                    w=[("fw13", wbi)])
                            for s in range(2):
                                a = ai % 2; ai += 1
                                cs = slice(s * 512, (s + 1) * 512)
                                for k in range(KT):
                                    P.pe(lambda k=k, a=a, wbi=wbi, cs=cs: nc.tensor.matmul(
                                        pa[a][:], wj[wbi][:, 0, k, :], hb[:, k, cs], start=(k == 0), stop=(k == KT - 1)),
                                        r=["fhb", ("fw13", wbi)], w=[("fpa", a)])
                                for k in range(KT):
                                    P.pe(lambda k=k, a=a, wbi=wbi, cs=cs: nc.tensor.matmul(
                                        pb[a][:], wj[wbi][:, 1, k, :], hb[:, k, cs], start=(k == 0), stop=(k == KT - 1)),
                                        r=["fhb", ("fw13", wbi)], w=[("fpb", a)])
                                if "ffn2" in self.dbg2s:
                                    continue
                                P.act(lambda a=a: nc.scalar.activation(out=sl_t[a][:], in_=pa[a][:], func=AF.Silu),
                                      r=[("fpa", a)], w=[("fsl", a)])
                                if "ffn3" in self.dbg2s:
                                    continue
                                if moe:
                                    P.dve(lambda a=a, e=e, cs=cs: nc.vector.tensor_tensor(
                                        sl_t[a][:], sl_t[a][:], cb[:, e, cs], ALU.mult),
                                        r=[("fsl", a), ("fcb", e)], w=[("fsl", a)])
                                P.dve(lambda a=a, jj=jj, cs=cs: nc.vector.tensor_tensor(
                                    g[:, jj, cs], pb[a][:], sl_t[a][:], ALU.mult),
                                    r=[("fpb", a), ("fsl", a)], w=[("fg", jj, s)])
                        for f in range(KT):
                            if self.dbg2s & {"ffn2", "ffn3", "ffn4"}:
                                break
                            for s in range(2):
                                y = yi % 2; yi += 1
                                cs = slice(s * 512, (s + 1) * 512)
                                for jj in range(JH):
                                    P.pe(lambda jj=jj, f=f, cs=cs, y=y, wb2=wb2: nc.tensor.matmul(
                                        pyy[y][:], w2e[wb2][:, jj, f * 128:(f + 1) * 128], g[:, jj, cs],
                                        start=(jj == 0), stop=(jj == JH - 1)),
                                        r=[("fw2", wb2), ("fg", jj, s)], w=[("fpy", y)])
                                if "ffn5" in self.dbg2s:
                                    continue
                                if True:
                                    P.dve(lambda f=f, y=y: nc.vector.tensor_scalar(
                                        ytmp[y][:], pyy[y][:], self.modv[:, 40 + f:41 + f], None, ALU.mult),
                                        r=[("fpy", y)], w=[("fyt", y)])
                                    P.dve(lambda f=f, cs=cs, y=y: nc.vector.tensor_tensor(
                                        xb[:, f, cs], xb[:, f, cs], ytmp[y][:], ALU.add),
                                        r=[("fyt", y), ("fxb", f, s)], w=[("fxb", f, s)])
                                    continue
                                P.dve(lambda f=f, cs=cs, y=y: nc.vector.scalar_tensor_tensor(
                                    xb[:, f, cs], pyy[y][:], self.modv[:, 40 + f:41 + f], xb[:, f, cs], ALU.mult, ALU.add),
                                    r=[("fpy", y), ("fxb", f, s)], w=[("fxb", f, s)])
                P.dma("sp", lambda sl=sl: nc.sync.dma_start(out=xTv[:, :, sl], in_=xb[:]),
                      r=[("fxb", f, s) for f in range(KT) for s in range(2)], w=[("dram", "xT", tb)])
                P.flush()

    def final_norm(self):
        nc, P = self.nc, self.P
        with ExitStack() as st:
            fg = self.sb(st, "fing", [128, KT], F32)
            xb = [self.sb(st, "ox", [128, KT, 512], F32) for _ in range(2)]
            sq = [self.sb(st, "osq", [128, KT, 512], BF16) for _ in range(2)]
            ms = [self.sb(st, "oms", [128, 512], F32) for _ in range(2)]
            yb = [self.sb(st, "oy", [128, KT, 512], F32) for _ in range(2)]
            ot = [self.sb(st, "oot", [128, D], F32) for _ in range(2)]
            pss = [self.ps(st, "ops", [128, 512]) for _ in range(2)]
            ptp = [self.ps(st, "optp", [128, 512]) for _ in range(2)]
            xTv = self.xT.rearrange("(kt p) t -> p kt t", p=128)
            P.dma("sp", lambda: nc.sync.dma_start(out=fg[:], in_=self.fin_g), w=["fing"])
            tc = [0]
            pc = [0]
            def stage1(tb):
                b = tb % 2
                sl = slice(tb * 512, (tb + 1) * 512)
                P.dma("sp", lambda b=b, sl=sl: nc.sync.dma_start(out=xb[b][:], in_=xTv[:, :, sl]), w=[("ox", b)])
                P.act(lambda b=b: nc.scalar.activation(out=sq[b][:], in_=xb[b][:], func=AF.Square),
                      r=[("ox", b)], w=[("osq", b)])
                for k in range(KT):
                    P.pe(lambda b=b, k=k: nc.tensor.matmul(pss[b][:], self.ones_b[:], sq[b][:, k, :],
                                                           start=(k == 0), stop=(k == KT - 1)),
                         r=[("osq", b), "onesb"], w=[("ops", b)])
                P.dve(lambda b=b: nc.vector.tensor_scalar(ms[b][:], pss[b][:], 1.0 / D, EPS, ALU.mult, ALU.add),
                      r=[("ops", b)], w=[("oms", b)])
                P.act(lambda b=b: nc.scalar.activation(out=ms[b][:], in_=ms[b][:], func=AF.Sqrt),
                      r=[("oms", b)], w=[("oms", b)])
                P.dve(lambda b=b: nc.vector.reciprocal(ms[b][:], ms[b][:]), r=[("oms", b)], w=[("oms", b)])
                for k in range(KT):
                    P.dve(lambda b=b, k=k: nc.vector.scalar_tensor_tensor(
                        yb[b][:, k, :], xb[b][:, k, :], fg[:, k:k + 1], ms[b][:], ALU.mult, ALU.mult),
                        r=[("ox", b), ("oms", b), "fing"], w=[("oy", b, k)])

            def stage2(tb):
                b = tb % 2
                for j in range(4):
                    o = tc[0] % 2; tc[0] += 1
                    for half in range(2):
                        p = pc[0] % 2; pc[0] += 1
                        for kk in range(4):
                            k = half * 4 + kk
                            P.pe(lambda b=b, k=k, kk=kk, j=j, p=p: nc.tensor.transpose(
                                ptp[p][:, kk * 128:(kk + 1) * 128], yb[b][:, k, j * 128:(j + 1) * 128], self.ident_f[:]),
                                r=[("oy", b, k), "identf"], w=[("optp", p)])
                        if half == 0:
                            P.dve(lambda o=o, p=p: nc.vector.tensor_copy(ot[o][:, 0:512], ptp[p][:]),
                                  r=[("optp", p)], w=[("oot", o, 0)])
                        else:
                            P.act(lambda o=o, p=p: nc.scalar.copy(ot[o][:, 512:1024], ptp[p][:]),
                                  r=[("optp", p)], w=[("oot", o, 1)])
                    row0 = tb * 512 + j * 128
                    P.dma("sp", lambda o=o, row0=row0: nc.sync.dma_start(out=self.out[row0:row0 + 128, :], in_=ot[o][:]),
                          r=[("oot", o, 0), ("oot", o, 1)], w=[("dram", "out", row0)])
            stage1(0)
            for tb in range(T // 512):
                if tb + 1 < T // 512:
                    stage1(tb + 1)
                stage2(tb)
            P.flush()

    def layer(self, l):
        self.phase_mod(l)
        self.phase_norm(0)
        if "a" in self.mixers:
            self.mixer_a(l)
        if "b" in self.mixers:
            self.mixer_b(l)
        if "c" in self.mixers:
            self.mixer_c(l)
        if "d" in self.mixers:
            self.mixer_d(l)
        if self.stop_after == ("mix", l):
            return "stop"
        self.phase_wout(l)
        if self.stop_after == ("wout", l):
            return "stop"
        if self.stop_after == ("preffn", l):
            return "stop"
        self.phase_ffn(l, moe=(l % 2 == 1))


def host_prep(inputs, b, shared=None):
    f = np.float32
    m = dict(shared) if shared is not None else host_shared(inputs)
    m["x"] = np.ascontiguousarray(inputs["x"][b], dtype=f)
    m["c"] = np.ascontiguousarray(np.asarray(inputs["c"][b], dtype=f).reshape(KT, 128).T)
    return m


def host_shared(inputs):
    f = np.float32
    m = {}
    m["ada_w"] = np.ascontiguousarray(np.asarray(inputs["ada_w"], dtype=f).reshape(DEPTH, KT, 128, 6 * D))
    m["ada_b"] = np.ascontiguousarray(np.asarray(inputs["ada_b"], dtype=f).reshape(DEPTH, 48, 128).transpose(0, 2, 1))
    m["mix_g"] = np.ascontiguousarray(np.asarray(inputs["mix_norm_g"], dtype=f).reshape(DEPTH, KT, 128).transpose(0, 2, 1))
    m["ffn_g"] = np.ascontiguousarray(np.asarray(inputs["ffn_norm_g"], dtype=f).reshape(DEPTH, KT, 128).transpose(0, 2, 1))
    m["fin_g"] = np.ascontiguousarray(np.asarray(inputs["final_norm_g"], dtype=f).reshape(KT, 128).T)
    m["ident"] = np.eye(128, dtype=f)
    w_in = np.asarray(inputs["w_in"], dtype=f)

    def cols(a, b):
        return w_in[:, :, a:b]
    sl_ = np.arange(128)[:, None]
    tl_ = np.arange(128)[None, :]

    def tiles(w):
        return np.ascontiguousarray(w.reshape(DEPTH, KT, 128, w.shape[-1]))
    m["w_out"] = np.ascontiguousarray(np.asarray(inputs["w_out"], dtype=f).reshape(DEPTH, KT, 128, D))

    def w13_layout(w1, w3):
        E_, _, F_ = w1.shape
        a = np.stack([w1, w3], axis=1)
        a = a.reshape(E_, 2, KT, 128, F_ // 128, 128)
        return np.ascontiguousarray(a.transpose(0, 4, 1, 3, 2, 5))

    m["ffn_w13"] = w13_layout(np.asarray(inputs["ffn_w1"], dtype=f), np.asarray(inputs["ffn_w3"], dtype=f))[None]
    m["ffn_w2"] = np.ascontiguousarray(np.asarray(inputs["ffn_w2"], dtype=f).reshape(1, 1, D_FF // 128, 128, D))
    m["moe_w13"] = w13_layout(np.asarray(inputs["moe_w1"], dtype=f)[0], np.asarray(inputs["moe_w3"], dtype=f)[0])[None]
    m["moe_w2"] = np.ascontiguousarray(np.asarray(inputs["moe_w2"], dtype=f).reshape(1, NEXP, D_FFE // 128, 128, D))
    m["router"] = np.ascontiguousarray(np.asarray(inputs["moe_router"], dtype=f)[0].reshape(KT, 128, 8))
    perm = np.concatenate([np.arange(16, 32), np.arange(0, 16)])
    z64 = np.zeros((DEPTH, D, 64), f)
    kr = cols(1152, 1184)
    m["w_b"] = tiles(np.concatenate([cols(768, 1024), cols(1024, 1152), z64, kr, z64, kr[:, :, perm]], axis=-1))
    wuq = np.asarray(inputs["mla_w_uq"], dtype=f).reshape(DEPTH, 256, 4, 96)
    wuqr = np.zeros_like(wuq)
    wuqr[:, :, :, 64:96] = wuq[:, :, :, 64:96][:, :, :, perm]
    m["w_uq"] = np.ascontiguousarray(wuq.reshape(DEPTH, 2, 128, 384))
    m["w_uqr"] = np.ascontiguousarray(wuqr.reshape(DEPTH, 2, 128, 384))
    wukv = np.asarray(inputs["mla_w_ukv"], dtype=f).reshape(DEPTH, 128, 4, 128)
    m["w_ukvk"] = np.ascontiguousarray(wukv[:, :, :, 0:64].reshape(DEPTH, 1, 128, 256))
    m["w_ukvv"] = np.ascontiguousarray(wukv[:, :, :, 64:128].reshape(DEPTH, 1, 128, 256))
    m["gq"] = np.ascontiguousarray(np.asarray(inputs["mla_q_norm_g"], dtype=f).reshape(DEPTH, 2, 128).transpose(0, 2, 1))
    m["gkv"] = np.ascontiguousarray(np.asarray(inputs["mla_kv_norm_g"], dtype=f).reshape(DEPTH, 128, 1))
    inv = (np.float32(10000.0) ** (-np.arange(0, 32, 2, dtype=f) / np.float32(32))).astype(f)
    ang = (np.arange(T, dtype=f)[:, None] * inv[None, :]).astype(f)
    cs_, sn_ = np.cos(ang).astype(f), np.sin(ang).astype(f)
    rope = np.zeros((128, 2, T), f)
    rope[64:80, 0] = cs_.T; rope[80:96, 0] = cs_.T
    rope[64:80, 1] = -sn_.T; rope[80:96, 1] = sn_.T
    m["rope"] = rope
    chunkmask = np.where(sl_ // 64 > tl_ // 64, f(NEG), f(0.0)).astype(f)
    m["diagmask"] = np.ascontiguousarray(np.tile(chunkmask, (1, 4)))
    m["w_a"] = tiles(np.concatenate([cols(0, 256), cols(256, 512), cols(512, 768)], axis=-1))
    slopes = (2.0 ** (-8.0 * np.arange(1, 5, dtype=f) / 4)).astype(f)
    tpos = np.arange(T, dtype=f)
    augq = np.zeros((4, 3, T), f); augk = np.zeros((4, 3, T), f)
    for h in range(4):
        augq[h, 0] = 1.0; augq[h, 1] = 1.0; augq[h, 2] = -slopes[h] * tpos
        augk[h, 0] = slopes[h] * (64.0 * (tpos // 64)); augk[h, 1] = slopes[h] * (tpos % 64); augk[h, 2] = 1.0
    m["augq"] = augq; m["augk"] = augk

    def corr_tile(sig):
        c = np.where(sl_ > tl_, -2.0 * sig * (sl_ - tl_), 0.0).astype(f)
        return np.where(sl_ // 64 > tl_ // 64, f(NEG), c).astype(f)
    ca = np.zeros((2, 128, 4, 128), f)
    for g in range(2):
        for j in range(4):
            ca[g, :, j, :] = corr_tile(slopes[2 * g + j // 2])
    m["corr_a"] = np.ascontiguousarray(ca.reshape(2, 128, 512))
    dl = np.asarray(inputs["diff_lambda"], dtype=f).reshape(DEPTH, 1, 128)
    m["dlam"] = np.ascontiguousarray(np.broadcast_to(dl, (DEPTH, 128, 128)))
    dg_ = np.asarray(inputs["diff_norm_g"], dtype=f).reshape(DEPTH, 1, 64)
    m["dng"] = np.ascontiguousarray(np.broadcast_to(dg_, (DEPTH, 128, 64)))
    ki = cols(2208, 2240)
    m["w_c"] = tiles(np.concatenate([cols(1184, 1440), cols(1440, 1696), cols(1696, 1952), cols(1952, 2208),
                                     ki, ki, ki, ki, cols(2240, 2248)], axis=-1))
    cc = np.zeros((128, 4, 128), f)
    for j in range(4):
        cc[:, j, :] = corr_tile(slopes[j])
    m["corr_c"] = np.ascontiguousarray(cc.reshape(128, 512))
    m["ident4"] = np.ascontiguousarray(np.tile(np.eye(128, dtype=f), (1, 4)))
    m["w_d"] = tiles(np.concatenate([cols(2248, 2504), cols(2504, 2760), cols(2760, 3016)], axis=-1))
    rb = np.asarray(inputs["band_rel_bias"], dtype=f)
    bd = np.empty((DEPTH, 5, 128, 4, 128), f)
    for dd in range(5):
        delta = 128 * dd + tl_ - sl_
        idx = np.clip(delta, -128, 128) + 128
        dc = (128 * dd + tl_) // 64 - sl_ // 64
        ok = (dc >= 0) & (dc <= 8)
        for h in range(4):
            g = rb[:, h, :][:, idx]
            bd[:, dd, :, h, :] = np.where(ok[None], g, f(NEG))
    m["bias_d"] = np.ascontiguousarray(bd.reshape(DEPTH, 5, 128, 512))
    return m


def kernel(**inputs):
    bld = Builder()
    nc = bld.build()
    shared = host_shared(inputs)
    in_maps = []
    for b in range(8):
        m = host_prep(inputs, b, shared)
        in_maps.append({k: v for k, v in m.items() if k in bld.inputs})
    res = run_bass_kernel_spmd(nc, in_maps, core_ids=list(range(8)))
    return np.stack([np.asarray(r["out"]) for r in res.results], axis=0).astype(np.float32)
```
